# Optimizing a Trainium2 kernel written in Bass

```python
import math
import jax, jax.numpy as jnp
from jax import lax
import numpy as np

D_MODEL = 2048
BATCH = 2
SEQ = 16384
DEPTH = 2

MEM_LEN = 256
EPS = 1e-6
NEG_BIG = -1e30

MOBA_HEADS = 8
MOBA_HEAD_DIM = 64
MOBA_WIDTH = MOBA_HEADS * MOBA_HEAD_DIM
MOBA_BLOCK = 256
MOBA_TOPK = 3
MOBA_QCHUNK = 64

SSM_WIDTH = D_MODEL // 2
SSM_HEAD_DIM = 64
SSM_HEADS = SSM_WIDTH // SSM_HEAD_DIM
SSM_GROUPS = 4
SSM_STATE = 128
SSM_CONV = 4
SSM_CHUNK = 128
SSM_CONV_DIM = SSM_WIDTH + 2 * SSM_GROUPS * SSM_STATE

MLSTM_HEADS = 4
MLSTM_HEAD_DIM = 128
MLSTM_WIDTH = MLSTM_HEADS * MLSTM_HEAD_DIM
MLSTM_CONV = 4
MLSTM_CHUNK = 128

MIX_WIDTH = MOBA_WIDTH + SSM_WIDTH + MLSTM_WIDTH

XATTN_HEADS = 4
XATTN_HEAD_DIM = 128
XATTN_WIDTH = XATTN_HEADS * XATTN_HEAD_DIM

PEER_HEADS = 8
PEER_NKEYS = 128
PEER_EXPERTS = PEER_NKEYS * PEER_NKEYS
PEER_KEY_DIM = 128
PEER_HALF = PEER_KEY_DIM // 2
PEER_TOPK = 16
PEER_TOKEN_BLOCK = 128

IN_SIZES = [MOBA_WIDTH, MOBA_WIDTH, MOBA_WIDTH,
            SSM_WIDTH, SSM_CONV_DIM, SSM_HEADS,
            MLSTM_WIDTH, MLSTM_WIDTH, MLSTM_WIDTH, MLSTM_HEADS, MLSTM_HEADS]
IN_WIDTH = sum(IN_SIZES)
IN_SPLITS = [sum(IN_SIZES[:i + 1]) for i in range(len(IN_SIZES) - 1)]

kernel_name = "hybrid_moba_ssd_mlstm_peer_trunk"


def rms_norm(x, g):
    xf = x.astype(jnp.float32)
    y = xf * lax.rsqrt(jnp.mean(xf * xf, axis=-1, keepdims=True) + EPS)
    return (y * g.astype(jnp.float32)).astype(x.dtype)


def causal_dwconv(x, w, b):
    K, C = w.shape
    y = lax.conv_general_dilated(
        x, w[:, None, :].astype(x.dtype), window_strides=(1,), padding=[(K - 1, 0)],
        dimension_numbers=('NWC', 'WIO', 'NWC'), feature_group_count=C)
    return y + b.astype(x.dtype)


def alibi_slopes(n_heads):
    return jnp.asarray(2.0 ** (-8.0 * np.arange(1, n_heads + 1) / n_heads), dtype=jnp.float32)


def moba_attention(q, k, v):
    B, S, H, hd = q.shape
    L = MOBA_BLOCK
    QC = MOBA_QCHUNK
    S_pad = -(-S // L) * L
    pad = S_pad - S
    if pad:
        cfg = ((0, 0), (0, pad), (0, 0), (0, 0))
        q, k, v = jnp.pad(q, cfg), jnp.pad(k, cfg), jnp.pad(v, cfg)
    nb = S_pad // L
    n_sel = min(MOBA_TOPK, nb)
    kb = k.transpose(0, 2, 1, 3).reshape(B, H, nb, L, hd)
    vb = v.transpose(0, 2, 1, 3).reshape(B, H, nb, L, hd)
    k_mean = jnp.mean(kb.astype(jnp.float32), axis=3).astype(q.dtype)
    slopes = alibi_slopes(H)
    scale = hd ** -0.5
    nq = S_pad // QC
    q_chunks = q.transpose(0, 2, 1, 3).reshape(B, H, nq, QC, hd).transpose(2, 0, 1, 3, 4)
    b_idx = jnp.arange(B)[:, None, None, None]
    h_idx = jnp.arange(H)[None, :, None, None]

    def one_chunk(args):
        c, qc = args
        start = c * QC
        blk = start // L
        t = (start + jnp.arange(QC)).astype(jnp.float32)
        gate = jnp.einsum('bhqd,bhnd->bhqn', qc, k_mean).astype(jnp.float32)
        gate = jnp.where(jnp.arange(nb) < blk, gate, -jnp.inf)
        _, sel = lax.top_k(gate, n_sel)
        valid = sel < blk
        k_sel = kb[b_idx, h_idx, sel]
        v_sel = vb[b_idx, h_idx, sel]
        pos_sel = (sel[..., None] * L + jnp.arange(L)).astype(jnp.float32)
        s_sel = jnp.einsum('bhqd,bhqjld->bhqjl', qc, k_sel).astype(jnp.float32) * scale
        s_sel = s_sel - slopes[None, :, None, None, None] * (t[None, None, :, None, None] - pos_sel)
        s_sel = jnp.where(valid[..., None], s_sel, -jnp.inf)
        k_own = lax.dynamic_index_in_dim(kb, blk, axis=2, keepdims=False)
        v_own = lax.dynamic_index_in_dim(vb, blk, axis=2, keepdims=False)
        pos_own = (blk * L + jnp.arange(L)).astype(jnp.float32)
        dist = t[:, None] - pos_own[None, :]
        s_own = jnp.einsum('bhqd,bhld->bhql', qc, k_own).astype(jnp.float32) * scale
        s_own = s_own - slopes[None, :, None, None] * dist
        s_own = jnp.where(dist >= 0, s_own, -jnp.inf)
        scores = jnp.concatenate([s_sel.reshape(B, H, QC, n_sel * L), s_own], axis=-1)
        p = jax.nn.softmax(scores, axis=-1).astype(v.dtype)
        p_sel = p[..., :n_sel * L].reshape(B, H, QC, n_sel, L)
        p_own = p[..., n_sel * L:]
        return (jnp.einsum('bhqjl,bhqjld->bhqd', p_sel, v_sel)
                + jnp.einsum('bhql,bhld->bhqd', p_own, v_own))

    out = lax.map(one_chunk, (jnp.arange(nq), q_chunks))
    out = out.transpose(1, 0, 3, 2, 4).reshape(B, S_pad, H * hd)
    return out[:, :S]


def mamba2_ssd(z, xbc, dt_raw, conv_w, conv_b, dt_bias, A_log, D_skip, norm_g):
    Bsz, S, _ = z.shape
    G, N, P, H = SSM_GROUPS, SSM_STATE, SSM_HEAD_DIM, SSM_HEADS
    R = H // G
    L = SSM_CHUNK
    nc = S // L
    xbc = jax.nn.silu(causal_dwconv(xbc, conv_w, conv_b)).astype(jnp.float32)
    xs, Bm, Cm = jnp.split(xbc, [SSM_WIDTH, SSM_WIDTH + G * N], axis=-1)
    dt = jax.nn.softplus(dt_raw.astype(jnp.float32) + dt_bias.astype(jnp.float32))
    A = -jnp.exp(A_log.astype(jnp.float32))
    xh = xs.reshape(Bsz, nc, L, G, R, P)
    Xdt = xh * dt.reshape(Bsz, nc, L, G, R)[..., None]
    a = (dt * A).reshape(Bsz, nc, L, G, R).transpose(0, 1, 3, 4, 2)
    cs = jnp.cumsum(a, axis=-1)
    Bm = Bm.reshape(Bsz, nc, L, G, N)
    Cm = Cm.reshape(Bsz, nc, L, G, N)
    causal = jnp.tril(jnp.ones((L, L), dtype=bool))
    decay = jnp.exp(jnp.where(causal, cs[..., :, None] - cs[..., None, :], -jnp.inf))
    CB = jnp.einsum('bclgn,bcsgn->bcgls', Cm, Bm)
    y_diag = jnp.einsum('bcgrls,bcsgrp->bclgrp', CB[:, :, :, None] * decay, Xdt)
    decay_to_end = jnp.exp(cs[..., -1:] - cs)
    chunk_states = jnp.einsum('bclgn,bcgrl,bclgrp->bcgrpn', Bm, decay_to_end, Xdt)
    chunk_decay = jnp.exp(cs[..., -1])

    def step(h, inp):
        st, dec = inp
        return h * dec[..., None, None] + st, h

    h0 = jnp.zeros((Bsz, G, R, P, N), jnp.float32)
    _, h_in = lax.scan(step, h0, (chunk_states.transpose(1, 0, 2, 3, 4, 5),
                                  chunk_decay.transpose(1, 0, 2, 3)))
    h_in = h_in.transpose(1, 0, 2, 3, 4, 5)
    y_off = jnp.einsum('bclgn,bcgrpn,bcgrl->bclgrp', Cm, h_in, jnp.exp(cs))
    y = y_diag + y_off + D_skip.astype(jnp.float32).reshape(G, R)[:, :, None] * xh
    y = y.reshape(Bsz, S, SSM_WIDTH) * jax.nn.silu(z.astype(jnp.float32))
    yg = y.reshape(Bsz, S, G, SSM_WIDTH // G)
    yg = yg * lax.rsqrt(jnp.mean(yg * yg, axis=-1, keepdims=True) + EPS)
    return (yg.reshape(Bsz, S, SSM_WIDTH) * norm_g.astype(jnp.float32)).astype(z.dtype)


def mlstm(u, v, o_pre, i_pre, f_pre, conv_w, conv_b, wq, wk, i_bias, f_bias, norm_g):
    Bsz, S, _ = u.shape
    H, Dh, L = MLSTM_HEADS, MLSTM_HEAD_DIM, MLSTM_CHUNK
    nc = S // L
    c = jax.nn.silu(causal_dwconv(u, conv_w, conv_b)).reshape(Bsz, S, H, Dh)
    q = jnp.einsum('bshd,hde->bhse', c, wq.astype(c.dtype)).astype(jnp.float32)
    k = jnp.einsum('bshd,hde->bhse', c, wk.astype(c.dtype)).astype(jnp.float32) * (Dh ** -0.5)
    vh = v.reshape(Bsz, S, H, Dh).transpose(0, 2, 1, 3).astype(jnp.float32)
    q = q.reshape(Bsz, H, nc, L, Dh)
    k = k.reshape(Bsz, H, nc, L, Dh)
    vh = vh.reshape(Bsz, H, nc, L, Dh)
    ig = (i_pre.astype(jnp.float32) + i_bias.astype(jnp.float32)).transpose(0, 2, 1).reshape(Bsz, H, nc, L)
    lf = jax.nn.log_sigmoid(f_pre.astype(jnp.float32) + f_bias.astype(jnp.float32)).transpose(0, 2, 1).reshape(Bsz, H, nc, L)
    b = jnp.cumsum(lf, axis=-1)
    causal = jnp.tril(jnp.ones((L, L), dtype=bool))
    Dm = jnp.where(causal, b[..., :, None] - b[..., None, :] + ig[..., None, :], -jnp.inf)
    m_intra = jnp.max(Dm, axis=-1)
    g_end = b[..., -1:] - b + ig
    a_loc = jnp.max(g_end, axis=-1)
    w_end = jnp.exp(g_end - a_loc[..., None])
    C_loc = jnp.einsum('bhcs,bhcsk,bhcsv->bhckv', w_end, k, vh)
    n_loc = jnp.einsum('bhcs,bhcsk->bhck', w_end, k)
    b_end = b[..., -1]

    def step(carry, inp):
        C, n, m = carry
        Cl, nl, al, be = inp
        m_new = jnp.maximum(be + m, al)
        sp = jnp.exp(be + m - m_new)
        sl = jnp.exp(al - m_new)
        C_new = sp[..., None, None] * C + sl[..., None, None] * Cl
        n_new = sp[..., None] * n + sl[..., None] * nl
        return (C_new, n_new, m_new), (C, n, m)

    init = (jnp.zeros((Bsz, H, Dh, Dh), jnp.float32), jnp.zeros((Bsz, H, Dh), jnp.float32),
            jnp.full((Bsz, H), NEG_BIG, jnp.float32))
    _, (C_in, n_in, m_in) = lax.scan(step, init, (jnp.moveaxis(C_loc, 2, 0), jnp.moveaxis(n_loc, 2, 0),
                                                  jnp.moveaxis(a_loc, 2, 0), jnp.moveaxis(b_end, 2, 0)))
    C_in = jnp.moveaxis(C_in, 0, 2)
    n_in = jnp.moveaxis(n_in, 0, 2)
    m_in = jnp.moveaxis(m_in, 0, 2)
    m_inter = b + m_in[..., None]
    m_t = jnp.maximum(m_inter, m_intra)
    S_qk = jnp.einsum('bhcld,bhcsd->bhcls', q, k) * jnp.exp(Dm - m_t[..., None])
    inter_w = jnp.exp(m_inter - m_t)
    num = (jnp.einsum('bhcls,bhcsd->bhcld', S_qk, vh)
           + inter_w[..., None] * jnp.einsum('bhcld,bhcde->bhcle', q, C_in))
    den = jnp.sum(S_qk, axis=-1) + inter_w * jnp.einsum('bhcld,bhcd->bhcl', q, n_in)
    h = num / jnp.maximum(jnp.abs(den), jnp.exp(-m_t))[..., None]
    h = h * lax.rsqrt(jnp.mean(h * h, axis=-1, keepdims=True) + EPS)
    h = h * norm_g.astype(jnp.float32).reshape(H, Dh)[None, :, None, None, :]
    h = h.reshape(Bsz, H, S, Dh).transpose(0, 2, 1, 3).reshape(Bsz, S, MLSTM_WIDTH)
    return (h * jax.nn.sigmoid(o_pre.astype(jnp.float32))).astype(u.dtype)


def memory_cross_attention(h, mem_n, w_q, w_kv, w_o):
    Bsz, S, _ = h.shape
    M = mem_n.shape[1]
    q = (h @ w_q).reshape(Bsz, S, XATTN_HEADS, XATTN_HEAD_DIM)
    kv = (mem_n @ w_kv).reshape(Bsz, M, 2, XATTN_HEADS, XATTN_HEAD_DIM)
    k, v = kv[:, :, 0], kv[:, :, 1]
    s = jnp.einsum('bshd,bmhd->bhsm', q, k).astype(jnp.float32) * (XATTN_HEAD_DIM ** -0.5)
    p = jax.nn.softmax(s, axis=-1).astype(v.dtype)
    o = jnp.einsum('bhsm,bmhd->bshd', p, v).reshape(Bsz, S, XATTN_WIDTH)
    return o @ w_o


def peer_ffn(h, w_query, sub_keys, expert_down, expert_up):
    Bsz, S, D = h.shape
    T = Bsz * S
    TB, K, NK, PH = PEER_TOKEN_BLOCK, PEER_TOPK, PEER_NKEYS, PEER_HEADS
    tokens = h.reshape(T // TB, TB, D)

    def one_block(xb):
        q = (xb @ w_query).reshape(TB, PH, 2, PEER_HALF)
        s = jnp.einsum('thpd,hpnd->thpn', q, sub_keys).astype(jnp.float32)
        top_s, top_i = lax.top_k(s, K)
        cand_s = (top_s[:, :, 0, :, None] + top_s[:, :, 1, None, :]).reshape(TB, PH, K * K)
        cand_i = (top_i[:, :, 0, :, None] * NK + top_i[:, :, 1, None, :]).reshape(TB, PH, K * K)
        best_s, best_j = lax.top_k(cand_s, K)
        e_idx = jnp.take_along_axis(cand_i, best_j, axis=-1)
        gate = jax.nn.softmax(best_s, axis=-1)
        u = expert_down[e_idx]
        act = jax.nn.gelu(jnp.einsum('thkd,td->thk', u, xb).astype(jnp.float32), approximate=False)
        vv = expert_up[e_idx]
        return jnp.einsum('thk,thkd->td', (gate * act).astype(vv.dtype), vv)

    return lax.map(one_block, tokens).reshape(Bsz, S, D)


def setup_inputs(seed: int = 0) -> dict:
    key = jax.random.key(seed)
    ks = jax.random.split(key, 32)
    f32 = jnp.float32

    def nrm(k, shape, scale):
        return jax.random.normal(k, shape, f32) * scale

    def gain(k, shape):
        return 1.0 + 0.02 * jax.random.normal(k, shape, f32)

    dt0 = jnp.exp(jax.random.uniform(ks[8], (DEPTH, SSM_HEADS), f32, math.log(1e-3), math.log(1e-1)))
    dt_bias = dt0 + jnp.log(-jnp.expm1(-dt0))
    A_log = jnp.log(jax.random.uniform(ks[9], (DEPTH, SSM_HEADS), f32, 1.0, 16.0))
    f_bias = jnp.linspace(3.0, 6.0, MLSTM_HEADS, dtype=f32)[None, :] + nrm(ks[17], (DEPTH, MLSTM_HEADS), 0.1)
    return {
        "x": nrm(ks[0], (BATCH, SEQ, D_MODEL), 1.0),
        "mem": nrm(ks[1], (BATCH, MEM_LEN, D_MODEL), 1.0),
        "w_in": nrm(ks[2], (DEPTH, D_MODEL, IN_WIDTH), D_MODEL ** -0.5),
        "w_out": nrm(ks[3], (DEPTH, MIX_WIDTH, D_MODEL), MIX_WIDTH ** -0.5),
        "mix_norm_g": gain(ks[4], (DEPTH, D_MODEL)),
        "ssm_conv_w": nrm(ks[5], (DEPTH, SSM_CONV, SSM_CONV_DIM), SSM_CONV ** -0.5),
        "ssm_conv_b": nrm(ks[6], (DEPTH, SSM_CONV_DIM), 0.02),
        "ssm_dt_bias": dt_bias,
        "ssm_A_log": A_log,
        "ssm_D": 1.0 + nrm(ks[10], (DEPTH, SSM_HEADS), 0.1),
        "ssm_norm_g": gain(ks[11], (DEPTH, SSM_WIDTH)),
        "mlstm_conv_w": nrm(ks[12], (DEPTH, MLSTM_CONV, MLSTM_WIDTH), MLSTM_CONV ** -0.5),
        "mlstm_conv_b": nrm(ks[13], (DEPTH, MLSTM_WIDTH), 0.02),
        "mlstm_wq": nrm(ks[14], (DEPTH, MLSTM_HEADS, MLSTM_HEAD_DIM, MLSTM_HEAD_DIM), MLSTM_HEAD_DIM ** -0.5),
        "mlstm_wk": nrm(ks[15], (DEPTH, MLSTM_HEADS, MLSTM_HEAD_DIM, MLSTM_HEAD_DIM), MLSTM_HEAD_DIM ** -0.5),
        "mlstm_i_bias": nrm(ks[16], (DEPTH, MLSTM_HEADS), 0.1),
        "mlstm_f_bias": f_bias,
        "mlstm_norm_g": gain(ks[18], (DEPTH, MLSTM_WIDTH)),
        "xattn_norm_g": gain(ks[19], (DEPTH, D_MODEL)),
        "mem_norm_g": gain(ks[20], (DEPTH, D_MODEL)),
        "xattn_w_q": nrm(ks[21], (DEPTH, D_MODEL, XATTN_WIDTH), D_MODEL ** -0.5),
        "xattn_w_kv": nrm(ks[22], (DEPTH, D_MODEL, 2 * XATTN_WIDTH), D_MODEL ** -0.5),
        "xattn_w_o": nrm(ks[23], (DEPTH, XATTN_WIDTH, D_MODEL), XATTN_WIDTH ** -0.5),
        "ffn_norm_g": gain(ks[24], (DEPTH, D_MODEL)),
        "peer_w_query": nrm(ks[25], (DEPTH, D_MODEL, PEER_HEADS * PEER_KEY_DIM), D_MODEL ** -0.5),
        "peer_sub_keys": nrm(ks[26], (DEPTH, PEER_HEADS, 2, PEER_NKEYS, PEER_HALF), PEER_HALF ** -0.5),
        "peer_down": nrm(ks[27], (DEPTH, PEER_EXPERTS, D_MODEL), D_MODEL ** -0.5),
        "peer_up": nrm(ks[28], (DEPTH, PEER_EXPERTS, D_MODEL), 0.5 * PEER_HEADS ** -0.5),
        "final_norm_g": gain(ks[29], (D_MODEL,)),
    }


def reference(x, mem, w_in, w_out, mix_norm_g, ssm_conv_w, ssm_conv_b, ssm_dt_bias, ssm_A_log, ssm_D,
              ssm_norm_g, mlstm_conv_w, mlstm_conv_b, mlstm_wq, mlstm_wk, mlstm_i_bias, mlstm_f_bias,
              mlstm_norm_g, xattn_norm_g, mem_norm_g, xattn_w_q, xattn_w_kv, xattn_w_o, ffn_norm_g,
              peer_w_query, peer_sub_keys, peer_down, peer_up, final_norm_g):
    Bsz, S, _ = x.shape
    for l in range(DEPTH):
        hn = rms_norm(x, mix_norm_g[l])
        proj = hn @ w_in[l]
        qa, ka, va, z, xbc, dt_raw, mu, mv, mo, mi, mf = jnp.split(proj, IN_SPLITS, axis=-1)
        hshape = (Bsz, S, MOBA_HEADS, MOBA_HEAD_DIM)
        y_a = moba_attention(qa.reshape(hshape), ka.reshape(hshape), va.reshape(hshape)).astype(hn.dtype)
        y_s = mamba2_ssd(z, xbc, dt_raw, ssm_conv_w[l], ssm_conv_b[l], ssm_dt_bias[l], ssm_A_log[l],
                         ssm_D[l], ssm_norm_g[l]).astype(hn.dtype)
        y_m = mlstm(mu, mv, mo, mi, mf, mlstm_conv_w[l], mlstm_conv_b[l], mlstm_wq[l], mlstm_wk[l],
                    mlstm_i_bias[l], mlstm_f_bias[l], mlstm_norm_g[l]).astype(hn.dtype)
        x = x + jnp.concatenate([y_a, y_s, y_m], axis=-1) @ w_out[l]
        x = x + memory_cross_attention(rms_norm(x, xattn_norm_g[l]), rms_norm(mem, mem_norm_g[l]),
                                       xattn_w_q[l], xattn_w_kv[l], xattn_w_o[l])
        x = x + peer_ffn(rms_norm(x, ffn_norm_g[l]), peer_w_query[l], peer_sub_keys[l], peer_down[l], peer_up[l])
    return rms_norm(x, final_norm_g)
```

```python
from contextlib import ExitStack
import concourse.bass as bass
import concourse.mybir as mybir

ENGS = ["tensor", "vector", "scalar", "gpsimd", "sync"]


class Prog:
    def __init__(self, nc, dma_slots=None):
        self.nc = nc
        self.ops = []
        self.stack = ExitStack()
        self.dma_slots = dma_slots or {"sync": 8, "gpsimd": 8, "scalar": 4}
        self._n = 0

    def sb(self, name, shape, dt):
        return self.stack.enter_context(self.nc.sbuf_tensor(name, list(shape), dt))

    def ps(self, name, shape, dt):
        return self.stack.enter_context(self.nc.psum_tensor(name, list(shape), dt))

    def op(self, eng, fn, reads=(), writes=()):
        self.ops.append(("c", eng, fn, tuple(reads), tuple(writes)))

    def dma(self, q, fn, reads=(), writes=()):
        self.ops.append(("d", q, fn, tuple(reads), tuple(writes)))

    def barrier(self):
        self.ops.append(("b", None, None, (), ()))

    def build(self, final_wait_eng="sync"):
        nc = self.nc
        st = self.stack
        esem = {e: st.enter_context(nc.semaphore("s_" + e)) for e in ENGS}
        dsem = {}
        for q, n in self.dma_slots.items():
            dsem[q] = [st.enter_context(nc.semaphore(f"d_{q}{i}")) for i in range(n)]
        ecount = {e: 0 for e in ENGS}
        dcount = {q: [0] * n for q, n in self.dma_slots.items()}
        dnext = {q: 0 for q in self.dma_slots}
        waited = {e: {} for e in ENGS}
        last_w = {}
        readers = {}
        streams = {e: [] for e in ENGS}
        semobj = {}
        all_tokens = []

        def need(eng, tok, waits):
            if tok is None:
                return
            sid, val = tok
            if waited[eng].get(sid, 0) >= val:
                return
            waited[eng][sid] = val
            waits.append(tok)

        pend = {e: {} for e in ENGS}
        latest = {}
        for kind, eng, fn, reads, writes in self.ops:
            if kind == "b":
                for e in ENGS:
                    pend[e] = dict(latest)
                continue
            waits = []
            if pend[eng]:
                for sid_, val_ in pend[eng].items():
                    need(eng, (sid_, val_), waits)
                pend[eng] = {}
            own = id(esem[eng])
            toks = []
            for r in reads:
                toks.append(last_w.get(r))
            for w in writes:
                toks.append(last_w.get(w))
                for t in readers.get(w, {}).items():
                    toks.append(t)
            for t in toks:
                if t is None:
                    continue
                if kind == "c" and eng == "tensor" and t[0] == own:
                    continue
                need(eng, t, waits)
            if kind == "c":
                ecount[eng] += 1
                tok = (id(esem[eng]), ecount[eng])
                semobj[tok[0]] = esem[eng]
                streams[eng].append((waits, fn, esem[eng], 1))
                waited[eng][tok[0]] = max(waited[eng].get(tok[0], 0), 0)
            else:
                slot = dnext[eng]
                dnext[eng] = (slot + 1) % len(dsem[eng])
                s = dsem[eng][slot]
                prev = dcount[eng][slot]
                if prev:
                    need(eng, (id(s), prev), waits)
                dcount[eng][slot] = prev + 16
                tok = (id(s), prev + 16)
                semobj[tok[0]] = s
                streams[eng].append((waits, fn, s, 16))
            all_tokens.append(tok)
            latest[tok[0]] = max(latest.get(tok[0], 0), tok[1])
            for r in reads:
                d = readers.setdefault(r, {})
                d[tok[0]] = max(d.get(tok[0], 0), tok[1])
            for w in writes:
                last_w[w] = tok
                readers[w] = {}

        fin = {}
        for sid, val in all_tokens:
            fin[sid] = max(fin.get(sid, 0), val)
        self.n_instr = {e: len(streams[e]) for e in ENGS}

        with nc.Block() as block:
            def emit(e, name):
                for waits, fn, s, inc in streams[name]:
                    for sid, val in waits:
                        e.wait_ge(semobj[sid], val)
                    fn(e).then_inc(s, inc)
                if name == final_wait_eng:
                    for sid, val in fin.items():
                        e.wait_ge(semobj[sid], val)

            @block.tensor
            def _(e):
                emit(e, "tensor")

            @block.vector
            def _(e):
                emit(e, "vector")

            @block.scalar
            def _(e):
                emit(e, "scalar")

            @block.gpsimd
            def _(e):
                emit(e, "gpsimd")

            @block.sync
            def _(e):
                emit(e, "sync")
        self.stack.close()


import numpy as np

import numpy as np
import concourse.bass as bass
import concourse.mybir as mybir

F32 = mybir.dt.float32
BF16 = mybir.dt.bfloat16
U32 = mybir.dt.uint32
I32 = mybir.dt.int32
AF = mybir.ActivationFunctionType
ALU = mybir.AluOpType
AX = mybir.AxisListType

D = 2048
EPS = 1e-6
NEG = -1e30
STAGE = 99
SUB = 99


class V:
    def __init__(self, res, ap):
        self.res, self.ap = res, ap

    def re(self, fn):
        return V(self.res, fn(self.ap))


class Tl:
    def __init__(self, P, name, shape, dt, psum=False):
        self.name = name
        self.t = (P.ps if psum else P.sb)("t_" + name, shape, dt)

    def __getitem__(self, idx):
        return V(self.name, self.t[idx])


class K:
    def __init__(self, nc):
        self.nc = nc
        self.P = Prog(nc)
        self._u = 0

    def tile(self, name, shape, dt, psum=False):
        if not hasattr(self, "_tiles"):
            self._tiles = {}
        if name in self._tiles:
            return self._tiles[name]
        t = Tl(self.P, name, shape, dt, psum)
        self._tiles[name] = t
        return t

    def dram(self, name, shape, dt, kind="Internal"):
        return V("dram:" + name, self.nc.dram_tensor(name, list(shape), dt, kind=kind).ap())

    @staticmethod
    def _r(*xs):
        return [x.res for x in xs if isinstance(x, V)]

    @staticmethod
    def _a(x):
        return x.ap if isinstance(x, V) else x

    def mm(self, out, lhsT, rhs, start=True, stop=True, **kw):
        self.P.op("tensor", lambda e: e.matmul(out.ap, lhsT=lhsT.ap, rhs=rhs.ap, start=start, stop=stop, **kw),
                  reads=self._r(lhsT, rhs), writes=self._r(out))

    def tr(self, out, in_, ident):
        self.P.op("tensor", lambda e: e.transpose(out.ap, in_.ap, ident.ap),
                  reads=self._r(in_, ident), writes=self._r(out))

    def act(self, out, in_, func, bias=None, scale=None, accum=None):
        kw = {}
        if bias is not None:
            kw["bias"] = self._a(bias)
        if scale is not None:
            kw["scale"] = self._a(scale)
        if accum is not None:
            kw["accum_out"] = accum.ap
        self.P.op("scalar", lambda e: e.activation(out=out.ap, in_=in_.ap, func=func, **kw),
                  reads=self._r(in_, bias, scale), writes=self._r(out, accum))

    def tt(self, eng, out, in0, in1, op):
        self.P.op(eng, lambda e: e.tensor_tensor(out=out.ap, in0=in0.ap, in1=in1.ap, op=op),
                  reads=self._r(in0, in1), writes=self._r(out))

    def ts(self, eng, out, in0, s1, s2, op0, op1=None, accum=None):
        kw = {}
        if op1 is not None:
            kw["op1"] = op1
        if accum is not None:
            kw["accum_out"] = accum.ap
        self.P.op(eng, lambda e: e.tensor_scalar(out=out.ap, in0=in0.ap, scalar1=self._a(s1), scalar2=self._a(s2), op0=op0, **kw),
                  reads=self._r(in0, s1, s2), writes=self._r(out, accum))

    def stt(self, out, in0, scalar, in1, op0, op1):
        self.P.op("vector", lambda e: e.scalar_tensor_tensor(out=out.ap, in0=in0.ap, scalar=self._a(scalar), in1=in1.ap, op0=op0, op1=op1),
                  reads=self._r(in0, scalar, in1), writes=self._r(out))

    def copy(self, eng, out, in_):
        if eng == "scalar":
            self.P.op("scalar", lambda e: e.activation(out=out.ap, in_=in_.ap, func=AF.Copy),
                      reads=self._r(in_), writes=self._r(out))
        else:
            self.P.op(eng, lambda e: e.tensor_copy(out=out.ap, in_=in_.ap), reads=self._r(in_), writes=self._r(out))

    def memset(self, eng, out, val):
        self.P.op(eng, lambda e: e.memset(out.ap, val), writes=self._r(out))

    def red(self, out, in_, op, axis=AX.X):
        self.P.op("vector", lambda e: e.tensor_reduce(out=out.ap, in_=in_.ap, axis=axis, op=op),
                  reads=self._r(in_), writes=self._r(out))

    def recip(self, out, in_):
        self.P.op("vector", lambda e: e.reciprocal(out=out.ap, in_=in_.ap), reads=self._r(in_), writes=self._r(out))

    def dma(self, q, out, in_, **kw):
        self.P.dma(q, lambda e: e.dma_start(out=out.ap, in_=in_.ap, **kw), reads=self._r(in_), writes=self._r(out))

    def gen(self, eng, fn, reads=(), writes=()):
        self.P.op(eng, fn, reads=self._r(*reads), writes=self._r(*writes))


def make_consts(k):
    c = {}
    c["ident_f"] = k.tile("ident_f", [128, 128], F32)
    c["ident_b"] = k.tile("ident_b", [128, 128], BF16)
    c["ones_b"] = k.tile("ones_b", [128, 128], BF16)
    c["ones_f"] = k.tile("ones_f", [128, 128], F32)
    c["iota_f"] = k.tile("iota_f", [128, 128], F32)
    c["tri_f"] = k.tile("tri_f", [128, 128], F32)
    c["upp_f"] = k.tile("upp_f", [128, 128], F32)
    k.memset("gpsimd", c["ones_f"][:], 1.0)
    k.memset("gpsimd", c["ones_b"][:], 1.0)
    for nm in ("ident_f", "ident_b"):
        t = c[nm]
        k.memset("gpsimd", t[:], 1.0)
        k.gen("gpsimd", lambda e, t=t: e.affine_select(out=t.t[:], in_=t.t[:], pattern=[[-1, 128]], compare_op=ALU.is_equal,
                                                       fill=0.0, base=0, channel_multiplier=1), reads=[t[:]], writes=[t[:]])
    t = c["tri_f"]
    k.memset("gpsimd", t[:], 1.0)
    k.gen("gpsimd", lambda e, t=t: e.affine_select(out=t.t[:], in_=t.t[:], pattern=[[1, 128]], compare_op=ALU.is_ge,
                                                   fill=0.0, base=0, channel_multiplier=-1), reads=[t[:]], writes=[t[:]])
    t2 = c["upp_f"]
    k.ts("gpsimd", t2[:], t[:], -1.0, 1.0, ALU.mult, ALU.add)
    c["eps"] = k.tile("eps_t", [128, 1], F32)
    k.memset("gpsimd", c["eps"][:], EPS)
    c["pb"] = [k.tile(f"pb{i}", [128, 512], F32, psum=True) for i in range(8)]
    it = c["iota_f"]
    k.gen("gpsimd", lambda e: e.iota(it.t[:], pattern=[[1, 128]], base=0, channel_multiplier=0,
                                     allow_small_or_imprecise_dtypes=True), writes=[it[:]])
    return c


def rms_fm(k, c, xt, g, hn, N, sq, pbank, rs):
    k.act(sq[:, :, 0:N], xt[:, :, 0:N], AF.Square)
    for cc in range(16):
        k.mm(pbank, c["ones_b"][:], sq[:, cc, 0:N], start=(cc == 0), stop=(cc == 15))
    k.act(rs[:, 0:N], pbank, AF.Ln, bias=c["eps"][:, 0:1], scale=1.0 / D)
    k.act(rs[:, 0:N], rs[:, 0:N], AF.Exp, scale=-0.5)
    for cc in range(16):
        k.stt(hn[:, cc, 0:N], xt[:, cc, 0:N], g[:, cc:cc + 1], rs[:, 0:N], ALU.mult, ALU.mult)


class PsumRot:
    def __init__(self, banks):
        self.banks = banks
        self.i = 0

    def next(self):
        b = self.banks[self.i % len(self.banks)]
        self.i += 1
        return b


def linear_fm(k, w_dram, Kc, M, rhs, N, wbufs, prot, evac, q="sync", mblk=256):
    wv = w_dram.re(lambda a: a.rearrange("(kc p) m -> p kc m", p=128))
    bi = 0
    for m0 in range(0, M, mblk):
        mw = min(mblk, M - m0)
        wb = wbufs[bi % len(wbufs)]
        bi += 1
        k.dma(q, wb[:, 0:Kc, 0:mw], wv.re(lambda a: a[:, :, m0:m0 + mw]))
        for mi in range(mw // 128):
            ps = prot.next()
            for kc in range(Kc):
                k.mm(ps[:, 0:N], wb[:, kc, mi * 128:(mi + 1) * 128], rhs[:, kc, 0:N], start=(kc == 0), stop=(kc == Kc - 1))
            evac((m0 // 128) + mi, ps[:, 0:N])


def emit_phase_b(k, c, io, NT, N=256, NIH=2, last=False, lname="L", NE=16384):
    nc = k.nc
    NI = 128 // NIH
    pb = c["pb"]
    wout_b = k.dram(lname + "wout_b", [2048, 2048], BF16)
    wq_b = k.dram(lname + "wq_b", [2048, 512], BF16)
    wkv_b = k.dram(lname + "wkv_b", [2048, 1024], BF16)
    wo_b = k.dram(lname + "wo_b", [512, 2048], BF16)
    wqry_b = k.dram(lname + "wqry_b", [2048, 1024], BF16)
    up_b = k.dram(lname + "up_b", [16384, 2048], BF16)
    downT_b = k.dram(lname + "downT_b", [128, 128, 2048], BF16)
    for dst, src, rows in ((wout_b, io["wout"], 2048), (wq_b, io["wq"], 2048), (wkv_b, io["wkv"], 2048),
                           (wo_b, io["wo"], 512), (wqry_b, io["wqry"], 2048), (up_b, io["up"], NE)):
        step = 512
        for r0 in range(0, rows, step):
            k.dma("gpsimd", dst.re(lambda a: a[r0:r0 + step, :]), src.re(lambda a: a[r0:r0 + step, :]))

    xt = k.tile("xt", [128, 16, N], F32)
    yt = k.tile("yt", [128, 16, N], BF16)
    hn = k.tile("hn", [128, 16, N], BF16)
    rs = k.tile("rs", [128, N], F32)
    wbufs = [k.tile(f"wb{i}", [128, 16, 256], BF16) for i in range(2)]
    gx = k.tile("gx", [128, 16], F32)
    gm = k.tile("gm", [128, 16], F32)
    gf = k.tile("gf", [128, 16], F32)
    gl_ = k.tile("gl", [128, 16], F32)
    KT = k.tile("KT", [128, 16, 128], F32)
    kxT = k.tile("kxT", [128, 4, 256], BF16)
    vx = k.tile("vx", [128, 2, 512], BF16)
    qx = k.tile("qx", [128, 4, N], BF16)
    ox = k.tile("ox", [128, 4, N], BF16)
    pT = [k.tile(f"pT{i}", [128, N], BF16) for i in range(2)]
    qp = k.tile("qp", [128, 8, N], F32)
    GG = k.tile("GG", [128, NI, N], BF16)
    dbuf = [k.tile(f"dbuf{i}", [128, 16, 128], BF16) for i in range(2)]
    ubuf = [k.tile(f"ubuf{i}", [128, 2048], BF16) for i in range(2)]
    glb = [k.tile(f"glb{i}", [128, N], BF16) for i in range(2)]
    At = [k.tile(f"At{i}", [128, NI], BF16) for i in range(4)]
    Bt = [k.tile(f"Bt{i}", [128, 128], BF16) for i in range(4)]
    iT = k.tile("iT", [128, N], F32)
    jT = k.tile("jT", [128, N], F32)
    gT = k.tile("gT", [128, N], F32)
    topv = k.tile("topv", [128, 16, 16], F32)
    topi = k.tile("topi", [128, 16, 16], U32)
    topif = k.tile("topif", [128, 16, 16], F32)
    scr = [k.tile(f"scr{i}", [128, 256], F32) for i in range(2)]
    cand = k.tile("cand", [128, 8, 256], F32)
    bv = k.tile("bv", [128, 8, 16], F32)
    bp = k.tile("bp", [128, 8, 16], U32)
    bpi = k.tile("bpi", [128, 8, 16], U32)
    akf = k.tile("akf", [128, 8, 16], F32)
    bkf = k.tile("bkf", [128, 8, 16], F32)
    gsel = k.tile("gsel", [128, 8, 16], F32)
    zs = k.tile("zs", [128, 8], F32)
    isel = k.tile("isel", [128, 128], F32)
    jsel = k.tile("jsel", [128, 128], F32)
    iota16 = k.tile("iota16", [128, 16], F32)
    k.copy("vector", iota16[:], c["iota_f"][:, 0:16])

    k.dma("sync", gx[:], io["gx"])
    k.dma("sync", gm[:], io["gm"])
    k.dma("sync", gf[:], io["gf"])
    if last:
        k.dma("sync", gl_[:], io["gl"])
    k.dma("sync", KT[:], io["KT"])

    assert 16 * N >= 2048
    for i in range(NE // 128):
        ld = V("xt", xt.t[:].rearrange("p a b -> p (a b)")[:, 0:2048])
        k.dma("sync", ld, io["down"].re(lambda a: a[i * 128:(i + 1) * 128, :]))
        tb = V("hn", hn.t[:].rearrange("p a b -> p (a b)")[:, 0:2048])
        for q4 in range(4):
            ps = pb[q4 % 4]
            for j4 in range(4):
                dc = q4 * 4 + j4
                k.tr(ps[:, j4 * 128:(j4 + 1) * 128], ld.re(lambda a: a[:, dc * 128:(dc + 1) * 128]), c["ident_f"][:])
            k.copy("vector" if q4 % 2 == 0 else "scalar", tb.re(lambda a: a[:, q4 * 512:(q4 + 1) * 512]), ps[:, 0:512])
        k.dma("sync", downT_b.re(lambda a: a[i]), tb)

    k.dma("sync", xt[:, :, 0:256], io["memT"].re(lambda a: a.rearrange("(c p) n -> p c n", p=128)))
    rms_fm(k, c, xt, gm, hn, 256, yt, pb[7][:, 0:256], rs)
    prot = PsumRot([pb[0], pb[1], pb[2], pb[3]])
    linear_fm(k, wkv_b.re(lambda a: a[:, 0:512]), 16, 512, hn, 256, wbufs, prot,
              lambda m, ps: k.copy("vector", kxT[:, m, :], ps))
    for half in range(2):
        wb = wbufs[half % 2]
        k.dma("sync", wb[:, :, 0:256], wkv_b.re(lambda a: a.rearrange("(kc p) m -> p kc m", p=128)[:, :, 512 + half * 256:512 + (half + 1) * 256]))
        for mc in range(2):
            ps = prot.next()
            for kc in range(16):
                k.mm(ps[:, 0:256], hn[:, kc, mc * 128:(mc + 1) * 128], wb[:, kc, 0:256], start=(kc == 0), stop=(kc == 15))
            k.copy("vector", vx[:, mc, half * 256:(half + 1) * 256], ps[:, 0:256])

    xTv = io["xT"].re(lambda a: a.rearrange("(c p) n -> p c n", p=128))
    yTv = io["yT"].re(lambda a: a.rearrange("(c p) n -> p c n", p=128))
    oTv = io["xoT"].re(lambda a: a.rearrange("(c p) n -> p c n", p=128))

    for ti in range(NT // N):
        n0 = ti * N
        k.dma("sync", xt[:, :, :], xTv.re(lambda a: a[:, :, n0:n0 + N]))
        k.dma("sync", yt[:, :, :], yTv.re(lambda a: a[:, :, n0:n0 + N]))
        prot = PsumRot([pb[0], pb[1], pb[2], pb[3]])

        def add_x(m, ps):
            k.tt("vector", xt[:, m, :], ps, xt[:, m, :], ALU.add)
        if STAGE >= 1:
            linear_fm(k, wout_b, 16, 2048, yt, N, wbufs, prot, add_x)
        if STAGE < 2:
            k.dma("sync", oTv.re(lambda a: a[:, :, n0:n0 + N]), xt[:, :, :])
            continue
        rms_fm(k, c, xt, gx, hn, N, yt, pb[7][:, 0:N], rs)
        linear_fm(k, wq_b, 16, 512, hn, N, wbufs, prot,
                  lambda m, ps: k.act(qx[:, m, :], ps, AF.Copy, scale=128 ** -0.5))
        for hd in range(4):
            for mc in range(2):
                ps = pb[4 + mc]
                k.mm(ps[:, 0:N], kxT[:, hd, mc * 128:(mc + 1) * 128], qx[:, hd, :])
                k.act(pT[mc][:, :], ps[:, 0:N], AF.Exp)
            for mc in range(2):
                k.mm(pb[6][:, 0:N], vx[:, mc, hd * 128:(hd + 1) * 128], pT[mc][:, :], start=(mc == 0), stop=(mc == 1))
            for mc in range(2):
                k.mm(pb[7][:, 0:N], c["ones_b"][:], pT[mc][:, :], start=(mc == 0), stop=(mc == 1))
            k.recip(rs[:, 0:N], pb[7][:, 0:N])
            k.tt("vector", ox[:, hd, :], pb[6][:, 0:N], rs[:, 0:N], ALU.mult)
        linear_fm(k, wo_b, 4, 2048, ox, N, wbufs, prot, add_x)
        if STAGE < 3:
            k.dma("sync", oTv.re(lambda a: a[:, :, n0:n0 + N]), xt[:, :, :])
            continue
        rms_fm(k, c, xt, gf, hn, N, yt, pb[7][:, 0:N], rs)
        linear_fm(k, wqry_b, 16, 1024, hn, N, wbufs, prot,
                  lambda m, ps: k.copy("scalar", qp[:, m, :], ps))
        for tc in range(N // 128):
            for hp in range(16):
                h, p = divmod(hp, 2)
                k.mm(pb[4 + hp // 4][:, (hp % 4) * 128:(hp % 4 + 1) * 128],
                     qp[:, h, tc * 128:(tc + 1) * 128], KT[:, hp, :])
            if SUB < 1:
                k.copy("vector", iT[:, tc * 128:(tc + 1) * 128], pb[4][:, 0:128])
                k.copy("vector", jT[:, tc * 128:(tc + 1) * 128], pb[5][:, 0:128])
                k.copy("vector", gT[:, tc * 128:(tc + 1) * 128], pb[7][:, 384:512])
                continue
            for hp in range(16):
                src = pb[4 + hp // 4][:, (hp % 4) * 128:(hp % 4 + 1) * 128]
                s_ = scr[hp % 2]
                k.gen("vector", lambda e, hp=hp, src=src: e.max(out=topv.t[:, hp, 0:8], in_=src.ap), reads=[src], writes=[topv[:]])
                k.gen("vector", lambda e, hp=hp, src=src: e.max_index(out=topi.t[:, hp, 0:8], in_max=topv.t[:, hp, 0:8], in_values=src.ap),
                      reads=[src, topv[:]], writes=[topi[:]])
                k.gen("vector", lambda e, hp=hp, src=src, s_=s_: e.match_replace(out=s_.t[:, 0:128], in_to_replace=topv.t[:, hp, 0:8], in_values=src.ap, imm_value=NEG),
                      reads=[src, topv[:]], writes=[s_[:]])
                k.gen("vector", lambda e, hp=hp, s_=s_: e.max(out=topv.t[:, hp, 8:16], in_=s_.t[:, 0:128]), reads=[s_[:]], writes=[topv[:]])
                k.gen("vector", lambda e, hp=hp, s_=s_: e.max_index(out=topi.t[:, hp, 8:16], in_max=topv.t[:, hp, 8:16], in_values=s_.t[:, 0:128]),
                      reads=[s_[:], topv[:]], writes=[topi[:]])
            if SUB < 2:
                k.copy("vector", iT[:, tc * 128:(tc + 1) * 128], V("topv", topv.t[:].rearrange("p a b -> p (a b)")[:, 0:128]))
                k.copy("vector", jT[:, tc * 128:(tc + 1) * 128], V("topi", topi.t[:].rearrange("p a b -> p (a b)")[:, 0:128]))
                k.copy("vector", gT[:, tc * 128:(tc + 1) * 128], V("topi", topi.t[:].rearrange("p a b -> p (a b)")[:, 128:256]))
                continue
            k.copy("vector", topif[:], topi[:])
            tv4 = topv.t[:].rearrange("p (h two) a -> p h two a", two=2)
            ti4 = topif.t[:].rearrange("p (h two) a -> p h two a", two=2)
            c4 = cand.t[:].rearrange("p h (a b) -> p h a b", a=16)
            k.tt("vector", V("cand", c4), V("topv", tv4[:, :, 0, :].unsqueeze(3).to_broadcast([128, 8, 16, 16])),
                 V("topv", tv4[:, :, 1, :].unsqueeze(2).to_broadcast([128, 8, 16, 16])), ALU.add)
            for h in range(8):
                s_ = scr[h % 2]
                k.gen("vector", lambda e, h=h: e.max(out=bv.t[:, h, 0:8], in_=cand.t[:, h, :]), reads=[cand[:]], writes=[bv[:]])
                k.gen("vector", lambda e, h=h: e.max_index(out=bp.t[:, h, 0:8], in_max=bv.t[:, h, 0:8], in_values=cand.t[:, h, :]),
                      reads=[cand[:], bv[:]], writes=[bp[:]])
                k.gen("vector", lambda e, h=h, s_=s_: e.match_replace(out=s_.t[:, :], in_to_replace=bv.t[:, h, 0:8], in_values=cand.t[:, h, :], imm_value=NEG),
                      reads=[cand[:], bv[:]], writes=[s_[:]])
                k.gen("vector", lambda e, h=h, s_=s_: e.max(out=bv.t[:, h, 8:16], in_=s_.t[:, :]), reads=[s_[:]], writes=[bv[:]])
                k.gen("vector", lambda e, h=h, s_=s_: e.max_index(out=bp.t[:, h, 8:16], in_max=bv.t[:, h, 8:16], in_values=s_.t[:, :]),
                      reads=[s_[:], bv[:]], writes=[bp[:]])
            if SUB < 3:
                k.copy("vector", iT[:, tc * 128:(tc + 1) * 128], V("bv", bv.t[:].rearrange("p a b -> p (a b)")))
                k.copy("vector", jT[:, tc * 128:(tc + 1) * 128], V("bp", bp.t[:].rearrange("p a b -> p (a b)")))
                continue
            k.tt("vector", gsel[:], bv[:], V("bv", bv.t[:, :, 0:1].to_broadcast([128, 8, 16])), ALU.subtract)
            k.act(gsel[:], gsel[:], AF.Exp)
            k.red(zs[:], gsel[:], ALU.add)
            k.recip(zs[:], zs[:])
            k.tt("vector", gsel[:], gsel[:], V("zs", zs.t[:, :].unsqueeze(2).to_broadcast([128, 8, 16])), ALU.mult)
            k.gen("vector", lambda e: e.tensor_single_scalar(out=bpi.t[:], in_=bp.t[:], scalar=4, op=ALU.logical_shift_right), reads=[bp[:]], writes=[bpi[:]])
            k.copy("vector", akf[:], bpi[:])
            k.gen("vector", lambda e: e.tensor_single_scalar(out=bpi.t[:], in_=bp.t[:], scalar=15, op=ALU.bitwise_and), reads=[bp[:]], writes=[bpi[:]])
            k.copy("vector", bkf[:], bpi[:])
            if SUB < 4:
                k.copy("vector", iT[:, tc * 128:(tc + 1) * 128], V("akf", akf.t[:].rearrange("p a b -> p (a b)")))
                k.copy("vector", jT[:, tc * 128:(tc + 1) * 128], V("bkf", bkf.t[:].rearrange("p a b -> p (a b)")))
                k.copy("vector", gT[:, tc * 128:(tc + 1) * 128], V("gsel", gsel.t[:].rearrange("p a b -> p (a b)")))
                continue
            io16 = iota16.t[:, :].unsqueeze(1).unsqueeze(1).to_broadcast([128, 8, 16, 16])
            for (kf, pp, dst) in ((akf, 0, isel), (bkf, 1, jsel)):
                k.tt("vector", V("cand", c4), V(kf.name, kf.t[:].unsqueeze(3).to_broadcast([128, 8, 16, 16])), V("iota16", io16), ALU.is_equal)
                k.tt("vector", V("cand", c4), V("cand", c4), V("topif", ti4[:, :, pp, :].unsqueeze(2).to_broadcast([128, 8, 16, 16])), ALU.mult)
                k.red(dst[:], V("cand", cand.t[:].rearrange("p h (a b) -> p (h a) b", a=16)), ALU.add)
            for (src, dstT) in ((isel[:], iT), (jsel[:], jT), (V("gsel", gsel.t[:].rearrange("p h a -> p (h a)")), gT)):
                k.tr(pb[0][:, 0:128], src, c["ident_f"][:])
                k.copy("vector", dstT[:, tc * 128:(tc + 1) * 128], pb[0][:, 0:128])
        if STAGE < 4:
            k.dma("sync", V("dram:xoT", oTv.ap[:, 0, n0:n0 + N]), iT[:, :])
            k.dma("sync", V("dram:xoT", oTv.ap[:, 1, n0:n0 + N]), jT[:, :])
            k.dma("sync", V("dram:xoT", oTv.ap[:, 2, n0:n0 + N]), gT[:, :])
            continue
        TPE = 512 // NI
        for ih in range(NIH):
            gro = PsumRot([pb[4], pb[5], pb[6], pb[7]])
            for t0 in range(0, N, TPE):
                ps = gro.next()
                for tq in range(TPE):
                    t = t0 + tq
                    a_ = At[t % 4]
                    b_ = Bt[t % 4]
                    k.ts("vector", a_[:, :], c["iota_f"][:, ih * NI:(ih + 1) * NI], iT[:, t:t + 1], gT[:, t:t + 1], ALU.is_equal, ALU.mult)
                    k.ts("gpsimd", b_[:, :], c["iota_f"][:, :], jT[:, t:t + 1], None, ALU.is_equal)
                    k.mm(ps[:, tq * NI:(tq + 1) * NI], b_[:, :], a_[:, :])
                k.copy("vector" if (t0 // TPE) % 2 == 0 else "scalar",
                       V("GG", GG.t[:, :, t0:t0 + TPE].rearrange("p i t -> p t i")),
                       V(ps.name, ps.t[:, 0:TPE * NI].rearrange("p (t i) -> p t i", i=NI)))
            hro = PsumRot([pb[4], pb[5], pb[6], pb[7]])
            for il in range(NI):
                i = ih * NI + il
                db = dbuf[il % 2]
                k.dma("sync", V(db.name, db.t[:].rearrange("p a b -> p (a b)")), downT_b.re(lambda a: a[i]))
                ps = hro.next()
                for kc in range(16):
                    k.mm(ps[:, 0:N], db[:, kc, :], hn[:, kc, :], start=(kc == 0), stop=(kc == 15))
                g_ = glb[il % 2]
                k.act(g_[:, :], ps[:, 0:N], AF.Gelu)
                k.tt("gpsimd", GG[:, il, :], g_[:, :], GG[:, il, :], ALU.mult)
            for dh in range(2):
                for b4 in range(4):
                    k.memset("vector", pb[b4][:, :], 0.0)
                for il in range(NI):
                    i = ih * NI + il
                    ub = ubuf[il % 2]
                    k.dma("sync", ub[:, 0:1024], up_b.re(lambda a: a[i * 128:(i + 1) * 128, dh * 1024:(dh + 1) * 1024]))
                    for dc in range(8):
                        k.mm(pb[dc // 2][:, (dc % 2) * 256:(dc % 2) * 256 + N], ub[:, dc * 128:(dc + 1) * 128], GG[:, il, :],
                             start=False, stop=False, skip_group_check=True)
                for dc in range(8):
                    k.tt("vector", xt[:, dh * 8 + dc, :], pb[dc // 2][:, (dc % 2) * 256:(dc % 2) * 256 + N], xt[:, dh * 8 + dc, :], ALU.add)
        if last:
            rms_fm(k, c, xt, gl_, hn, N, yt, pb[7][:, 0:N], rs)
            for cc in range(16):
                k.stt(xt[:, cc, :], xt[:, cc, :], gl_[:, cc:cc + 1], rs[:, 0:N], ALU.mult, ALU.mult)
        k.dma("sync", oTv.re(lambda a: a[:, :, n0:n0 + N]), xt[:, :, :])


def make_KT(subk):
    KT = np.zeros((128, 16, 128), np.float32)
    for h in range(8):
        for p in range(2):
            KT[64 * p:64 * p + 64, 2 * h + p, :] = subk[h, p].T
    return KT


WA = 1800
C_MI, C_MF = 896, 1024
C_V, C_Z, C_MV, C_MO, C_SM = 1152, 1280, 1536, 1664, 1792
PEN = 10000.0
SA = 99
SUBA = 99


def emit_phase_a(k, c, io, S):
    N = 256
    NB = S // 256
    pb = c["pb"]
    T = k.tile
    w_sb = T("w_sb", [128, 16, WA], BF16)
    kT_all = T("kT_all", [128, S], BF16)
    V_all = T("V_all", [128, S // 128, 2, 66], BF16)
    xt = T("xt", [128, 16, N], F32)
    hn = T("hn", [128, 16, N], BF16)
    rs = T("rs", [128, N], F32)
    gmx = T("gmx", [128, 16], F32)
    qz = [T(f"qz{h}", [128, N], BF16) for h in range(2)]
    kmT = T("kmT", [128, 64], BF16)
    kmf = T("kmf", [128, 64], F32)
    ksum = T("ksum", [128, 1], F32)
    gate_sb = [T(f"gate_sb{h}", [128, 64], F32) for h in range(2)]
    cst = [T(f"cst{h}", [128, 64], F32) for h in range(2)]
    gtmp = T("gtmp", [128, 64], F32)
    m8 = T("m8", [128, 8], F32)
    Ttab = T("Ttab", [128, 2, 2, 64], F32)
    ownc = T("ownc", [128, 2, 2], F32)
    skk = T("skk", [128, 2, 256], BF16)
    cbias = T("cbias", [128, 2, 256], BF16)
    onesrow = T("onesrow", [128, 128], BF16)
    onesrow_f = T("onesrow_f", [128, 128], F32)
    Pb = [T(f"Pb{i}", [128, 256], BF16) for i in range(2)]
    PT = [T(f"PT{i}", [128, 2, 128], BF16) for i in range(2)]
    ya_tok = T("ya_tok", [128, 128], F32)
    rden = T("rden", [128, 1], F32)
    yT_tile = T("yT_tile", [128, 4, N], BF16)
    cw = T("cw", [128, 5, 4], F32)
    cb = T("cb", [128, 5], F32)
    hp = T("hp", [128, 16], F32)
    Aneg = T("Aneg", [128, 4], F32)
    nfb = T("nfb", [128, 1], F32)
    gsn = T("gsn", [128, 256], F32)
    gmn = T("gmn", [128, 128], F32)
    wq_m = T("wq_m", [128, 128], F32)
    wk_m = T("wk_m", [128, 128], F32)
    cin = T("cin", [128, 5, 3 + N], F32)
    cacc = T("cacc", [128, N], F32)
    cout = T("cout", [128, 5, N], F32)
    zs = [T(f"zs{i}", [128, 256], F32) for i in range(2)]
    mv_sb = [T(f"mv_sb{i}", [128, 128], F32) for i in range(2)]
    sig = [T(f"sig{i}", [128, 128], F32) for i in range(2)]
    sm_sb = [T(f"sm_sb{i}", [128, 8], F32) for i in range(2)]
    dt4 = T("dt4", [128, 4], F32)
    a4 = T("a4", [128, 4], F32)
    cs4 = T("cs4", [128, 4], F32)
    ecs = T("ecs", [128, 4], F32)
    dte = T("dte", [128, 4], F32)
    cdb = T("cdb", [128, 4], F32)
    xs_sb = T("xs_sb", [128, 256], F32)
    Xdt = T("Xdt", [128, 256], F32)
    Xdte = T("Xdte", [128, 256], F32)
    B_tok = T("B_tok", [128, 128], F32)
    CBm = T("CBm", [128, 128], F32)
    Ah = [T(f"Ah{i}", [128, 128], F32) for i in range(2)]
    Eh = [T(f"Eh{i}", [128, 128], F32) for i in range(2)]
    MT = [T(f"MT{i}", [128, 128], F32) for i in range(2)]
    hstate = T("hstate", [128, 256], F32)
    y1 = T("y1", [128, 256], F32)
    y2 = T("y2", [128, 256], F32)
    ssq = T("ssq", [128, 1], F32)
    ysn = T("ysn", [128, 256], F32)
    qmT = T("qmT", [128, N], F32)
    kmT2 = T("kmT2", [128, N], F32)
    k_tok = T("k_tok", [128, 128], F32)
    KQm = T("KQm", [128, 128], F32)
    vw = T("vw", [128, 130], F32)
    vwe = T("vwe", [128, 130], F32)
    Cn = T("Cn", [128, 130], F32)
    t1 = T("t1", [128, 130], F32)
    t2 = T("t2", [128, 130], F32)
    dd = T("dd", [128, 1], F32)
    hm = T("hm", [128, 128], F32)
    hm2 = T("hm2", [128, 128], F32)
    cols = T("cols", [128, 8], F32)
    spsl = T("spsl", [128, 2], F32)
    NR = 12
    R = T("R", [128, NR, 128], F32)
    rowsb = T("rowsb", [128, 512], F32)
    ms = T("ms", [128, 8], F32)
    R_L1, R_IG, R_BN, R_W, R_CM, R_MX, R_EU, R_IW, R_EMT, R_EW, R_WE, R_Z = range(12)

    wv = io["win"].re(lambda a: a.rearrange("(kc p) m -> p kc m", p=128))
    for q4 in range(4):
        k.dma("gpsimd", w_sb[:, q4 * 4:(q4 + 1) * 4, :], wv.re(lambda a: a[:, q4 * 4:(q4 + 1) * 4, :]))
    for dst, nm in ((gmx, "gmix"), (cw, "cw"), (cb, "cb"), (hp, "hp"), (gsn, "gsn"), (gmn, "gmn"), (wq_m, "wqm"), (wk_m, "wkm"),
                    (Ttab, "Ttab"), (ownc, "ownc")):
        k.dma("sync", dst[:], io[nm])
    k.dma("sync", xt[:, 0:2, :], io["skk"])
    k.dma("sync", xt[:, 2:4, :], io["cbias"])
    k.copy("vector", skk[:], xt[:, 0:2, :])
    k.copy("vector", cbias[:], xt[:, 2:4, :])
    k.memset("vector", onesrow[:], 0.0)
    k.memset("vector", onesrow[0:1, :], 1.0)
    k.memset("vector", onesrow_f[:], 0.0)
    k.memset("vector", onesrow_f[0:1, :], 1.0)
    k.act(Aneg[:], hp[:, 4:8], AF.Exp)
    k.ts("vector", Aneg[:], Aneg[:], -1.0, None, ALU.mult)
    k.ts("vector", nfb[:], hp[:, 13:14], -1.0, None, ALU.mult)
    for h in range(2):
        k.memset("vector", qz[h][:], 0.0)
        k.memset("vector", gate_sb[h][:], NEG)
        k.memset("vector", cst[h][:], 0.0)
    k.memset("vector", kmT[:], 0.0)
    k.memset("vector", kmf[:], 0.0)
    k.memset("gpsimd", V_all[:], 1.0)
    k.memset("vector", cin[:], 0.0)
    k.memset("vector", hstate[:], 0.0)
    k.memset("vector", Cn[:], 0.0)
    k.memset("vector", vw[:], 0.0)
    k.memset("vector", vwe[:], 0.0)
    k.memset("gpsimd", R[:], 0.0)
    k.memset("vector", ms[:], 0.0)
    k.memset("vector", ms[0:1, 0:1], NEG)

    k.memset("vector", yT_tile[:], 0.0)
    xTv = io["xT"].re(lambda a: a.rearrange("(c p) n -> p c n", p=128))
    yTv = io["yT"].re(lambda a: a.rearrange("(c p) n -> p c n", p=128))
    rotA = PsumRot([pb[0], pb[1]])
    one1 = c["ones_f"][:, 0:1]

    for ti in range(S // N):
        t0 = ti * N
        blk = ti
        k.dma("sync", xt[:, :, :], xTv.re(lambda a: a[:, :, t0:t0 + N]))
        if SUBA < 0.1:
            k.dma("sync", yTv.re(lambda a: a[:, :, t0:t0 + N]), yT_tile[:, :, :])
            continue
        rms_fm(k, c, xt, gmx, hn, N, hn, pb[7][:, 0:N], rs)
        for m in range(7 if SUBA >= 1 else (0 if SUBA < 0.3 else (1 if SUBA < 0.5 else 2))):
            ps = rotA.next()
            for kc in range(16):
                k.mm(ps[:, 0:N], w_sb[:, kc, m * 128:(m + 1) * 128], hn[:, kc, :], start=(kc == 0), stop=(kc == 15))
            if m == 0:
                k.act(qz[0][0:64, :], ps[0:64, 0:N], AF.Copy, scale=0.125)
                k.act(qz[1][64:128, :], ps[64:128, 0:N], AF.Copy, scale=0.125)
            elif m == 1:
                k.act(kT_all[:, t0:t0 + N], ps[:, 0:N], AF.Copy, accum=ksum[:])
                k.ts("vector", kmf[:, blk:blk + 1], ksum[:], 1.0 / 256, None, ALU.mult)
                k.copy("vector", kmT[:], kmf[:])
            else:
                j = m - 2
                k.copy("vector" if j % 2 == 0 else "scalar", cin[:, j, 3:3 + N], ps[:, 0:N])
        if SUBA < 2:
            k.dma("sync", yTv.re(lambda a: a[:, :, t0:t0 + N]), yT_tile[:, :, :])
            continue
        for r, col in ((0, C_MI), (1, C_MF)):
            for kc in range(16):
                k.mm(pb[7][:, r * 256:(r + 1) * 256], w_sb[:, kc, col:col + 128], hn[:, kc, :], start=(kc == 0), stop=(kc == 15))
        k.copy("vector", rowsb[0:1, :], pb[7][0:1, 0:512])
        if SUBA < 3:
            k.dma("sync", yTv.re(lambda a: a[:, :, t0:t0 + N]), yT_tile[:, :, :])
            continue
        for ch in range(2):
            c0 = ch * 128
            b1 = rotA.next()
            b2 = rotA.next()
            for kc in range(16):
                k.mm(b1[:, 0:512], hn[:, kc, c0:c0 + 128], w_sb[:, kc, C_V:C_MO], start=(kc == 0), stop=(kc == 15))
            for kc in range(16):
                k.mm(b2[:, 0:136], hn[:, kc, c0:c0 + 128], w_sb[:, kc, C_MO:C_MO + 136], start=(kc == 0), stop=(kc == 15))
            gch = ti * 2 + ch
            k.copy("vector", V("V_all", V_all.t[:, gch, :, 0:64]), V(b1.name, b1.t[:, 0:128].rearrange("p (h d) -> p h d", h=2)))
            k.act(zs[ch][:], b1[:, 128:384], AF.Silu)
            k.copy("vector", mv_sb[ch][:], b1[:, 384:512])
            k.act(sig[ch][:], b2[:, 0:128], AF.Sigmoid)
            k.copy("vector", sm_sb[ch][:], b2[:, 128:136])

        si = 0
        for qc in range(2 if SA >= 1 else 0):
            qs = slice(qc * 128, (qc + 1) * 128)
            for h in range(2):
                if blk > 0:
                    k.mm(pb[7][:, 0:64], qz[h][:, qs], kmT[:, 0:64])
                    k.copy("vector", gate_sb[h][:, 0:blk], pb[7][:, 0:blk])
                    k.gen("vector", lambda e, h=h: e.max(out=m8.t[:, :], in_=gate_sb[h].t[:, :]), reads=[gate_sb[h][:]], writes=[m8[:]])
                    k.ts("vector", gtmp[:, 0:blk], gate_sb[h][:, 0:blk], m8[:, 2:3], PEN, ALU.is_ge, ALU.mult)
                    k.tt("vector", cst[h][:, 0:blk], gtmp[:, 0:blk], Ttab[:, qc, h, NB - 1 - blk:NB - 1], ALU.add)
                for n in range(blk + 1):
                    sp_ = pb[2 + si % 2]
                    pt_ = pb[4 + si % 2]
                    P_ = Pb[si % 2]
                    PT_ = PT[si % 2]
                    si += 1
                    own = (n == blk)
                    k.mm(sp_[:, 0:256], qz[h][:, qs], kT_all[:, n * 256:(n + 1) * 256], start=True, stop=False)
                    k.mm(sp_[:, 0:256], onesrow[:], skk[:, h, :], start=False, stop=(not own))
                    if own:
                        k.mm(sp_[:, 0:256], c["ident_b"][:], cbias[:, qc, :], start=False, stop=True)
                    bias = ownc[:, qc, h:h + 1] if own else cst[h][:, n:n + 1]
                    k.act(P_[:, :], sp_[:, 0:256], AF.Exp, bias=bias)
                    ptv = V(pt_.name, pt_.t[:, 0:128].bitcast(BF16).rearrange("p (c q) -> p c q", c=2))
                    for kc in range(2):
                        k.tr(V(pt_.name, ptv.ap[:, kc, :]), P_[:, kc * 128:(kc + 1) * 128], c["ident_b"][:])
                    k.copy("vector", PT_[:, :, :], ptv)
                    for kc in range(2):
                        k.mm(pb[6][:, 0:66], PT_[:, kc, :], V("V_all", V_all.t[:, n * 2 + kc, h, :]),
                             start=(n == 0 and kc == 0), stop=(own and kc == 1))
                k.recip(rden[:], pb[6][:, 64:65])
                k.ts("vector", ya_tok[:, h * 64:(h + 1) * 64], pb[6][:, 0:64], rden[:, 0:1], None, ALU.mult)
            k.tr(pb[7][:, 0:128], ya_tok[:], c["ident_f"][:])
            k.copy("scalar", yT_tile[:, 0, qs], pb[7][:, 0:128])

        if SA < 2:
            k.dma("sync", yTv.re(lambda a: a[:, :, t0:t0 + N]), yT_tile[:, :, :])
            continue
        for j in range(5):
            k.ts("vector", cacc[:], cin[:, j, 0:N], cw[:, j, 0:1], None, ALU.mult)
            for tp in range(1, 4):
                k.stt(cacc[:], cin[:, j, tp:tp + N], cw[:, j, tp:tp + 1], cacc[:], ALU.mult, ALU.add)
            k.act(cout[:, j, :], cacc[:], AF.Silu, bias=cb[:, j:j + 1])
            k.copy("vector", cin[:, j, 0:3], cin[:, j, N:N + 3])
        rot = PsumRot([pb[0], pb[1], pb[2], pb[3], pb[4], pb[5], pb[6]])
        ps = rot.next()
        k.mm(ps[:, 0:N], wq_m[:], cout[:, 4, :])
        k.copy("vector", qmT[:], ps[:, 0:N])
        ps = rot.next()
        k.mm(ps[:, 0:N], wk_m[:], cout[:, 4, :])
        k.act(kmT2[:], ps[:, 0:N], AF.Copy, scale=128 ** -0.5)

        for ch in range(2 if SA >= 3 else 0):
            cs_ = slice(ch * 128, (ch + 1) * 128)
            k.tt("vector", dt4[:], sm_sb[ch][:, 0:4], hp[:, 0:4], ALU.add)
            k.act(dt4[:], dt4[:], AF.Exp)
            k.act(dt4[:], dt4[:], AF.Ln, bias=one1)
            k.tt("vector", a4[:], dt4[:], Aneg[:], ALU.mult)
            k.mm(pb[7][:, 0:4], c["tri_f"][:], a4[:])
            k.mm(pb[7][:, 4:8], c["ones_f"][:], a4[:])
            k.copy("vector", cs4[:], pb[7][:, 0:4])
            k.act(ecs[:], pb[7][:, 0:4], AF.Exp)
            k.tt("vector", dte[:], pb[7][:, 4:8], cs4[:], ALU.subtract)
            k.act(dte[:], dte[:], AF.Exp)
            k.act(cdb[:], pb[7][:, 4:8], AF.Exp)
            pxs = rot.next()
            for j in range(2):
                k.tr(pxs[:, j * 128:(j + 1) * 128], cout[:, j, cs_], c["ident_f"][:])
            k.copy("scalar", xs_sb[:], pxs[:, 0:256])
            for h in range(4):
                hs = slice(h * 64, (h + 1) * 64)
                k.ts("vector", Xdt[:, hs], xs_sb[:, hs], dt4[:, h:h + 1], None, ALU.mult)
                k.ts("vector", Xdte[:, hs], Xdt[:, hs], dte[:, h:h + 1], None, ALU.mult)
            pbt = rot.next()
            k.tr(pbt[:, 0:128], cout[:, 2, cs_], c["ident_f"][:])
            k.copy("scalar", B_tok[:], pbt[:, 0:128])
            pcb = rot.next()
            k.mm(pcb[:, 0:128], cout[:, 2, cs_], cout[:, 3, cs_])
            k.tt("vector", CBm[:], pcb[:, 0:128], c["tri_f"][:], ALU.mult)
            pyd = rot.next()
            for h in range(4):
                A_, E_, M_ = Ah[h % 2], Eh[h % 2], MT[h % 2]
                k.ts("vector", A_[:], c["upp_f"][:], a4[:, h:h + 1], None, ALU.mult)
                pdm = rot.next()
                k.mm(pdm[:, 0:128], A_[:], c["tri_f"][:])
                k.act(E_[:], pdm[:, 0:128], AF.Exp)
                k.tt("gpsimd", M_[:], E_[:], CBm[:], ALU.mult)
                k.mm(pyd[:, h * 64:(h + 1) * 64], M_[:], Xdt[:, h * 64:(h + 1) * 64])
            pyo = rot.next()
            k.mm(pyo[:, 0:256], cout[:, 3, cs_], hstate[:])
            pst = rot.next()
            k.mm(pst[:, 0:256], B_tok[:], Xdte[:])
            for h in range(4):
                hs = slice(h * 64, (h + 1) * 64)
                k.stt(y1[:, hs], xs_sb[:, hs], hp[:, 8 + h:9 + h], pyd[:, hs], ALU.mult, ALU.add)
                k.stt(y2[:, hs], pyo[:, hs], ecs[:, h:h + 1], y1[:, hs], ALU.mult, ALU.add)
                k.stt(hstate[:, hs], hstate[:, hs], cdb[:, h:h + 1], pst[:, hs], ALU.mult, ALU.add)
            k.tt("vector", y2[:], y2[:], zs[ch][:], ALU.mult)
            k.act(y1[:], y2[:], AF.Square, accum=ssq[:])
            k.act(ssq[:], ssq[:], AF.Ln, bias=c["eps"][:, 0:1], scale=1.0 / 256)
            k.act(ssq[:], ssq[:], AF.Exp, scale=-0.5)
            k.stt(ysn[:], y2[:], ssq[:, 0:1], gsn[:], ALU.mult, ALU.mult)
            for j in range(2):
                pt2 = rot.next()
                k.tr(pt2[:, 0:128], ysn[:, j * 128:(j + 1) * 128], c["ident_f"][:])
                k.copy("scalar", yT_tile[:, 1 + j, cs_], pt2[:, 0:128])

            if SA < 4:
                continue
            def row(r):
                return R[0:1, r, :]
            k.act(row(R_L1), rowsb[0:1, 256 + ch * 128:256 + (ch + 1) * 128], AF.Exp, bias=nfb[0:1, 0:1], scale=-1.0)
            k.act(row(R_L1), row(R_L1), AF.Ln, bias=one1.re(lambda a: a[0:1, :]))
            k.ts("vector", row(R_IG), rowsb[0:1, ch * 128:(ch + 1) * 128], hp[0:1, 12:13], None, ALU.add)
            k.gen("vector", lambda e, ch=ch: e.tensor_tensor_scan(out=R.t[0:1, R_BN, :], data0=R.t[0:1, R_L1, :],
                                                                   data1=R.t[0:1, R_Z, :], initial=0.0, op0=ALU.add, op1=ALU.add),
                  reads=[R[:]], writes=[R[:]])
            k.tt("vector", row(R_W), row(R_IG), row(R_BN), ALU.add)
            k.gen("vector", lambda e, ch=ch: e.tensor_tensor_scan(out=R.t[0:1, R_CM, :], data0=R.t[0:1, R_W, :],
                                                                   data1=R.t[0:1, R_W, :], initial=NEG, op0=ALU.max, op1=ALU.max),
                  reads=[R[:]], writes=[R[:]])
            k.ts("vector", row(R_MX), row(R_CM), ms[0:1, 0:1], None, ALU.max)
            k.act(row(R_EU), row(R_MX), AF.Exp, scale=-1.0)
            k.act(row(R_IW), row(R_MX), AF.Exp, scale=-1.0, bias=ms[0:1, 0:1])
            k.tt("vector", row(R_EMT), row(R_BN), row(R_MX), ALU.subtract)
            k.act(row(R_EMT), row(R_EMT), AF.Exp)
            k.act(row(R_EW), row(R_W), AF.Exp)
            cmL = R[0:1, R_CM, 127:128]
            bnL = R[0:1, R_BN, 127:128]
            k.ts("vector", ms[0:1, 1:2], cmL, -1.0, None, ALU.mult)
            k.act(row(R_WE), row(R_W), AF.Exp, bias=ms[0:1, 1:2])
            k.tt("vector", ms[0:1, 2:3], ms[0:1, 0:1], bnL, ALU.subtract)
            k.tt("vector", ms[0:1, 3:4], cmL, bnL, ALU.subtract)
            k.tt("vector", ms[0:1, 4:5], ms[0:1, 2:3], ms[0:1, 3:4], ALU.max)
            k.ts("vector", ms[0:1, 5:6], ms[0:1, 4:5], -1.0, None, ALU.mult)
            k.act(R[0:1, R_Z + 0, 0:0 + 1] if False else ms[0:1, 6:7], ms[0:1, 2:3], AF.Exp, bias=ms[0:1, 5:6])
            k.act(ms[0:1, 7:8], ms[0:1, 3:4], AF.Exp, bias=ms[0:1, 5:6])
            pcl = rot.next()
            for ci, r in enumerate((R_EU, R_IW, R_EMT, R_EW, R_WE)):
                k.mm(pcl[:, 2 * ci:2 * ci + 2], R[:, r, :], c["ones_f"][:, 0:2])
            k.mm(pcl[:, 16:18], onesrow_f[:], ms[:, 6:8])
            k.copy("vector", cols[:, 0:5], V(pcl.name, pcl.t[:, 0:10].rearrange("p (c two) -> p c two", two=2)[:, :, 0]))
            k.copy("vector", spsl[:], pcl[:, 16:18])
            pkt = rot.next()
            k.mm(pkt[:, 0:128], cout[:, 4, cs_], wk_m[:])
            k.act(k_tok[:], pkt[:, 0:128], AF.Copy, scale=128 ** -0.5)
            pkq = rot.next()
            k.mm(pkq[:, 0:128], kmT2[:, cs_], qmT[:, cs_])
            k.tt("vector", KQm[:], pkq[:, 0:128], c["tri_f"][:], ALU.mult)
            k.ts("vector", vw[:, 0:128], mv_sb[ch][:], cols[:, 3:4], None, ALU.mult)
            k.copy("vector", vw[:, 128:129], cols[:, 3:4])
            k.ts("vector", vwe[:, 0:128], mv_sb[ch][:], cols[:, 4:5], None, ALU.mult)
            k.copy("vector", vwe[:, 128:129], cols[:, 4:5])
            pin = rot.next()
            k.mm(pin[:, 0:130], KQm[:], vw[:])
            pit = rot.next()
            k.mm(pit[:, 0:130], qmT[:, cs_], Cn[:])
            k.ts("vector", t1[:], pin[:, 0:130], cols[:, 0:1], None, ALU.mult)
            k.stt(t2[:], pit[:, 0:130], cols[:, 1:2], t1[:], ALU.mult, ALU.add)
            k.ts("vector", dd[:], t2[:, 128:129], -1.0, None, ALU.mult)
            k.tt("vector", dd[:], dd[:], t2[:, 128:129], ALU.max)
            k.tt("vector", dd[:], dd[:], cols[:, 2:3], ALU.max)
            k.recip(dd[:], dd[:])
            k.ts("vector", hm[:], t2[:, 0:128], dd[:, 0:1], None, ALU.mult)
            k.act(hm2[:], hm[:], AF.Square, accum=ssq[:])
            k.act(ssq[:], ssq[:], AF.Ln, bias=c["eps"][:, 0:1], scale=1.0 / 128)
            k.act(ssq[:], ssq[:], AF.Exp, scale=-0.5)
            k.stt(hm2[:], hm[:], ssq[:, 0:1], gmn[:], ALU.mult, ALU.mult)
            k.tt("vector", hm2[:], hm2[:], sig[ch][:], ALU.mult)
            pt3 = rot.next()
            k.tr(pt3[:, 0:128], hm2[:], c["ident_f"][:])
            k.copy("scalar", yT_tile[:, 3, cs_], pt3[:, 0:128])
            pcl2 = rot.next()
            k.mm(pcl2[:, 0:130], k_tok[:], vwe[:])
            k.ts("vector", t1[:], pcl2[:, 0:130], spsl[:, 1:2], None, ALU.mult)
            k.stt(Cn[:], Cn[:], spsl[:, 0:1], t1[:], ALU.mult, ALU.add)
            k.copy("vector", ms[0:1, 0:1], ms[0:1, 4:5])
        k.dma("sync", yTv.re(lambda a: a[:, :, t0:t0 + N]), yT_tile[:, :, :])


def phase_a_host_inputs(inp, l, b, g, S):
    w_in = inp["w_in"][l]
    o = np.cumsum([0, 512, 512, 512, 1024, 2048, 16, 512, 512, 512, 4, 4])
    oq, ok, ov, oz, oxbc, odt, omu, omv, omo, omi, omf = o[:11]
    win = np.zeros((2048, WA), np.float32)
    win[:, 0:128] = w_in[:, oq + 128 * g: oq + 128 * (g + 1)]
    win[:, 128:256] = w_in[:, ok + 128 * g: ok + 128 * (g + 1)]
    win[:, 256:512] = w_in[:, oxbc + 256 * g: oxbc + 256 * (g + 1)]
    win[:, 512:640] = w_in[:, oxbc + 1024 + 128 * g: oxbc + 1024 + 128 * (g + 1)]
    win[:, 640:768] = w_in[:, oxbc + 1536 + 128 * g: oxbc + 1536 + 128 * (g + 1)]
    win[:, 768:896] = w_in[:, omu + 128 * g: omu + 128 * (g + 1)]
    win[:, C_V:C_V + 128] = w_in[:, ov + 128 * g: ov + 128 * (g + 1)]
    win[:, C_Z:C_Z + 256] = w_in[:, oz + 256 * g: oz + 256 * (g + 1)]
    win[:, C_MV:C_MV + 128] = w_in[:, omv + 128 * g: omv + 128 * (g + 1)]
    win[:, C_MO:C_MO + 128] = w_in[:, omo + 128 * g: omo + 128 * (g + 1)]
    win[:, C_SM:C_SM + 4] = w_in[:, odt + 4 * g: odt + 4 * (g + 1)]
    win[:, C_MI] = w_in[:, omi + g]
    win[:, C_MF] = w_in[:, omf + g]
    cw = np.zeros((128, 5, 4), np.float32)
    cb = np.zeros((128, 5), np.float32)
    scw, scb = inp["ssm_conv_w"][l], inp["ssm_conv_b"][l]
    chans = [np.arange(256 * g, 256 * g + 128), np.arange(256 * g + 128, 256 * g + 256),
             np.arange(1024 + 128 * g, 1024 + 128 * (g + 1)), np.arange(1536 + 128 * g, 1536 + 128 * (g + 1))]
    for j, ch in enumerate(chans):
        cw[:, j, :] = scw[:, ch].T
        cb[:, j] = scb[ch]
    cw[:, 4, :] = inp["mlstm_conv_w"][l][:, 128 * g:128 * (g + 1)].T
    cb[:, 4] = inp["mlstm_conv_b"][l][128 * g:128 * (g + 1)]
    hp = np.zeros((128, 16), np.float32)
    hp[:, 0:4] = inp["ssm_dt_bias"][l][4 * g:4 * g + 4]
    hp[:, 4:8] = inp["ssm_A_log"][l][4 * g:4 * g + 4]
    hp[:, 8:12] = inp["ssm_D"][l][4 * g:4 * g + 4]
    hp[:, 12] = inp["mlstm_i_bias"][l][g]
    hp[:, 13] = inp["mlstm_f_bias"][l][g]
    gsn = np.broadcast_to(inp["ssm_norm_g"][l][256 * g:256 * (g + 1)], (128, 256)).copy()
    gmn = np.broadcast_to(inp["mlstm_norm_g"][l][128 * g:128 * (g + 1)], (128, 128)).copy()
    NB = S // 256
    slopes = 2.0 ** (-8.0 * (np.arange(1, 9)) / 8)
    Ttab = np.zeros((128, 2, 2, 64), np.float32)
    ownc = np.zeros((128, 2, 2), np.float32)
    skk = np.zeros((128, 2, 256), np.float32)
    cbias = np.zeros((128, 2, 256), np.float32)
    tl = np.arange(128)
    for par in range(2):
        qq = tl + 128 * par
        for hh in range(2):
            sl = slopes[2 * g + hh]
            for m in range(NB):
                Ttab[:, par, hh, m] = -PEN - sl * (qq + 256.0 * (NB - 1 - m))
            ownc[:, par, hh] = -sl * qq
        cbias[:, par, :] = np.where(np.arange(256)[None, :] <= qq[:, None], 0.0, -PEN)
    for hh in range(2):
        skk[0, hh, :] = slopes[2 * g + hh] * np.arange(256)
    gm = inp["mix_norm_g"][l]
    return dict(win=win, gmix=np.ascontiguousarray(gm.reshape(16, 128).T), cw=cw, cb=cb, hp=hp, gsn=gsn, gmn=gmn,
                wqm=np.ascontiguousarray(inp["mlstm_wq"][l][g]), wkm=np.ascontiguousarray(inp["mlstm_wk"][l][g]),
                Ttab=Ttab, ownc=ownc, skk=skk, cbias=cbias)


A_INPUT_SHAPES = dict(win=[2048, WA], gmix=[128, 16], cw=[128, 5, 4], cb=[128, 5], hp=[128, 16], gsn=[128, 256], gmn=[128, 128],
                      wqm=[128, 128], wkm=[128, 128], Ttab=[128, 2, 2, 64], ownc=[128, 2, 2], skk=[128, 2, 256], cbias=[128, 2, 256])


from concourse.bass_utils import run_bass_kernel_spmd

SEQ = 16384
_CACHE = {}


def _build_a(S):
    key = ("a", S)
    if key in _CACHE:
        return _CACHE[key]
    nc = bass.Bass("TRN2", target_bir_lowering=False)
    k = K(nc)
    io = {nm: V("dram:" + nm, nc.dram_tensor(nm, list(shp), F32, kind="ExternalInput").ap()) for nm, shp in A_INPUT_SHAPES.items()}
    io["xT"] = V("dram:xT", nc.dram_tensor("xT", [2048, S], F32, kind="ExternalInput").ap())
    io["yT"] = V("dram:yT", nc.dram_tensor("yT", [512, S], BF16, kind="ExternalOutput").ap())
    c = make_consts(k)
    emit_phase_a(k, c, io, S)
    k.P.build()
    _CACHE[key] = nc
    return nc


B_SHAPES = dict(wout=[2048, 2048], wq=[2048, 512], wkv=[2048, 1024], wo=[512, 2048], wqry=[2048, 1024], KT=[128, 16, 128],
                down=[16384, 2048], up=[16384, 2048], gx=[128, 16], gm=[128, 16], gf=[128, 16], gl=[128, 16], memT=[2048, 256])


def _build_b(NT, last):
    key = ("b", NT, last)
    if key in _CACHE:
        return _CACHE[key]
    nc = bass.Bass("TRN2", target_bir_lowering=False)
    k = K(nc)
    io = {nm: V("dram:" + nm, nc.dram_tensor(nm, list(shp), F32, kind="ExternalInput").ap()) for nm, shp in B_SHAPES.items()}
    io["xT"] = V("dram:xT", nc.dram_tensor("xT", [2048, NT], F32, kind="ExternalInput").ap())
    io["yT"] = V("dram:yT", nc.dram_tensor("yT", [2048, NT], BF16, kind="ExternalInput").ap())
    io["xoT"] = V("dram:xoT", nc.dram_tensor("xoT", [2048, NT], F32, kind="ExternalOutput").ap())
    c = make_consts(k)
    emit_phase_b(k, c, io, NT, last=last)
    k.P.build()
    _CACHE[key] = nc
    return nc


def _gfm(g):
    return np.ascontiguousarray(np.asarray(g, np.float32).reshape(16, 128).T)


def kernel(**inputs):
    inp = {k_: np.asarray(v) for k_, v in inputs.items()}
    x = inp["x"]
    Bsz, S, _ = x.shape
    NT = S // 4
    xT = [np.ascontiguousarray(x[b].T) for b in range(Bsz)]
    memT = [np.ascontiguousarray(inp["mem"][b].T) for b in range(Bsz)]
    perm = np.concatenate([np.concatenate([128 * g + np.arange(128), 512 + 256 * g + np.arange(256), 1536 + 128 * g + np.arange(128)])
                           for g in range(4)])
    depth = inp["w_in"].shape[0]
    for l in range(depth):
        ncA = _build_a(S)
        in_maps = []
        for core in range(8):
            b, g = divmod(core, 4)
            m = phase_a_host_inputs(inp, l, b, g, S)
            m["xT"] = xT[b]
            in_maps.append(m)
        resA = run_bass_kernel_spmd(ncA, in_maps, core_ids=list(range(8))).results
        yT = [np.concatenate([np.asarray(resA[b * 4 + g]["yT"]) for g in range(4)], axis=0) for b in range(Bsz)]
        del resA, in_maps
        last = (l == depth - 1)
        ncB = _build_b(NT, last)
        common = dict(wout=np.ascontiguousarray(inp["w_out"][l][perm]), wq=inp["xattn_w_q"][l], wkv=inp["xattn_w_kv"][l], wo=inp["xattn_w_o"][l],
                      wqry=inp["peer_w_query"][l], KT=make_KT(inp["peer_sub_keys"][l]), down=inp["peer_down"][l], up=inp["peer_up"][l],
                      gx=_gfm(inp["xattn_norm_g"][l]), gm=_gfm(inp["mem_norm_g"][l]), gf=_gfm(inp["ffn_norm_g"][l]), gl=_gfm(inp["final_norm_g"]))
        in_maps = []
        for core in range(8):
            b, j = divmod(core, 4)
            sl = slice(j * NT, (j + 1) * NT)
            m = dict(common)
            m["xT"] = np.ascontiguousarray(xT[b][:, sl])
            m["yT"] = np.ascontiguousarray(yT[b][:, sl])
            m["memT"] = memT[b]
            in_maps.append(m)
        resB = run_bass_kernel_spmd(ncB, in_maps, core_ids=list(range(8))).results
        for core in range(8):
            b, j = divmod(core, 4)
            xT[b][:, j * NT:(j + 1) * NT] = np.asarray(resB[core]["xoT"])
        del resB, in_maps
    out = np.stack([np.ascontiguousarray(xT[b].T) for b in range(Bsz)], axis=0).astype(np.float32)
    return out
```

```python
from contextlib import ExitStack
import concourse.bass as bass
import concourse.mybir as mybir

ENGS = ["tensor", "vector", "scalar", "gpsimd", "sync"]


class Prog:
    def __init__(self, nc, dma_slots=None):
        self.nc = nc
        self.ops = []
        self.stack = ExitStack()
        self.dma_slots = dma_slots or {"sync": 8, "gpsimd": 8, "scalar": 4}
        self._n = 0

    def sb(self, name, shape, dt):
        return self.stack.enter_context(self.nc.sbuf_tensor(name, list(shape), dt))

    def ps(self, name, shape, dt):
        return self.stack.enter_context(self.nc.psum_tensor(name, list(shape), dt))

    def op(self, eng, fn, reads=(), writes=()):
        self.ops.append(("c", eng, fn, tuple(reads), tuple(writes)))

    def dma(self, q, fn, reads=(), writes=()):
        self.ops.append(("d", q, fn, tuple(reads), tuple(writes)))

    def barrier(self):
        self.ops.append(("b", None, None, (), ()))

    def build(self, final_wait_eng="sync"):
        nc = self.nc
        st = self.stack
        esem = {e: st.enter_context(nc.semaphore("s_" + e)) for e in ENGS}
        dsem = {}
        for q, n in self.dma_slots.items():
            dsem[q] = [st.enter_context(nc.semaphore(f"d_{q}{i}")) for i in range(n)]
        ecount = {e: 0 for e in ENGS}
        dcount = {q: [0] * n for q, n in self.dma_slots.items()}
        dnext = {q: 0 for q in self.dma_slots}
        waited = {e: {} for e in ENGS}
        last_w = {}
        readers = {}
        streams = {e: [] for e in ENGS}
        semobj = {}
        all_tokens = []

        def need(eng, tok, waits):
            if tok is None:
                return
            sid, val = tok
            if waited[eng].get(sid, 0) >= val:
                return
            waited[eng][sid] = val
            waits.append(tok)

        pend = {e: {} for e in ENGS}
        latest = {}
        for kind, eng, fn, reads, writes in self.ops:
            if kind == "b":
                for e in ENGS:
                    pend[e] = dict(latest)
                continue
            waits = []
            if pend[eng]:
                for sid_, val_ in pend[eng].items():
                    need(eng, (sid_, val_), waits)
                pend[eng] = {}
            own = id(esem[eng])
            toks = []
            for r in reads:
                toks.append(last_w.get(r))
            for w in writes:
                toks.append(last_w.get(w))
                for t in readers.get(w, {}).items():
                    toks.append(t)
            for t in toks:
                if t is None:
                    continue
                if kind == "c" and eng == "tensor" and t[0] == own:
                    continue
                need(eng, t, waits)
            if kind == "c":
                ecount[eng] += 1
                tok = (id(esem[eng]), ecount[eng])
                semobj[tok[0]] = esem[eng]
                streams[eng].append((waits, fn, esem[eng], 1))
                waited[eng][tok[0]] = max(waited[eng].get(tok[0], 0), 0)
            else:
                slot = dnext[eng]
                dnext[eng] = (slot + 1) % len(dsem[eng])
                s = dsem[eng][slot]
                prev = dcount[eng][slot]
                if prev:
                    need(eng, (id(s), prev), waits)
                dcount[eng][slot] = prev + 16
                tok = (id(s), prev + 16)
                semobj[tok[0]] = s
                streams[eng].append((waits, fn, s, 16))
            all_tokens.append(tok)
            latest[tok[0]] = max(latest.get(tok[0], 0), tok[1])
            for r in reads:
                d = readers.setdefault(r, {})
                d[tok[0]] = max(d.get(tok[0], 0), tok[1])
            for w in writes:
                last_w[w] = tok
                readers[w] = {}

        fin = {}
        for sid, val in all_tokens:
            fin[sid] = max(fin.get(sid, 0), val)
        self.n_instr = {e: len(streams[e]) for e in ENGS}

        with nc.Block() as block:
            def emit(e, name):
                for waits, fn, s, inc in streams[name]:
                    for sid, val in waits:
                        e.wait_ge(semobj[sid], val)
                    fn(e).then_inc(s, inc)
                if name == final_wait_eng:
                    for sid, val in fin.items():
                        e.wait_ge(semobj[sid], val)

            @block.tensor
            def _(e):
                emit(e, "tensor")

            @block.vector
            def _(e):
                emit(e, "vector")

            @block.scalar
            def _(e):
                emit(e, "scalar")

            @block.gpsimd
            def _(e):
                emit(e, "gpsimd")

            @block.sync
            def _(e):
                emit(e, "sync")
        self.stack.close()


import numpy as np

import numpy as np
import concourse.bass as bass
import concourse.mybir as mybir

F32 = mybir.dt.float32
BF16 = mybir.dt.bfloat16
U32 = mybir.dt.uint32
I32 = mybir.dt.int32
AF = mybir.ActivationFunctionType
ALU = mybir.AluOpType
AX = mybir.AxisListType

D = 2048
EPS = 1e-6
NEG = -1e30
STAGE = 99
SUB = 99


class V:
    def __init__(self, res, ap):
        self.res, self.ap = res, ap

    def re(self, fn):
        return V(self.res, fn(self.ap))


class Tl:
    def __init__(self, P, name, shape, dt, psum=False, view=None):
        self.name = name
        if view is not None:
            self.t = view
        else:
            self.t = (P.ps if psum else P.sb)("t_" + name, shape, dt)

    def __getitem__(self, idx):
        return V(self.name, self.t[idx])


_DT_BYTES = {}


def _dt_bytes(dt):
    if dt in (F32, U32, I32):
        return 4
    if dt == BF16:
        return 2
    raise ValueError(dt)


class K:
    def __init__(self, nc):
        self.nc = nc
        self.P = Prog(nc)
        self._u = 0

    def use_arena(self, nbytes):
        self._arena = self.P.sb("arena", [128, nbytes // 2], BF16)
        self._arena_n = nbytes
        self._arena_off = 0

    def arena_mark(self):
        return self._arena_off

    def arena_reset(self, mark):
        self._arena_off = mark

    def tile(self, name, shape, dt, psum=False):
        if not hasattr(self, "_tiles"):
            self._tiles = {}
        if name in self._tiles:
            return self._tiles[name]
        arena = getattr(self, "_arena", None)
        if arena is None or psum:
            t = Tl(self.P, name, shape, dt, psum)
        else:
            nel = 1
            for d_ in shape[1:]:
                nel *= d_
            nb = (nel * _dt_bytes(dt) + 31) // 32 * 32
            off = self._arena_off
            assert off + nb <= self._arena_n, f"arena overflow at {name}: {off}+{nb} > {self._arena_n}"
            self._arena_off = off + nb
            v = arena[0:shape[0], off // 2:(off + nel * _dt_bytes(dt)) // 2]
            if dt != BF16:
                v = v.bitcast(dt)
            if len(shape) > 2:
                names = " ".join(f"d{i}" for i in range(1, len(shape)))
                kw = {f"d{i}": shape[i] for i in range(1, len(shape))}
                v = v.rearrange(f"p ({names}) -> p {names}", **kw)
            t = Tl(self.P, name, shape, dt, view=v)
        self._tiles[name] = t
        return t

    def dram(self, name, shape, dt, kind="Internal"):
        if not hasattr(self, "_drams"):
            self._drams = {}
        if name not in self._drams:
            self._drams[name] = V("dram:" + name, self.nc.dram_tensor(name, list(shape), dt, kind=kind).ap())
        return self._drams[name]

    @staticmethod
    def _r(*xs):
        return [x.res for x in xs if isinstance(x, V)]

    @staticmethod
    def _a(x):
        return x.ap if isinstance(x, V) else x

    def mm(self, out, lhsT, rhs, start=True, stop=True, **kw):
        self.P.op("tensor", lambda e: e.matmul(out.ap, lhsT=lhsT.ap, rhs=rhs.ap, start=start, stop=stop, **kw),
                  reads=self._r(lhsT, rhs), writes=self._r(out))

    def tr(self, out, in_, ident):
        self.P.op("tensor", lambda e: e.transpose(out.ap, in_.ap, ident.ap),
                  reads=self._r(in_, ident), writes=self._r(out))

    def act(self, out, in_, func, bias=None, scale=None, accum=None):
        kw = {}
        if bias is not None:
            kw["bias"] = self._a(bias)
        if scale is not None:
            kw["scale"] = self._a(scale)
        if accum is not None:
            kw["accum_out"] = accum.ap
        self.P.op("scalar", lambda e: e.activation(out=out.ap, in_=in_.ap, func=func, **kw),
                  reads=self._r(in_, bias, scale), writes=self._r(out, accum))

    def tt(self, eng, out, in0, in1, op):
        self.P.op(eng, lambda e: e.tensor_tensor(out=out.ap, in0=in0.ap, in1=in1.ap, op=op),
                  reads=self._r(in0, in1), writes=self._r(out))

    def ts(self, eng, out, in0, s1, s2, op0, op1=None, accum=None):
        kw = {}
        if op1 is not None:
            kw["op1"] = op1
        if accum is not None:
            kw["accum_out"] = accum.ap
        self.P.op(eng, lambda e: e.tensor_scalar(out=out.ap, in0=in0.ap, scalar1=self._a(s1), scalar2=self._a(s2), op0=op0, **kw),
                  reads=self._r(in0, s1, s2), writes=self._r(out, accum))

    def stt(self, out, in0, scalar, in1, op0, op1):
        self.P.op("vector", lambda e: e.scalar_tensor_tensor(out=out.ap, in0=in0.ap, scalar=self._a(scalar), in1=in1.ap, op0=op0, op1=op1),
                  reads=self._r(in0, scalar, in1), writes=self._r(out))

    def copy(self, eng, out, in_):
        if eng == "scalar":
            self.P.op("scalar", lambda e: e.activation(out=out.ap, in_=in_.ap, func=AF.Copy),
                      reads=self._r(in_), writes=self._r(out))
        else:
            self.P.op(eng, lambda e: e.tensor_copy(out=out.ap, in_=in_.ap), reads=self._r(in_), writes=self._r(out))

    def memset(self, eng, out, val):
        self.P.op(eng, lambda e: e.memset(out.ap, val), writes=self._r(out))

    def red(self, out, in_, op, axis=AX.X):
        self.P.op("vector", lambda e: e.tensor_reduce(out=out.ap, in_=in_.ap, axis=axis, op=op),
                  reads=self._r(in_), writes=self._r(out))

    def recip(self, out, in_):
        self.P.op("vector", lambda e: e.reciprocal(out=out.ap, in_=in_.ap), reads=self._r(in_), writes=self._r(out))

    def dma(self, q, out, in_, **kw):
        self.P.dma(q, lambda e: e.dma_start(out=out.ap, in_=in_.ap, **kw), reads=self._r(in_), writes=self._r(out))

    def gen(self, eng, fn, reads=(), writes=()):
        self.P.op(eng, fn, reads=self._r(*reads), writes=self._r(*writes))


def make_consts(k):
    c = {}
    c["ident_f"] = k.tile("ident_f", [128, 128], F32)
    c["ident_b"] = k.tile("ident_b", [128, 128], BF16)
    c["ones_b"] = k.tile("ones_b", [128, 128], BF16)
    c["ones_f"] = k.tile("ones_f", [128, 128], F32)
    c["iota_f"] = k.tile("iota_f", [128, 128], F32)
    c["tri_f"] = k.tile("tri_f", [128, 128], F32)
    c["upp_f"] = k.tile("upp_f", [128, 128], F32)
    k.memset("gpsimd", c["ones_f"][:], 1.0)
    k.memset("gpsimd", c["ones_b"][:], 1.0)
    for nm in ("ident_f", "ident_b"):
        t = c[nm]
        k.memset("gpsimd", t[:], 1.0)
        k.gen("gpsimd", lambda e, t=t: e.affine_select(out=t.t[:], in_=t.t[:], pattern=[[-1, 128]], compare_op=ALU.is_equal,
                                                       fill=0.0, base=0, channel_multiplier=1), reads=[t[:]], writes=[t[:]])
    t = c["tri_f"]
    k.memset("gpsimd", t[:], 1.0)
    k.gen("gpsimd", lambda e, t=t: e.affine_select(out=t.t[:], in_=t.t[:], pattern=[[1, 128]], compare_op=ALU.is_ge,
                                                   fill=0.0, base=0, channel_multiplier=-1), reads=[t[:]], writes=[t[:]])
    t2 = c["upp_f"]
    k.ts("gpsimd", t2[:], t[:], -1.0, 1.0, ALU.mult, ALU.add)
    c["eps"] = k.tile("eps_t", [128, 1], F32)
    k.memset("gpsimd", c["eps"][:], EPS)
    c["pb"] = [k.tile(f"pb{i}", [128, 512], F32, psum=True) for i in range(8)]
    it = c["iota_f"]
    k.gen("gpsimd", lambda e: e.iota(it.t[:], pattern=[[1, 128]], base=0, channel_multiplier=0,
                                     allow_small_or_imprecise_dtypes=True), writes=[it[:]])
    return c


def rms_fm(k, c, xt, g, hn, N, sq, pbank, rs):
    k.act(sq[:, :, 0:N], xt[:, :, 0:N], AF.Square)
    for cc in range(16):
        k.mm(pbank, c["ones_b"][:], sq[:, cc, 0:N], start=(cc == 0), stop=(cc == 15))
    k.act(rs[:, 0:N], pbank, AF.Ln, bias=c["eps"][:, 0:1], scale=1.0 / D)
    k.act(rs[:, 0:N], rs[:, 0:N], AF.Exp, scale=-0.5)
    for cc in range(16):
        k.stt(hn[:, cc, 0:N], xt[:, cc, 0:N], g[:, cc:cc + 1], rs[:, 0:N], ALU.mult, ALU.mult)


class PsumRot:
    def __init__(self, banks):
        self.banks = banks
        self.i = 0

    def next(self):
        b = self.banks[self.i % len(self.banks)]
        self.i += 1
        return b


def linear_fm(k, w_dram, Kc, M, rhs, N, wbufs, prot, evac, q="sync", mblk=256):
    wv = w_dram.re(lambda a: a.rearrange("(kc p) m -> p kc m", p=128))
    bi = 0
    for m0 in range(0, M, mblk):
        mw = min(mblk, M - m0)
        wb = wbufs[bi % len(wbufs)]
        bi += 1
        k.dma(q, wb[:, 0:Kc, 0:mw], wv.re(lambda a: a[:, :, m0:m0 + mw]))
        for mi in range(mw // 128):
            ps = prot.next()
            for kc in range(Kc):
                k.mm(ps[:, 0:N], wb[:, kc, mi * 128:(mi + 1) * 128], rhs[:, kc, 0:N], start=(kc == 0), stop=(kc == Kc - 1))
            evac((m0 // 128) + mi, ps[:, 0:N])


def emit_phase_b(k, c, io, NT, N=256, NIH=2, last=False, lname="L", NE=16384, sel=None, NTS=None):
    nc = k.nc
    NI = 128 // NIH
    pb = c["pb"]
    wout_b = k.dram(lname + "wout_b", [2048, 2048], BF16)
    wq_b = k.dram(lname + "wq_b", [2048, 512], BF16)
    wkv_b = k.dram(lname + "wkv_b", [2048, 1024], BF16)
    wo_b = k.dram(lname + "wo_b", [512, 2048], BF16)
    wqry_b = k.dram(lname + "wqry_b", [2048, 1024], BF16)
    up_b = k.dram(lname + "up_b", [16384, 2048], BF16)
    downT_b = k.dram(lname + "downT_b", [128, 128, 2048], BF16)
    for dst, src, rows in ((wout_b, io["wout"], 2048), (wq_b, io["wq"], 2048), (wkv_b, io["wkv"], 2048),
                           (wo_b, io["wo"], 512), (wqry_b, io["wqry"], 2048), (up_b, io["up"], NE)):
        step = 512
        for r0 in range(0, rows, step):
            k.dma("gpsimd", dst.re(lambda a: a[r0:r0 + step, :]), src.re(lambda a: a[r0:r0 + step, :]))

    xt = k.tile("xt", [128, 16, N], F32)
    yt = k.tile("yt", [128, 16, N], BF16)
    hn = k.tile("hn", [128, 16, N], BF16)
    rs = k.tile("rs", [128, N], F32)
    wbufs = [k.tile(f"wb{i}", [128, 16, 256], BF16) for i in range(2)]
    gx = k.tile("gx", [128, 16], F32)
    gm = k.tile("gm", [128, 16], F32)
    gf = k.tile("gf", [128, 16], F32)
    gl_ = k.tile("gl", [128, 16], F32)
    KT = k.tile("KT", [128, 16, 128], F32)
    kxT = k.tile("kxT", [128, 4, 256], BF16)
    vx = k.tile("vx", [128, 2, 512], BF16)
    qx = k.tile("qx", [128, 4, N], BF16)
    ox = k.tile("ox", [128, 4, N], BF16)
    pT = [k.tile(f"pT{i}", [128, N], BF16) for i in range(2)]
    qp = k.tile("qp", [128, 8, N], F32)
    GG = k.tile("GG", [128, NI, N], BF16)
    dbuf = [k.tile(f"dbuf{i}", [128, 16, 128], BF16) for i in range(4)]
    ubuf = [k.tile(f"ubuf{i}", [128, 1024], BF16) for i in range(4)]
    glb = [k.tile(f"glb{i}", [128, N], BF16) for i in range(4)]
    At = [k.tile(f"At{i}", [128, NI], BF16) for i in range(4)]
    Bt = [k.tile(f"Bt{i}", [128, 128], BF16) for i in range(4)]
    iT = k.tile("iT", [128, N], F32)
    jT = k.tile("jT", [128, N], F32)
    gT = k.tile("gT", [128, N], F32)
    topv = k.tile("topv", [128, 16, 16], F32)
    topi = k.tile("topi", [128, 16, 16], U32)
    topif = k.tile("topif", [128, 16, 16], F32)
    scr = [k.tile(f"scr{i}", [128, 256], F32) for i in range(2)]
    cand = k.tile("cand", [128, 8, 256], F32)
    bv = k.tile("bv", [128, 8, 16], F32)
    bp = k.tile("bp", [128, 8, 16], U32)
    bpi = k.tile("bpi", [128, 8, 16], U32)
    akf = k.tile("akf", [128, 8, 16], F32)
    bkf = k.tile("bkf", [128, 8, 16], F32)
    gsel = k.tile("gsel", [128, 8, 16], F32)
    zs = k.tile("zs", [128, 8], F32)
    isel = k.tile("isel", [128, 128], F32)
    jsel = k.tile("jsel", [128, 128], F32)
    iota16 = k.tile("iota16", [128, 16], F32)
    k.copy("vector", iota16[:], c["iota_f"][:, 0:16])

    k.dma("sync", gx[:], io["gx"])
    k.dma("sync", gm[:], io["gm"])
    k.dma("sync", gf[:], io["gf"])
    if last:
        k.dma("sync", gl_[:], io["gl"])
    k.dma("sync", KT[:], io["KT"])

    assert 16 * N >= 2048
    for i in range(NE // 128):
        ld = V("xt", xt.t[:].rearrange("p a b -> p (a b)")[:, 0:2048])
        k.dma("sync", ld, io["down"].re(lambda a: a[i * 128:(i + 1) * 128, :]))
        tb = V("hn", hn.t[:].rearrange("p a b -> p (a b)")[:, 0:2048])
        for q4 in range(4):
            ps = pb[q4 % 4]
            for j4 in range(4):
                dc = q4 * 4 + j4
                k.tr(ps[:, j4 * 128:(j4 + 1) * 128], ld.re(lambda a: a[:, dc * 128:(dc + 1) * 128]), c["ident_f"][:])
            k.copy("vector" if q4 % 2 == 0 else "scalar", tb.re(lambda a: a[:, q4 * 512:(q4 + 1) * 512]), ps[:, 0:512])
        k.dma("sync", downT_b.re(lambda a: a[i]), tb)

    k.dma("sync", xt[:, :, 0:256], io["memT"].re(lambda a: a.rearrange("(c p) n -> p c n", p=128)))
    rms_fm(k, c, xt, gm, hn, 256, yt, pb[7][:, 0:256], rs)
    prot = PsumRot([pb[0], pb[1], pb[2], pb[3]])
    linear_fm(k, wkv_b.re(lambda a: a[:, 0:512]), 16, 512, hn, 256, wbufs, prot,
              lambda m, ps: k.copy("vector", kxT[:, m, :], ps))
    for half in range(2):
        wb = wbufs[half % 2]
        k.dma("sync", wb[:, :, 0:256], wkv_b.re(lambda a: a.rearrange("(kc p) m -> p kc m", p=128)[:, :, 512 + half * 256:512 + (half + 1) * 256]))
        for mc in range(2):
            ps = prot.next()
            for kc in range(16):
                k.mm(ps[:, 0:256], hn[:, kc, mc * 128:(mc + 1) * 128], wb[:, kc, 0:256], start=(kc == 0), stop=(kc == 15))
            k.copy("vector", vx[:, mc, half * 256:(half + 1) * 256], ps[:, 0:256])

    xTv = io["xT"].re(lambda a: a.rearrange("(c p) n -> p c n", p=128))
    yTv = io["yT"].re(lambda a: a.rearrange("(c p) n -> p c n", p=128))
    oTv = io["xoT"].re(lambda a: a.rearrange("(c p) n -> p c n", p=128))

    for ti in range(NT // N):
        n0 = ti * N
        if sel is None:
            k.dma("sync", xt[:, :, :], xTv.re(lambda a: a[:, :, n0:n0 + N]))
            k.dma("sync", yt[:, :, :], yTv.re(lambda a: a[:, :, n0:n0 + N]))
        else:
            xtmp = V("GG", GG.t[:, 0:32, :].rearrange("p a b -> p (a b)").bitcast(F32).rearrange("p (c n) -> p c n", c=16))
            ytmp = V("GG", GG.t[:, 32:48, :])
            for j in range(4):
                cj = j * NTS + n0
                k.dma("sync", xtmp, xTv.re(lambda a: a[:, :, cj:cj + N]))
                k.dma("sync", ytmp, yTv.re(lambda a: a[:, :, cj:cj + N]))
                if j == 0:
                    k.ts("vector", xt[:, :, :], xtmp, sel[:, 0:1], None, ALU.mult)
                    k.ts("vector", yt[:, :, :], ytmp, sel[:, 0:1], None, ALU.mult)
                else:
                    k.stt(xt[:, :, :], xtmp, sel[:, j:j + 1], xt[:, :, :], ALU.mult, ALU.add)
                    k.stt(yt[:, :, :], ytmp, sel[:, j:j + 1], yt[:, :, :], ALU.mult, ALU.add)
        prot = PsumRot([pb[0], pb[1], pb[2], pb[3]])

        def add_x(m, ps):
            k.tt("vector", xt[:, m, :], ps, xt[:, m, :], ALU.add)
        if STAGE >= 1:
            linear_fm(k, wout_b, 16, 2048, yt, N, wbufs, prot, add_x)
        if STAGE < 2:
            k.dma("sync", oTv.re(lambda a: a[:, :, n0:n0 + N]), xt[:, :, :])
            continue
        rms_fm(k, c, xt, gx, hn, N, yt, pb[7][:, 0:N], rs)
        linear_fm(k, wq_b, 16, 512, hn, N, wbufs, prot,
                  lambda m, ps: k.act(qx[:, m, :], ps, AF.Copy, scale=128 ** -0.5))
        for hd in range(4):
            for mc in range(2):
                ps = pb[4 + mc]
                k.mm(ps[:, 0:N], kxT[:, hd, mc * 128:(mc + 1) * 128], qx[:, hd, :])
                k.act(pT[mc][:, :], ps[:, 0:N], AF.Exp)
            for mc in range(2):
                k.mm(pb[6][:, 0:N], vx[:, mc, hd * 128:(hd + 1) * 128], pT[mc][:, :], start=(mc == 0), stop=(mc == 1))
            for mc in range(2):
                k.mm(pb[7][:, 0:N], c["ones_b"][:], pT[mc][:, :], start=(mc == 0), stop=(mc == 1))
            k.recip(rs[:, 0:N], pb[7][:, 0:N])
            k.tt("vector", ox[:, hd, :], pb[6][:, 0:N], rs[:, 0:N], ALU.mult)
        linear_fm(k, wo_b, 4, 2048, ox, N, wbufs, prot, add_x)
        if STAGE < 3:
            k.dma("sync", oTv.re(lambda a: a[:, :, n0:n0 + N]), xt[:, :, :])
            continue
        rms_fm(k, c, xt, gf, hn, N, yt, pb[7][:, 0:N], rs)
        linear_fm(k, wqry_b, 16, 1024, hn, N, wbufs, prot,
                  lambda m, ps: k.copy("scalar", qp[:, m, :], ps))
        for tc in range(N // 128):
            for hp in range(16):
                h, p = divmod(hp, 2)
                k.mm(pb[4 + hp // 4][:, (hp % 4) * 128:(hp % 4 + 1) * 128],
                     qp[:, h, tc * 128:(tc + 1) * 128], KT[:, hp, :])
            if SUB < 1:
                k.copy("vector", iT[:, tc * 128:(tc + 1) * 128], pb[4][:, 0:128])
                k.copy("vector", jT[:, tc * 128:(tc + 1) * 128], pb[5][:, 0:128])
                k.copy("vector", gT[:, tc * 128:(tc + 1) * 128], pb[7][:, 384:512])
                continue
            for hp in range(16):
                src = pb[4 + hp // 4][:, (hp % 4) * 128:(hp % 4 + 1) * 128]
                s_ = scr[hp % 2]
                k.gen("vector", lambda e, hp=hp, src=src: e.max(out=topv.t[:, hp, 0:8], in_=src.ap), reads=[src], writes=[topv[:]])
                k.gen("vector", lambda e, hp=hp, src=src: e.max_index(out=topi.t[:, hp, 0:8], in_max=topv.t[:, hp, 0:8], in_values=src.ap),
                      reads=[src, topv[:]], writes=[topi[:]])
                k.gen("vector", lambda e, hp=hp, src=src, s_=s_: e.match_replace(out=s_.t[:, 0:128], in_to_replace=topv.t[:, hp, 0:8], in_values=src.ap, imm_value=NEG),
                      reads=[src, topv[:]], writes=[s_[:]])
                k.gen("vector", lambda e, hp=hp, s_=s_: e.max(out=topv.t[:, hp, 8:16], in_=s_.t[:, 0:128]), reads=[s_[:]], writes=[topv[:]])
                k.gen("vector", lambda e, hp=hp, s_=s_: e.max_index(out=topi.t[:, hp, 8:16], in_max=topv.t[:, hp, 8:16], in_values=s_.t[:, 0:128]),
                      reads=[s_[:], topv[:]], writes=[topi[:]])
            if SUB < 2:
                k.copy("vector", iT[:, tc * 128:(tc + 1) * 128], V("topv", topv.t[:].rearrange("p a b -> p (a b)")[:, 0:128]))
                k.copy("vector", jT[:, tc * 128:(tc + 1) * 128], V("topi", topi.t[:].rearrange("p a b -> p (a b)")[:, 0:128]))
                k.copy("vector", gT[:, tc * 128:(tc + 1) * 128], V("topi", topi.t[:].rearrange("p a b -> p (a b)")[:, 128:256]))
                continue
            k.copy("vector", topif[:], topi[:])
            tv4 = topv.t[:].rearrange("p (h two) a -> p h two a", two=2)
            ti4 = topif.t[:].rearrange("p (h two) a -> p h two a", two=2)
            c4 = cand.t[:].rearrange("p h (a b) -> p h a b", a=16)
            k.tt("vector", V("cand", c4), V("topv", tv4[:, :, 0, :].unsqueeze(3).to_broadcast([128, 8, 16, 16])),
                 V("topv", tv4[:, :, 1, :].unsqueeze(2).to_broadcast([128, 8, 16, 16])), ALU.add)
            for h in range(8):
                s_ = scr[h % 2]
                k.gen("vector", lambda e, h=h: e.max(out=bv.t[:, h, 0:8], in_=cand.t[:, h, :]), reads=[cand[:]], writes=[bv[:]])
                k.gen("vector", lambda e, h=h: e.max_index(out=bp.t[:, h, 0:8], in_max=bv.t[:, h, 0:8], in_values=cand.t[:, h, :]),
                      reads=[cand[:], bv[:]], writes=[bp[:]])
                k.gen("vector", lambda e, h=h, s_=s_: e.match_replace(out=s_.t[:, :], in_to_replace=bv.t[:, h, 0:8], in_values=cand.t[:, h, :], imm_value=NEG),
                      reads=[cand[:], bv[:]], writes=[s_[:]])
                k.gen("vector", lambda e, h=h, s_=s_: e.max(out=bv.t[:, h, 8:16], in_=s_.t[:, :]), reads=[s_[:]], writes=[bv[:]])
                k.gen("vector", lambda e, h=h, s_=s_: e.max_index(out=bp.t[:, h, 8:16], in_max=bv.t[:, h, 8:16], in_values=s_.t[:, :]),
                      reads=[s_[:], bv[:]], writes=[bp[:]])
            if SUB < 3:
                k.copy("vector", iT[:, tc * 128:(tc + 1) * 128], V("bv", bv.t[:].rearrange("p a b -> p (a b)")))
                k.copy("vector", jT[:, tc * 128:(tc + 1) * 128], V("bp", bp.t[:].rearrange("p a b -> p (a b)")))
                continue
            k.tt("vector", gsel[:], bv[:], V("bv", bv.t[:, :, 0:1].to_broadcast([128, 8, 16])), ALU.subtract)
            k.act(gsel[:], gsel[:], AF.Exp)
            k.red(zs[:], gsel[:], ALU.add)
            k.recip(zs[:], zs[:])
            k.tt("vector", gsel[:], gsel[:], V("zs", zs.t[:, :].unsqueeze(2).to_broadcast([128, 8, 16])), ALU.mult)
            k.gen("vector", lambda e: e.tensor_single_scalar(out=bpi.t[:], in_=bp.t[:], scalar=4, op=ALU.logical_shift_right), reads=[bp[:]], writes=[bpi[:]])
            k.copy("vector", akf[:], bpi[:])
            k.gen("vector", lambda e: e.tensor_single_scalar(out=bpi.t[:], in_=bp.t[:], scalar=15, op=ALU.bitwise_and), reads=[bp[:]], writes=[bpi[:]])
            k.copy("vector", bkf[:], bpi[:])
            if SUB < 4:
                k.copy("vector", iT[:, tc * 128:(tc + 1) * 128], V("akf", akf.t[:].rearrange("p a b -> p (a b)")))
                k.copy("vector", jT[:, tc * 128:(tc + 1) * 128], V("bkf", bkf.t[:].rearrange("p a b -> p (a b)")))
                k.copy("vector", gT[:, tc * 128:(tc + 1) * 128], V("gsel", gsel.t[:].rearrange("p a b -> p (a b)")))
                continue
            io16 = iota16.t[:, :].unsqueeze(1).unsqueeze(1).to_broadcast([128, 8, 16, 16])
            for (kf, pp, dst) in ((akf, 0, isel), (bkf, 1, jsel)):
                k.tt("vector", V("cand", c4), V(kf.name, kf.t[:].unsqueeze(3).to_broadcast([128, 8, 16, 16])), V("iota16", io16), ALU.is_equal)
                k.tt("vector", V("cand", c4), V("cand", c4), V("topif", ti4[:, :, pp, :].unsqueeze(2).to_broadcast([128, 8, 16, 16])), ALU.mult)
                k.red(dst[:], V("cand", cand.t[:].rearrange("p h (a b) -> p (h a) b", a=16)), ALU.add)
            for (src, dstT) in ((isel[:], iT), (jsel[:], jT), (V("gsel", gsel.t[:].rearrange("p h a -> p (h a)")), gT)):
                k.tr(pb[0][:, 0:128], src, c["ident_f"][:])
                k.copy("vector", dstT[:, tc * 128:(tc + 1) * 128], pb[0][:, 0:128])
        if STAGE < 4:
            k.dma("sync", V("dram:xoT", oTv.ap[:, 0, n0:n0 + N]), iT[:, :])
            k.dma("sync", V("dram:xoT", oTv.ap[:, 1, n0:n0 + N]), jT[:, :])
            k.dma("sync", V("dram:xoT", oTv.ap[:, 2, n0:n0 + N]), gT[:, :])
            continue
        TPE = 512 // NI
        for ih in range(NIH):
            gro = PsumRot([pb[4], pb[5], pb[6], pb[7]])
            for t0 in range(0, N, TPE):
                ps = gro.next()
                for tq in range(TPE):
                    t = t0 + tq
                    a_ = At[t % 4]
                    b_ = Bt[t % 4]
                    k.ts("vector", a_[:, :], c["iota_f"][:, ih * NI:(ih + 1) * NI], iT[:, t:t + 1], gT[:, t:t + 1], ALU.is_equal, ALU.mult)
                    k.ts("gpsimd", b_[:, :], c["iota_f"][:, :], jT[:, t:t + 1], None, ALU.is_equal)
                    k.mm(ps[:, tq * NI:(tq + 1) * NI], b_[:, :], a_[:, :])
                k.copy("vector" if (t0 // TPE) % 2 == 0 else "scalar",
                       V("GG", GG.t[:, :, t0:t0 + TPE].rearrange("p i t -> p t i")),
                       V(ps.name, ps.t[:, 0:TPE * NI].rearrange("p (t i) -> p t i", i=NI)))
            hro = PsumRot([pb[4], pb[5], pb[6], pb[7]])
            for il in range(NI):
                i = ih * NI + il
                db = dbuf[il % 4]
                k.dma("sync" if il % 2 == 0 else "scalar", V(db.name, db.t[:].rearrange("p a b -> p (a b)")), downT_b.re(lambda a: a[i]))
                ps = hro.next()
                for kc in range(16):
                    k.mm(ps[:, 0:N], db[:, kc, :], hn[:, kc, :], start=(kc == 0), stop=(kc == 15))
                g_ = glb[il % 4]
                k.act(g_[:, :], ps[:, 0:N], AF.Gelu)
                k.tt("gpsimd", GG[:, il, :], g_[:, :], GG[:, il, :], ALU.mult)
            for dh in range(2):
                for b4 in range(4):
                    k.memset("vector", pb[b4][:, :], 0.0)
                for il in range(NI):
                    i = ih * NI + il
                    ub = ubuf[il % 4]
                    k.dma("sync" if il % 2 == 0 else "scalar", ub[:, 0:1024], up_b.re(lambda a: a[i * 128:(i + 1) * 128, dh * 1024:(dh + 1) * 1024]))
                    for dc in range(8):
                        k.mm(pb[dc // 2][:, (dc % 2) * 256:(dc % 2) * 256 + N], ub[:, dc * 128:(dc + 1) * 128], GG[:, il, :],
                             start=False, stop=False, skip_group_check=True)
                for dc in range(8):
                    k.tt("vector", xt[:, dh * 8 + dc, :], pb[dc // 2][:, (dc % 2) * 256:(dc % 2) * 256 + N], xt[:, dh * 8 + dc, :], ALU.add)
        if last:
            rms_fm(k, c, xt, gl_, hn, N, yt, pb[7][:, 0:N], rs)
            for cc in range(16):
                k.stt(xt[:, cc, :], xt[:, cc, :], gl_[:, cc:cc + 1], rs[:, 0:N], ALU.mult, ALU.mult)
        k.dma("sync", oTv.re(lambda a: a[:, :, n0:n0 + N]), xt[:, :, :])


def make_KT(subk):
    KT = np.zeros((128, 16, 128), np.float32)
    for h in range(8):
        for p in range(2):
            KT[64 * p:64 * p + 64, 2 * h + p, :] = subk[h, p].T
    return KT


WA = 1800
C_MI, C_MF = 896, 1024
C_V, C_Z, C_MV, C_MO, C_SM = 1152, 1280, 1536, 1664, 1792
PEN = 10000.0
SA = 99
SUBA = 99


def emit_phase_a(k, c, io, S):
    N = 256
    NB = S // 256
    pb = c["pb"]
    T = k.tile
    w_sb = T("w_sb", [128, 16, WA], BF16)
    kT_all = T("kT_all", [128, S], BF16)
    V_all = T("V_all", [128, S // 128, 2, 66], BF16)
    xt = T("xt", [128, 16, N], F32)
    hn = T("hn", [128, 16, N], BF16)
    rs = T("rs", [128, N], F32)
    gmx = T("gmx", [128, 16], F32)
    qz = [T(f"qz{h}", [128, N], BF16) for h in range(2)]
    kmT = T("kmT", [128, 64], BF16)
    kmf = T("kmf", [128, 64], F32)
    ksum = T("ksum", [128, 1], F32)
    gate_sb = [T(f"gate_sb{h}", [128, 64], F32) for h in range(2)]
    cst = [T(f"cst{h}", [128, 64], F32) for h in range(2)]
    gtmp = T("gtmp", [128, 64], F32)
    m8 = T("m8", [128, 8], F32)
    Ttab = T("Ttab", [128, 2, 2, 64], F32)
    ownc = T("ownc", [128, 2, 2], F32)
    skk = T("skk", [128, 2, 256], BF16)
    cbias = T("cbias", [128, 2, 256], BF16)
    onesrow = T("onesrow", [128, 128], BF16)
    onesrow_f = T("onesrow_f", [128, 128], F32)
    Pb = [T(f"Pb{i}", [128, 256], BF16) for i in range(2)]
    PT = [T(f"PT{i}", [128, 2, 128], BF16) for i in range(3)]
    ya_tok = T("ya_tok", [128, 128], F32)
    rden = T("rden", [128, 1], F32)
    yT_tile = T("yT_tile", [128, 4, N], BF16)
    cw = T("cw", [128, 5, 4], F32)
    cb = T("cb", [128, 5], F32)
    hp = T("hp", [128, 16], F32)
    Aneg = T("Aneg", [128, 4], F32)
    nfb = T("nfb", [128, 1], F32)
    gsn = T("gsn", [128, 256], F32)
    gmn = T("gmn", [128, 128], F32)
    wq_m = T("wq_m", [128, 128], F32)
    wk_m = T("wk_m", [128, 128], F32)
    cin = T("cin", [128, 5, 3 + N], F32)
    cacc = T("cacc", [128, N], F32)
    cout = T("cout", [128, 5, N], F32)
    zs = [T(f"zs{i}", [128, 256], F32) for i in range(2)]
    mv_sb = [T(f"mv_sb{i}", [128, 128], F32) for i in range(2)]
    sig = [T(f"sig{i}", [128, 128], F32) for i in range(2)]
    sm_sb = [T(f"sm_sb{i}", [128, 8], F32) for i in range(2)]
    dt4 = T("dt4", [128, 4], F32)
    a4 = T("a4", [128, 4], F32)
    cs4 = T("cs4", [128, 4], F32)
    ecs = T("ecs", [128, 4], F32)
    dte = T("dte", [128, 4], F32)
    cdb = T("cdb", [128, 4], F32)
    xs_sb = T("xs_sb", [128, 256], F32)
    Xdt = T("Xdt", [128, 256], F32)
    Xdte = T("Xdte", [128, 256], F32)
    B_tok = T("B_tok", [128, 128], F32)
    CBm = T("CBm", [128, 128], F32)
    Ah = [T(f"Ah{i}", [128, 128], F32) for i in range(2)]
    Eh = [T(f"Eh{i}", [128, 128], F32) for i in range(2)]
    MT = [T(f"MT{i}", [128, 128], F32) for i in range(2)]
    hstate = T("hstate", [128, 256], F32)
    y1 = T("y1", [128, 256], F32)
    y2 = T("y2", [128, 256], F32)
    ssq = T("ssq", [128, 1], F32)
    ysn = T("ysn", [128, 256], F32)
    qmT = T("qmT", [128, N], F32)
    kmT2 = T("kmT2", [128, N], F32)
    k_tok = T("k_tok", [128, 128], F32)
    KQm = T("KQm", [128, 128], F32)
    vw = T("vw", [128, 130], F32)
    vwe = T("vwe", [128, 130], F32)
    Cn = T("Cn", [128, 130], F32)
    t1 = T("t1", [128, 130], F32)
    t2 = T("t2", [128, 130], F32)
    dd = T("dd", [128, 1], F32)
    hm = T("hm", [128, 128], F32)
    hm2 = T("hm2", [128, 128], F32)
    cols = T("cols", [128, 8], F32)
    spsl = T("spsl", [128, 2], F32)
    NR = 12
    R = T("R", [128, NR, 128], F32)
    rowsb = T("rowsb", [128, 512], F32)
    ms = T("ms", [128, 8], F32)
    R_L1, R_IG, R_BN, R_W, R_CM, R_MX, R_EU, R_IW, R_EMT, R_EW, R_WE, R_Z = range(12)

    wv = io["win"].re(lambda a: a.rearrange("(kc p) m -> p kc m", p=128))
    for q4 in range(4):
        k.dma("gpsimd", w_sb[:, q4 * 4:(q4 + 1) * 4, :], wv.re(lambda a: a[:, q4 * 4:(q4 + 1) * 4, :]))
    for dst, nm in ((gmx, "gmix"), (cw, "cw"), (cb, "cb"), (hp, "hp"), (gsn, "gsn"), (gmn, "gmn"), (wq_m, "wqm"), (wk_m, "wkm"),
                    (Ttab, "Ttab"), (ownc, "ownc")):
        k.dma("sync", dst[:], io[nm])
    k.dma("sync", xt[:, 0:2, :], io["skk"])
    k.dma("sync", xt[:, 2:4, :], io["cbias"])
    k.copy("vector", skk[:], xt[:, 0:2, :])
    k.copy("vector", cbias[:], xt[:, 2:4, :])
    k.memset("vector", onesrow[:], 0.0)
    k.memset("vector", onesrow[0:1, :], 1.0)
    k.memset("vector", onesrow_f[:], 0.0)
    k.memset("vector", onesrow_f[0:1, :], 1.0)
    k.act(Aneg[:], hp[:, 4:8], AF.Exp)
    k.ts("vector", Aneg[:], Aneg[:], -1.0, None, ALU.mult)
    k.ts("vector", nfb[:], hp[:, 13:14], -1.0, None, ALU.mult)
    for h in range(2):
        k.memset("vector", qz[h][:], 0.0)
        k.memset("vector", gate_sb[h][:], NEG)
        k.memset("vector", cst[h][:], 0.0)
    k.memset("vector", kmT[:], 0.0)
    k.memset("vector", kmf[:], 0.0)
    k.memset("gpsimd", V_all[:], 1.0)
    k.memset("vector", cin[:], 0.0)
    k.memset("vector", hstate[:], 0.0)
    k.memset("vector", Cn[:], 0.0)
    k.memset("vector", vw[:], 0.0)
    k.memset("vector", vwe[:], 0.0)
    k.memset("gpsimd", R[:], 0.0)
    k.memset("vector", ms[:], 0.0)
    k.memset("vector", ms[0:1, 0:1], NEG)

    k.memset("vector", yT_tile[:], 0.0)
    xTv = io["xT"].re(lambda a: a.rearrange("(c p) n -> p c n", p=128))
    yTv = io["yT"].re(lambda a: a.rearrange("(c p) n -> p c n", p=128))
    rotA = PsumRot([pb[0], pb[1]])
    one1 = c["ones_f"][:, 0:1]

    for ti in range(S // N):
        t0 = ti * N
        blk = ti
        k.dma("sync", xt[:, :, :], xTv.re(lambda a: a[:, :, t0:t0 + N]))
        if SUBA < 0.1:
            k.dma("sync", yTv.re(lambda a: a[:, :, t0:t0 + N]), yT_tile[:, :, :])
            continue
        rms_fm(k, c, xt, gmx, hn, N, hn, pb[7][:, 0:N], rs)
        for m in range(7 if SUBA >= 1 else (0 if SUBA < 0.3 else (1 if SUBA < 0.5 else 2))):
            ps = rotA.next()
            for kc in range(16):
                k.mm(ps[:, 0:N], w_sb[:, kc, m * 128:(m + 1) * 128], hn[:, kc, :], start=(kc == 0), stop=(kc == 15))
            if m == 0:
                k.act(qz[0][0:64, :], ps[0:64, 0:N], AF.Copy, scale=0.125)
                k.act(qz[1][64:128, :], ps[64:128, 0:N], AF.Copy, scale=0.125)
            elif m == 1:
                k.act(kT_all[:, t0:t0 + N], ps[:, 0:N], AF.Copy, accum=ksum[:])
                k.ts("vector", kmf[:, blk:blk + 1], ksum[:], 1.0 / 256, None, ALU.mult)
                k.copy("vector", kmT[:], kmf[:])
            else:
                j = m - 2
                k.copy("vector" if j % 2 == 0 else "scalar", cin[:, j, 3:3 + N], ps[:, 0:N])
        if SUBA < 2:
            k.dma("sync", yTv.re(lambda a: a[:, :, t0:t0 + N]), yT_tile[:, :, :])
            continue
        for r, col in ((0, C_MI), (1, C_MF)):
            for kc in range(16):
                k.mm(pb[7][:, r * 256:(r + 1) * 256], w_sb[:, kc, col:col + 128], hn[:, kc, :], start=(kc == 0), stop=(kc == 15))
        k.copy("vector", rowsb[0:1, :], pb[7][0:1, 0:512])
        if SUBA < 3:
            k.dma("sync", yTv.re(lambda a: a[:, :, t0:t0 + N]), yT_tile[:, :, :])
            continue
        for ch in range(2):
            c0 = ch * 128
            b1 = rotA.next()
            b2 = rotA.next()
            for kc in range(16):
                k.mm(b1[:, 0:512], hn[:, kc, c0:c0 + 128], w_sb[:, kc, C_V:C_MO], start=(kc == 0), stop=(kc == 15))
            for kc in range(16):
                k.mm(b2[:, 0:136], hn[:, kc, c0:c0 + 128], w_sb[:, kc, C_MO:C_MO + 136], start=(kc == 0), stop=(kc == 15))
            gch = ti * 2 + ch
            k.copy("vector", V("V_all", V_all.t[:, gch, :, 0:64]), V(b1.name, b1.t[:, 0:128].rearrange("p (h d) -> p h d", h=2)))
            k.act(zs[ch][:], b1[:, 128:384], AF.Silu)
            k.copy("vector", mv_sb[ch][:], b1[:, 384:512])
            k.act(sig[ch][:], b2[:, 0:128], AF.Sigmoid)
            k.copy("vector", sm_sb[ch][:], b2[:, 128:136])

        si = 0
        for qc in range(2 if SA >= 1 else 0):
            qs = slice(qc * 128, (qc + 1) * 128)
            for h in range(2):
                if blk > 0:
                    k.mm(pb[7][:, 0:64], qz[h][:, qs], kmT[:, 0:64])
                    k.copy("vector", gate_sb[h][:, 0:blk], pb[7][:, 0:blk])
                    k.gen("vector", lambda e, h=h: e.max(out=m8.t[:, :], in_=gate_sb[h].t[:, :]), reads=[gate_sb[h][:]], writes=[m8[:]])
                    k.ts("vector", gtmp[:, 0:blk], gate_sb[h][:, 0:blk], m8[:, 2:3], PEN, ALU.is_ge, ALU.mult)
                    k.tt("vector", cst[h][:, 0:blk], gtmp[:, 0:blk], Ttab[:, qc, h, NB - 1 - blk:NB - 1], ALU.add)
                nblk = blk + 1
                ug0 = si
                si += nblk

                def st1(n, h=h, qs=qs, qc=qc, ug0=ug0):
                    u = ug0 + n
                    sp_, P_ = pb[2 + u % 2], Pb[u % 2]
                    own = (n == blk)
                    k.mm(sp_[:, 0:256], qz[h][:, qs], kT_all[:, n * 256:(n + 1) * 256], start=True, stop=False)
                    k.mm(sp_[:, 0:256], onesrow[:], skk[:, h, :], start=False, stop=(not own))
                    if own:
                        k.mm(sp_[:, 0:256], c["ident_b"][:], cbias[:, qc, :], start=False, stop=True)
                    bias = ownc[:, qc, h:h + 1] if own else cst[h][:, n:n + 1]
                    k.act(P_[:, :], sp_[:, 0:256], AF.Exp, bias=bias)

                def st2(n, ug0=ug0):
                    u = ug0 + n
                    pt_, P_, PT_ = pb[4 + u % 2], Pb[u % 2], PT[u % 3]
                    ptv = V(pt_.name, pt_.t[:, 0:128].bitcast(BF16).rearrange("p (c q) -> p c q", c=2))
                    for kc in range(2):
                        k.tr(V(pt_.name, ptv.ap[:, kc, :]), P_[:, kc * 128:(kc + 1) * 128], c["ident_b"][:])
                    k.copy("vector", PT_[:, :, :], ptv)

                def st3(n, h=h, ug0=ug0):
                    u = ug0 + n
                    PT_ = PT[u % 3]
                    own = (n == blk)
                    for kc in range(2):
                        k.mm(pb[6][:, 0:66], PT_[:, kc, :], V("V_all", V_all.t[:, n * 2 + kc, h, :]),
                             start=(n == 0 and kc == 0), stop=(own and kc == 1))

                for step in range(nblk + 2):
                    if step < nblk:
                        st1(step)
                    if 1 <= step <= nblk:
                        st2(step - 1)
                    if step >= 2:
                        st3(step - 2)
                k.recip(rden[:], pb[6][:, 64:65])
                k.ts("vector", ya_tok[:, h * 64:(h + 1) * 64], pb[6][:, 0:64], rden[:, 0:1], None, ALU.mult)
            k.tr(pb[7][:, 0:128], ya_tok[:], c["ident_f"][:])
            k.copy("scalar", yT_tile[:, 0, qs], pb[7][:, 0:128])

        if SA < 2:
            k.dma("sync", yTv.re(lambda a: a[:, :, t0:t0 + N]), yT_tile[:, :, :])
            continue
        for j in range(5):
            k.ts("vector", cacc[:], cin[:, j, 0:N], cw[:, j, 0:1], None, ALU.mult)
            for tp in range(1, 4):
                k.stt(cacc[:], cin[:, j, tp:tp + N], cw[:, j, tp:tp + 1], cacc[:], ALU.mult, ALU.add)
            k.act(cout[:, j, :], cacc[:], AF.Silu, bias=cb[:, j:j + 1])
            k.copy("vector", cin[:, j, 0:3], cin[:, j, N:N + 3])
        rot = PsumRot([pb[0], pb[1], pb[2], pb[3], pb[4], pb[5], pb[6]])
        ps = rot.next()
        k.mm(ps[:, 0:N], wq_m[:], cout[:, 4, :])
        k.copy("vector", qmT[:], ps[:, 0:N])
        ps = rot.next()
        k.mm(ps[:, 0:N], wk_m[:], cout[:, 4, :])
        k.act(kmT2[:], ps[:, 0:N], AF.Copy, scale=128 ** -0.5)

        for ch in range(2 if SA >= 3 else 0):
            cs_ = slice(ch * 128, (ch + 1) * 128)
            k.tt("vector", dt4[:], sm_sb[ch][:, 0:4], hp[:, 0:4], ALU.add)
            k.act(dt4[:], dt4[:], AF.Exp)
            k.act(dt4[:], dt4[:], AF.Ln, bias=one1)
            k.tt("vector", a4[:], dt4[:], Aneg[:], ALU.mult)
            k.mm(pb[7][:, 0:4], c["tri_f"][:], a4[:])
            k.mm(pb[7][:, 4:8], c["ones_f"][:], a4[:])
            k.copy("vector", cs4[:], pb[7][:, 0:4])
            k.act(ecs[:], pb[7][:, 0:4], AF.Exp)
            k.tt("vector", dte[:], pb[7][:, 4:8], cs4[:], ALU.subtract)
            k.act(dte[:], dte[:], AF.Exp)
            k.act(cdb[:], pb[7][:, 4:8], AF.Exp)
            pxs = rot.next()
            for j in range(2):
                k.tr(pxs[:, j * 128:(j + 1) * 128], cout[:, j, cs_], c["ident_f"][:])
            k.copy("scalar", xs_sb[:], pxs[:, 0:256])
            for h in range(4):
                hs = slice(h * 64, (h + 1) * 64)
                k.ts("vector", Xdt[:, hs], xs_sb[:, hs], dt4[:, h:h + 1], None, ALU.mult)
                k.ts("vector", Xdte[:, hs], Xdt[:, hs], dte[:, h:h + 1], None, ALU.mult)
            pbt = rot.next()
            k.tr(pbt[:, 0:128], cout[:, 2, cs_], c["ident_f"][:])
            k.copy("scalar", B_tok[:], pbt[:, 0:128])
            pcb = rot.next()
            k.mm(pcb[:, 0:128], cout[:, 2, cs_], cout[:, 3, cs_])
            k.tt("vector", CBm[:], pcb[:, 0:128], c["tri_f"][:], ALU.mult)
            pyd = rot.next()
            for h in range(4):
                A_, E_, M_ = Ah[h % 2], Eh[h % 2], MT[h % 2]
                k.ts("vector", A_[:], c["upp_f"][:], a4[:, h:h + 1], None, ALU.mult)
                pdm = rot.next()
                k.mm(pdm[:, 0:128], A_[:], c["tri_f"][:])
                k.act(E_[:], pdm[:, 0:128], AF.Exp)
                k.tt("gpsimd", M_[:], E_[:], CBm[:], ALU.mult)
                k.mm(pyd[:, h * 64:(h + 1) * 64], M_[:], Xdt[:, h * 64:(h + 1) * 64])
            pyo = rot.next()
            k.mm(pyo[:, 0:256], cout[:, 3, cs_], hstate[:])
            pst = rot.next()
            k.mm(pst[:, 0:256], B_tok[:], Xdte[:])
            for h in range(4):
                hs = slice(h * 64, (h + 1) * 64)
                k.stt(y1[:, hs], xs_sb[:, hs], hp[:, 8 + h:9 + h], pyd[:, hs], ALU.mult, ALU.add)
                k.stt(y2[:, hs], pyo[:, hs], ecs[:, h:h + 1], y1[:, hs], ALU.mult, ALU.add)
                k.stt(hstate[:, hs], hstate[:, hs], cdb[:, h:h + 1], pst[:, hs], ALU.mult, ALU.add)
            k.tt("vector", y2[:], y2[:], zs[ch][:], ALU.mult)
            k.act(y1[:], y2[:], AF.Square, accum=ssq[:])
            k.act(ssq[:], ssq[:], AF.Ln, bias=c["eps"][:, 0:1], scale=1.0 / 256)
            k.act(ssq[:], ssq[:], AF.Exp, scale=-0.5)
            k.stt(ysn[:], y2[:], ssq[:, 0:1], gsn[:], ALU.mult, ALU.mult)
            for j in range(2):
                pt2 = rot.next()
                k.tr(pt2[:, 0:128], ysn[:, j * 128:(j + 1) * 128], c["ident_f"][:])
                k.copy("scalar", yT_tile[:, 1 + j, cs_], pt2[:, 0:128])

            if SA < 4:
                continue
            def row(r):
                return R[0:1, r, :]
            k.act(row(R_L1), rowsb[0:1, 256 + ch * 128:256 + (ch + 1) * 128], AF.Exp, bias=nfb[0:1, 0:1], scale=-1.0)
            k.act(row(R_L1), row(R_L1), AF.Ln, bias=one1.re(lambda a: a[0:1, :]))
            k.ts("vector", row(R_IG), rowsb[0:1, ch * 128:(ch + 1) * 128], hp[0:1, 12:13], None, ALU.add)
            k.gen("vector", lambda e, ch=ch: e.tensor_tensor_scan(out=R.t[0:1, R_BN, :], data0=R.t[0:1, R_L1, :],
                                                                   data1=R.t[0:1, R_Z, :], initial=0.0, op0=ALU.add, op1=ALU.add),
                  reads=[R[:]], writes=[R[:]])
            k.tt("vector", row(R_W), row(R_IG), row(R_BN), ALU.add)
            k.gen("vector", lambda e, ch=ch: e.tensor_tensor_scan(out=R.t[0:1, R_CM, :], data0=R.t[0:1, R_W, :],
                                                                   data1=R.t[0:1, R_W, :], initial=NEG, op0=ALU.max, op1=ALU.max),
                  reads=[R[:]], writes=[R[:]])
            k.ts("vector", row(R_MX), row(R_CM), ms[0:1, 0:1], None, ALU.max)
            k.act(row(R_EU), row(R_MX), AF.Exp, scale=-1.0)
            k.act(row(R_IW), row(R_MX), AF.Exp, scale=-1.0, bias=ms[0:1, 0:1])
            k.tt("vector", row(R_EMT), row(R_BN), row(R_MX), ALU.subtract)
            k.act(row(R_EMT), row(R_EMT), AF.Exp)
            k.act(row(R_EW), row(R_W), AF.Exp)
            cmL = R[0:1, R_CM, 127:128]
            bnL = R[0:1, R_BN, 127:128]
            k.ts("vector", ms[0:1, 1:2], cmL, -1.0, None, ALU.mult)
            k.act(row(R_WE), row(R_W), AF.Exp, bias=ms[0:1, 1:2])
            k.tt("vector", ms[0:1, 2:3], ms[0:1, 0:1], bnL, ALU.subtract)
            k.tt("vector", ms[0:1, 3:4], cmL, bnL, ALU.subtract)
            k.tt("vector", ms[0:1, 4:5], ms[0:1, 2:3], ms[0:1, 3:4], ALU.max)
            k.ts("vector", ms[0:1, 5:6], ms[0:1, 4:5], -1.0, None, ALU.mult)
            k.act(R[0:1, R_Z + 0, 0:0 + 1] if False else ms[0:1, 6:7], ms[0:1, 2:3], AF.Exp, bias=ms[0:1, 5:6])
            k.act(ms[0:1, 7:8], ms[0:1, 3:4], AF.Exp, bias=ms[0:1, 5:6])
            pcl = rot.next()
            for ci, r in enumerate((R_EU, R_IW, R_EMT, R_EW, R_WE)):
                k.mm(pcl[:, 2 * ci:2 * ci + 2], R[:, r, :], c["ones_f"][:, 0:2])
            k.mm(pcl[:, 16:18], onesrow_f[:], ms[:, 6:8])
            k.copy("vector", cols[:, 0:5], V(pcl.name, pcl.t[:, 0:10].rearrange("p (c two) -> p c two", two=2)[:, :, 0]))
            k.copy("vector", spsl[:], pcl[:, 16:18])
            pkt = rot.next()
            k.mm(pkt[:, 0:128], cout[:, 4, cs_], wk_m[:])
            k.act(k_tok[:], pkt[:, 0:128], AF.Copy, scale=128 ** -0.5)
            pkq = rot.next()
            k.mm(pkq[:, 0:128], kmT2[:, cs_], qmT[:, cs_])
            k.tt("vector", KQm[:], pkq[:, 0:128], c["tri_f"][:], ALU.mult)
            k.ts("vector", vw[:, 0:128], mv_sb[ch][:], cols[:, 3:4], None, ALU.mult)
            k.copy("vector", vw[:, 128:129], cols[:, 3:4])
            k.ts("vector", vwe[:, 0:128], mv_sb[ch][:], cols[:, 4:5], None, ALU.mult)
            k.copy("vector", vwe[:, 128:129], cols[:, 4:5])
            pin = rot.next()
            k.mm(pin[:, 0:130], KQm[:], vw[:])
            pit = rot.next()
            k.mm(pit[:, 0:130], qmT[:, cs_], Cn[:])
            k.ts("vector", t1[:], pin[:, 0:130], cols[:, 0:1], None, ALU.mult)
            k.stt(t2[:], pit[:, 0:130], cols[:, 1:2], t1[:], ALU.mult, ALU.add)
            k.ts("vector", dd[:], t2[:, 128:129], -1.0, None, ALU.mult)
            k.tt("vector", dd[:], dd[:], t2[:, 128:129], ALU.max)
            k.tt("vector", dd[:], dd[:], cols[:, 2:3], ALU.max)
            k.recip(dd[:], dd[:])
            k.ts("vector", hm[:], t2[:, 0:128], dd[:, 0:1], None, ALU.mult)
            k.act(hm2[:], hm[:], AF.Square, accum=ssq[:])
            k.act(ssq[:], ssq[:], AF.Ln, bias=c["eps"][:, 0:1], scale=1.0 / 128)
            k.act(ssq[:], ssq[:], AF.Exp, scale=-0.5)
            k.stt(hm2[:], hm[:], ssq[:, 0:1], gmn[:], ALU.mult, ALU.mult)
            k.tt("vector", hm2[:], hm2[:], sig[ch][:], ALU.mult)
            pt3 = rot.next()
            k.tr(pt3[:, 0:128], hm2[:], c["ident_f"][:])
            k.copy("scalar", yT_tile[:, 3, cs_], pt3[:, 0:128])
            pcl2 = rot.next()
            k.mm(pcl2[:, 0:130], k_tok[:], vwe[:])
            k.ts("vector", t1[:], pcl2[:, 0:130], spsl[:, 1:2], None, ALU.mult)
            k.stt(Cn[:], Cn[:], spsl[:, 0:1], t1[:], ALU.mult, ALU.add)
            k.copy("vector", ms[0:1, 0:1], ms[0:1, 4:5])
        k.dma("sync", yTv.re(lambda a: a[:, :, t0:t0 + N]), yT_tile[:, :, :])


def phase_a_host_inputs(inp, l, b, g, S):
    w_in = inp["w_in"][l]
    o = np.cumsum([0, 512, 512, 512, 1024, 2048, 16, 512, 512, 512, 4, 4])
    oq, ok, ov, oz, oxbc, odt, omu, omv, omo, omi, omf = o[:11]
    win = np.zeros((2048, WA), np.float32)
    win[:, 0:128] = w_in[:, oq + 128 * g: oq + 128 * (g + 1)]
    win[:, 128:256] = w_in[:, ok + 128 * g: ok + 128 * (g + 1)]
    win[:, 256:512] = w_in[:, oxbc + 256 * g: oxbc + 256 * (g + 1)]
    win[:, 512:640] = w_in[:, oxbc + 1024 + 128 * g: oxbc + 1024 + 128 * (g + 1)]
    win[:, 640:768] = w_in[:, oxbc + 1536 + 128 * g: oxbc + 1536 + 128 * (g + 1)]
    win[:, 768:896] = w_in[:, omu + 128 * g: omu + 128 * (g + 1)]
    win[:, C_V:C_V + 128] = w_in[:, ov + 128 * g: ov + 128 * (g + 1)]
    win[:, C_Z:C_Z + 256] = w_in[:, oz + 256 * g: oz + 256 * (g + 1)]
    win[:, C_MV:C_MV + 128] = w_in[:, omv + 128 * g: omv + 128 * (g + 1)]
    win[:, C_MO:C_MO + 128] = w_in[:, omo + 128 * g: omo + 128 * (g + 1)]
    win[:, C_SM:C_SM + 4] = w_in[:, odt + 4 * g: odt + 4 * (g + 1)]
    win[:, C_MI] = w_in[:, omi + g]
    win[:, C_MF] = w_in[:, omf + g]
    cw = np.zeros((128, 5, 4), np.float32)
    cb = np.zeros((128, 5), np.float32)
    scw, scb = inp["ssm_conv_w"][l], inp["ssm_conv_b"][l]
    chans = [np.arange(256 * g, 256 * g + 128), np.arange(256 * g + 128, 256 * g + 256),
             np.arange(1024 + 128 * g, 1024 + 128 * (g + 1)), np.arange(1536 + 128 * g, 1536 + 128 * (g + 1))]
    for j, ch in enumerate(chans):
        cw[:, j, :] = scw[:, ch].T
        cb[:, j] = scb[ch]
    cw[:, 4, :] = inp["mlstm_conv_w"][l][:, 128 * g:128 * (g + 1)].T
    cb[:, 4] = inp["mlstm_conv_b"][l][128 * g:128 * (g + 1)]
    hp = np.zeros((128, 16), np.float32)
    hp[:, 0:4] = inp["ssm_dt_bias"][l][4 * g:4 * g + 4]
    hp[:, 4:8] = inp["ssm_A_log"][l][4 * g:4 * g + 4]
    hp[:, 8:12] = inp["ssm_D"][l][4 * g:4 * g + 4]
    hp[:, 12] = inp["mlstm_i_bias"][l][g]
    hp[:, 13] = inp["mlstm_f_bias"][l][g]
    gsn = np.broadcast_to(inp["ssm_norm_g"][l][256 * g:256 * (g + 1)], (128, 256)).copy()
    gmn = np.broadcast_to(inp["mlstm_norm_g"][l][128 * g:128 * (g + 1)], (128, 128)).copy()
    NB = S // 256
    slopes = 2.0 ** (-8.0 * (np.arange(1, 9)) / 8)
    Ttab = np.zeros((128, 2, 2, 64), np.float32)
    ownc = np.zeros((128, 2, 2), np.float32)
    skk = np.zeros((128, 2, 256), np.float32)
    cbias = np.zeros((128, 2, 256), np.float32)
    tl = np.arange(128)
    for par in range(2):
        qq = tl + 128 * par
        for hh in range(2):
            sl = slopes[2 * g + hh]
            for m in range(NB):
                Ttab[:, par, hh, m] = -PEN - sl * (qq + 256.0 * (NB - 1 - m))
            ownc[:, par, hh] = -sl * qq
        cbias[:, par, :] = np.where(np.arange(256)[None, :] <= qq[:, None], 0.0, -PEN)
    for hh in range(2):
        skk[0, hh, :] = slopes[2 * g + hh] * np.arange(256)
    gm = inp["mix_norm_g"][l]
    return dict(win=win, gmix=np.ascontiguousarray(gm.reshape(16, 128).T), cw=cw, cb=cb, hp=hp, gsn=gsn, gmn=gmn,
                wqm=np.ascontiguousarray(inp["mlstm_wq"][l][g]), wkm=np.ascontiguousarray(inp["mlstm_wk"][l][g]),
                Ttab=Ttab, ownc=ownc, skk=skk, cbias=cbias)


A_INPUT_SHAPES = dict(win=[2048, WA], gmix=[128, 16], cw=[128, 5, 4], cb=[128, 5], hp=[128, 16], gsn=[128, 256], gmn=[128, 128],
                      wqm=[128, 128], wkm=[128, 128], Ttab=[128, 2, 2, 64], ownc=[128, 2, 2], skk=[128, 2, 256], cbias=[128, 2, 256])


from concourse.bass_utils import run_bass_kernel_spmd

SEQ = 16384
_CACHE = {}


def _build_a(S):
    key = ("a", S)
    if key in _CACHE:
        return _CACHE[key]
    nc = bass.Bass("TRN2", target_bir_lowering=False)
    k = K(nc)
    io = {nm: V("dram:" + nm, nc.dram_tensor(nm, list(shp), F32, kind="ExternalInput").ap()) for nm, shp in A_INPUT_SHAPES.items()}
    io["xT"] = V("dram:xT", nc.dram_tensor("xT", [2048, S], F32, kind="ExternalInput").ap())
    io["yT"] = V("dram:yT", nc.dram_tensor("yT", [512, S], BF16, kind="ExternalOutput").ap())
    c = make_consts(k)
    emit_phase_a(k, c, io, S)
    k.P.build()
    _CACHE[key] = nc
    return nc


B_SHAPES = dict(wout=[2048, 2048], wq=[2048, 512], wkv=[2048, 1024], wo=[512, 2048], wqry=[2048, 1024], KT=[128, 16, 128],
                down=[16384, 2048], up=[16384, 2048], gx=[128, 16], gm=[128, 16], gf=[128, 16], gl=[128, 16], memT=[2048, 256])


def _build_b(NT, last):
    key = ("b", NT, last)
    if key in _CACHE:
        return _CACHE[key]
    nc = bass.Bass("TRN2", target_bir_lowering=False)
    k = K(nc)
    io = {nm: V("dram:" + nm, nc.dram_tensor(nm, list(shp), F32, kind="ExternalInput").ap()) for nm, shp in B_SHAPES.items()}
    io["xT"] = V("dram:xT", nc.dram_tensor("xT", [2048, NT], F32, kind="ExternalInput").ap())
    io["yT"] = V("dram:yT", nc.dram_tensor("yT", [2048, NT], BF16, kind="ExternalInput").ap())
    io["xoT"] = V("dram:xoT", nc.dram_tensor("xoT", [2048, NT], F32, kind="ExternalOutput").ap())
    c = make_consts(k)
    emit_phase_b(k, c, io, NT, last=last)
    k.P.build()
    _CACHE[key] = nc
    return nc


def _gfm(g):
    return np.ascontiguousarray(np.asarray(g, np.float32).reshape(16, 128).T)


def kernel(**inputs):
    inp = {k_: np.asarray(v) for k_, v in inputs.items()}
    x = inp["x"]
    Bsz, S, _ = x.shape
    NT = S // 4
    xT = [np.ascontiguousarray(x[b].T) for b in range(Bsz)]
    memT = [np.ascontiguousarray(inp["mem"][b].T) for b in range(Bsz)]
    perm = np.concatenate([np.concatenate([128 * g + np.arange(128), 512 + 256 * g + np.arange(256), 1536 + 128 * g + np.arange(128)])
                           for g in range(4)])
    depth = inp["w_in"].shape[0]
    for l in range(depth):
        ncA = _build_a(S)
        in_maps = []
        for core in range(8):
            b, g = divmod(core, 4)
            m = phase_a_host_inputs(inp, l, b, g, S)
            m["xT"] = xT[b]
            in_maps.append(m)
        resA = run_bass_kernel_spmd(ncA, in_maps, core_ids=list(range(8))).results
        yT = [np.concatenate([np.asarray(resA[b * 4 + g]["yT"]) for g in range(4)], axis=0) for b in range(Bsz)]
        del resA, in_maps
        last = (l == depth - 1)
        ncB = _build_b(NT, last)
        common = dict(wout=np.ascontiguousarray(inp["w_out"][l][perm]), wq=inp["xattn_w_q"][l], wkv=inp["xattn_w_kv"][l], wo=inp["xattn_w_o"][l],
                      wqry=inp["peer_w_query"][l], KT=make_KT(inp["peer_sub_keys"][l]), down=inp["peer_down"][l], up=inp["peer_up"][l],
                      gx=_gfm(inp["xattn_norm_g"][l]), gm=_gfm(inp["mem_norm_g"][l]), gf=_gfm(inp["ffn_norm_g"][l]), gl=_gfm(inp["final_norm_g"]))
        in_maps = []
        for core in range(8):
            b, j = divmod(core, 4)
            sl = slice(j * NT, (j + 1) * NT)
            m = dict(common)
            m["xT"] = np.ascontiguousarray(xT[b][:, sl])
            m["yT"] = np.ascontiguousarray(yT[b][:, sl])
            m["memT"] = memT[b]
            in_maps.append(m)
        resB = run_bass_kernel_spmd(ncB, in_maps, core_ids=list(range(8))).results
        for core in range(8):
            b, j = divmod(core, 4)
            xT[b][:, j * NT:(j + 1) * NT] = np.asarray(resB[core]["xoT"])
        del resB, in_maps
    out = np.stack([np.ascontiguousarray(xT[b].T) for b in range(Bsz)], axis=0).astype(np.float32)
    return out
```

```python
from contextlib import ExitStack
import concourse.bass as bass
import concourse.mybir as mybir

ENGS = ["tensor", "vector", "scalar", "gpsimd", "sync"]


class Prog:
    def __init__(self, nc, dma_slots=None):
        self.nc = nc
        self.ops = []
        self.stack = ExitStack()
        self.dma_slots = dma_slots or {"sync": 8, "gpsimd": 8, "scalar": 4}
        self._n = 0

    def sb(self, name, shape, dt):
        return self.stack.enter_context(self.nc.sbuf_tensor(name, list(shape), dt))

    def ps(self, name, shape, dt):
        return self.stack.enter_context(self.nc.psum_tensor(name, list(shape), dt))

    def op(self, eng, fn, reads=(), writes=()):
        self.ops.append(("c", eng, fn, tuple(reads), tuple(writes)))

    def dma(self, q, fn, reads=(), writes=()):
        self.ops.append(("d", q, fn, tuple(reads), tuple(writes)))

    def barrier(self):
        self.ops.append(("b", None, None, (), ()))

    def build(self, final_wait_eng="sync"):
        nc = self.nc
        st = self.stack
        esem = {e: st.enter_context(nc.semaphore("s_" + e)) for e in ENGS}
        dsem = {}
        for q, n in self.dma_slots.items():
            dsem[q] = [st.enter_context(nc.semaphore(f"d_{q}{i}")) for i in range(n)]
        ecount = {e: 0 for e in ENGS}
        dcount = {q: [0] * n for q, n in self.dma_slots.items()}
        dnext = {q: 0 for q in self.dma_slots}
        waited = {e: {} for e in ENGS}
        last_w = {}
        readers = {}
        streams = {e: [] for e in ENGS}
        semobj = {}
        all_tokens = []

        def need(eng, tok, waits):
            if tok is None:
                return
            sid, val = tok
            if waited[eng].get(sid, 0) >= val:
                return
            waited[eng][sid] = val
            waits.append(tok)

        pend = {e: {} for e in ENGS}
        latest = {}
        for kind, eng, fn, reads, writes in self.ops:
            if kind == "b":
                for e in ENGS:
                    pend[e] = dict(latest)
                continue
            waits = []
            if pend[eng]:
                for sid_, val_ in pend[eng].items():
                    need(eng, (sid_, val_), waits)
                pend[eng] = {}
            own = id(esem[eng])
            toks = []
            for r in reads:
                toks.append(last_w.get(r))
            for w in writes:
                toks.append(last_w.get(w))
                for t in readers.get(w, {}).items():
                    toks.append(t)
            for t in toks:
                if t is None:
                    continue
                if kind == "c" and eng == "tensor" and t[0] == own:
                    continue
                need(eng, t, waits)
            if kind == "c":
                ecount[eng] += 1
                tok = (id(esem[eng]), ecount[eng])
                semobj[tok[0]] = esem[eng]
                streams[eng].append((waits, fn, esem[eng], 1))
                waited[eng][tok[0]] = max(waited[eng].get(tok[0], 0), 0)
            else:
                slot = dnext[eng]
                dnext[eng] = (slot + 1) % len(dsem[eng])
                s = dsem[eng][slot]
                prev = dcount[eng][slot]
                if prev:
                    need(eng, (id(s), prev), waits)
                dcount[eng][slot] = prev + 16
                tok = (id(s), prev + 16)
                semobj[tok[0]] = s
                streams[eng].append((waits, fn, s, 16))
            all_tokens.append(tok)
            latest[tok[0]] = max(latest.get(tok[0], 0), tok[1])
            for r in reads:
                d = readers.setdefault(r, {})
                d[tok[0]] = max(d.get(tok[0], 0), tok[1])
            for w in writes:
                last_w[w] = tok
                readers[w] = {}

        fin = {}
        for sid, val in all_tokens:
            fin[sid] = max(fin.get(sid, 0), val)
        self.n_instr = {e: len(streams[e]) for e in ENGS}

        with nc.Block() as block:
            def emit(e, name):
                for waits, fn, s, inc in streams[name]:
                    for sid, val in waits:
                        e.wait_ge(semobj[sid], val)
                    fn(e).then_inc(s, inc)
                if name == final_wait_eng:
                    for sid, val in fin.items():
                        e.wait_ge(semobj[sid], val)

            @block.tensor
            def _(e):
                emit(e, "tensor")

            @block.vector
            def _(e):
                emit(e, "vector")

            @block.scalar
            def _(e):
                emit(e, "scalar")

            @block.gpsimd
            def _(e):
                emit(e, "gpsimd")

            @block.sync
            def _(e):
                emit(e, "sync")
        self.stack.close()


import numpy as np

import numpy as np
import concourse.bass as bass
import concourse.mybir as mybir

F32 = mybir.dt.float32
BF16 = mybir.dt.bfloat16
U32 = mybir.dt.uint32
I32 = mybir.dt.int32
AF = mybir.ActivationFunctionType
ALU = mybir.AluOpType
AX = mybir.AxisListType

D = 2048
EPS = 1e-6
NEG = -1e30
STAGE = 99
SUB = 99


class V:
    def __init__(self, res, ap):
        self.res, self.ap = res, ap

    def re(self, fn):
        return V(self.res, fn(self.ap))


class Tl:
    def __init__(self, P, name, shape, dt, psum=False, view=None):
        self.name = name
        if view is not None:
            self.t = view
        else:
            self.t = (P.ps if psum else P.sb)("t_" + name, shape, dt)

    def __getitem__(self, idx):
        return V(self.name, self.t[idx])


_DT_BYTES = {}


def _dt_bytes(dt):
    if dt in (F32, U32, I32):
        return 4
    if dt == BF16:
        return 2
    raise ValueError(dt)


class K:
    def __init__(self, nc):
        self.nc = nc
        self.P = Prog(nc)
        self._u = 0

    def use_arena(self, nbytes):
        self._arena = self.P.sb("arena", [128, nbytes // 2], BF16)
        self._arena_n = nbytes
        self._arena_off = 0

    def arena_mark(self):
        return self._arena_off

    def arena_reset(self, mark):
        self._arena_off = mark

    def tile(self, name, shape, dt, psum=False):
        if not hasattr(self, "_tiles"):
            self._tiles = {}
        if name in self._tiles:
            return self._tiles[name]
        arena = getattr(self, "_arena", None)
        if arena is None or psum:
            t = Tl(self.P, name, shape, dt, psum)
        else:
            nel = 1
            for d_ in shape[1:]:
                nel *= d_
            nb = (nel * _dt_bytes(dt) + 31) // 32 * 32
            off = self._arena_off
            assert off + nb <= self._arena_n, f"arena overflow at {name}: {off}+{nb} > {self._arena_n}"
            self._arena_off = off + nb
            v = arena[0:shape[0], off // 2:(off + nel * _dt_bytes(dt)) // 2]
            if dt != BF16:
                v = v.bitcast(dt)
            if len(shape) > 2:
                names = " ".join(f"d{i}" for i in range(1, len(shape)))
                kw = {f"d{i}": shape[i] for i in range(1, len(shape))}
                v = v.rearrange(f"p ({names}) -> p {names}", **kw)
            t = Tl(self.P, name, shape, dt, view=v)
        self._tiles[name] = t
        return t

    def dram(self, name, shape, dt, kind="Internal"):
        if not hasattr(self, "_drams"):
            self._drams = {}
        if name not in self._drams:
            self._drams[name] = V("dram:" + name, self.nc.dram_tensor(name, list(shape), dt, kind=kind).ap())
        return self._drams[name]

    @staticmethod
    def _r(*xs):
        return [x.res for x in xs if isinstance(x, V)]

    @staticmethod
    def _a(x):
        return x.ap if isinstance(x, V) else x

    def mm(self, out, lhsT, rhs, start=True, stop=True, **kw):
        self.P.op("tensor", lambda e: e.matmul(out.ap, lhsT=lhsT.ap, rhs=rhs.ap, start=start, stop=stop, **kw),
                  reads=self._r(lhsT, rhs), writes=self._r(out))

    def tr(self, out, in_, ident):
        self.P.op("tensor", lambda e: e.transpose(out.ap, in_.ap, ident.ap),
                  reads=self._r(in_, ident), writes=self._r(out))

    def act(self, out, in_, func, bias=None, scale=None, accum=None):
        kw = {}
        if bias is not None:
            kw["bias"] = self._a(bias)
        if scale is not None:
            kw["scale"] = self._a(scale)
        if accum is not None:
            kw["accum_out"] = accum.ap
        self.P.op("scalar", lambda e: e.activation(out=out.ap, in_=in_.ap, func=func, **kw),
                  reads=self._r(in_, bias, scale), writes=self._r(out, accum))

    def tt(self, eng, out, in0, in1, op):
        self.P.op(eng, lambda e: e.tensor_tensor(out=out.ap, in0=in0.ap, in1=in1.ap, op=op),
                  reads=self._r(in0, in1), writes=self._r(out))

    def ts(self, eng, out, in0, s1, s2, op0, op1=None, accum=None):
        kw = {}
        if op1 is not None:
            kw["op1"] = op1
        if accum is not None:
            kw["accum_out"] = accum.ap
        self.P.op(eng, lambda e: e.tensor_scalar(out=out.ap, in0=in0.ap, scalar1=self._a(s1), scalar2=self._a(s2), op0=op0, **kw),
                  reads=self._r(in0, s1, s2), writes=self._r(out, accum))

    def stt(self, out, in0, scalar, in1, op0, op1):
        self.P.op("vector", lambda e: e.scalar_tensor_tensor(out=out.ap, in0=in0.ap, scalar=self._a(scalar), in1=in1.ap, op0=op0, op1=op1),
                  reads=self._r(in0, scalar, in1), writes=self._r(out))

    def copy(self, eng, out, in_):
        if eng == "scalar":
            self.P.op("scalar", lambda e: e.activation(out=out.ap, in_=in_.ap, func=AF.Copy),
                      reads=self._r(in_), writes=self._r(out))
        else:
            self.P.op(eng, lambda e: e.tensor_copy(out=out.ap, in_=in_.ap), reads=self._r(in_), writes=self._r(out))

    def memset(self, eng, out, val):
        self.P.op(eng, lambda e: e.memset(out.ap, val), writes=self._r(out))

    def red(self, out, in_, op, axis=AX.X):
        self.P.op("vector", lambda e: e.tensor_reduce(out=out.ap, in_=in_.ap, axis=axis, op=op),
                  reads=self._r(in_), writes=self._r(out))

    def recip(self, out, in_):
        self.P.op("vector", lambda e: e.reciprocal(out=out.ap, in_=in_.ap), reads=self._r(in_), writes=self._r(out))

    def dma(self, q, out, in_, **kw):
        self.P.dma(q, lambda e: e.dma_start(out=out.ap, in_=in_.ap, **kw), reads=self._r(in_), writes=self._r(out))

    def gen(self, eng, fn, reads=(), writes=()):
        self.P.op(eng, fn, reads=self._r(*reads), writes=self._r(*writes))


def make_consts(k):
    c = {}
    c["ident_f"] = k.tile("ident_f", [128, 128], F32)
    c["ident_b"] = k.tile("ident_b", [128, 128], BF16)
    c["ones_b"] = k.tile("ones_b", [128, 128], BF16)
    c["ones_f"] = k.tile("ones_f", [128, 128], F32)
    c["iota_f"] = k.tile("iota_f", [128, 128], F32)
    c["tri_f"] = k.tile("tri_f", [128, 128], F32)
    c["upp_f"] = k.tile("upp_f", [128, 128], F32)
    k.memset("gpsimd", c["ones_f"][:], 1.0)
    k.memset("gpsimd", c["ones_b"][:], 1.0)
    for nm in ("ident_f", "ident_b"):
        t = c[nm]
        k.memset("gpsimd", t[:], 1.0)
        k.gen("gpsimd", lambda e, t=t: e.affine_select(out=t.t[:], in_=t.t[:], pattern=[[-1, 128]], compare_op=ALU.is_equal,
                                                       fill=0.0, base=0, channel_multiplier=1), reads=[t[:]], writes=[t[:]])
    t = c["tri_f"]
    k.memset("gpsimd", t[:], 1.0)
    k.gen("gpsimd", lambda e, t=t: e.affine_select(out=t.t[:], in_=t.t[:], pattern=[[1, 128]], compare_op=ALU.is_ge,
                                                   fill=0.0, base=0, channel_multiplier=-1), reads=[t[:]], writes=[t[:]])
    t2 = c["upp_f"]
    k.ts("gpsimd", t2[:], t[:], -1.0, 1.0, ALU.mult, ALU.add)
    c["eps"] = k.tile("eps_t", [128, 1], F32)
    k.memset("gpsimd", c["eps"][:], EPS)
    c["pb"] = [k.tile(f"pb{i}", [128, 512], F32, psum=True) for i in range(8)]
    it = c["iota_f"]
    k.gen("gpsimd", lambda e: e.iota(it.t[:], pattern=[[1, 128]], base=0, channel_multiplier=0,
                                     allow_small_or_imprecise_dtypes=True), writes=[it[:]])
    return c


def rms_fm(k, c, xt, g, hn, N, sq, pbank, rs):
    k.act(sq[:, :, 0:N], xt[:, :, 0:N], AF.Square)
    for cc in range(16):
        k.mm(pbank, c["ones_b"][:], sq[:, cc, 0:N], start=(cc == 0), stop=(cc == 15))
    k.act(rs[:, 0:N], pbank, AF.Ln, bias=c["eps"][:, 0:1], scale=1.0 / D)
    k.act(rs[:, 0:N], rs[:, 0:N], AF.Exp, scale=-0.5)
    for cc in range(16):
        k.stt(hn[:, cc, 0:N], xt[:, cc, 0:N], g[:, cc:cc + 1], rs[:, 0:N], ALU.mult, ALU.mult)


class PsumRot:
    def __init__(self, banks):
        self.banks = banks
        self.i = 0

    def next(self):
        b = self.banks[self.i % len(self.banks)]
        self.i += 1
        return b


def linear_fm(k, w_dram, Kc, M, rhs, N, wbufs, prot, evac, q="sync", mblk=256):
    wv = w_dram.re(lambda a: a.rearrange("(kc p) m -> p kc m", p=128))
    bi = 0
    for m0 in range(0, M, mblk):
        mw = min(mblk, M - m0)
        wb = wbufs[bi % len(wbufs)]
        bi += 1
        k.dma(q, wb[:, 0:Kc, 0:mw], wv.re(lambda a: a[:, :, m0:m0 + mw]))
        for mi in range(mw // 128):
            ps = prot.next()
            for kc in range(Kc):
                k.mm(ps[:, 0:N], wb[:, kc, mi * 128:(mi + 1) * 128], rhs[:, kc, 0:N], start=(kc == 0), stop=(kc == Kc - 1))
            evac((m0 // 128) + mi, ps[:, 0:N])


def emit_phase_b(k, c, io, NT, N=256, NIH=2, last=False, lname="L", NE=16384, sel=None, NTS=None):
    nc = k.nc
    NI = 128 // NIH
    pb = c["pb"]
    wout_b = k.dram(lname + "wout_b", [2048, 2048], BF16)
    wq_b = k.dram(lname + "wq_b", [2048, 512], BF16)
    wkv_b = k.dram(lname + "wkv_b", [2048, 1024], BF16)
    wo_b = k.dram(lname + "wo_b", [512, 2048], BF16)
    wqry_b = k.dram(lname + "wqry_b", [2048, 1024], BF16)
    up_b = k.dram(lname + "up_b", [16384, 2048], BF16)
    downT_b = k.dram(lname + "downT_b", [128, 128, 2048], BF16)
    for dst, src, rows in ((wout_b, io["wout"], 2048), (wq_b, io["wq"], 2048), (wkv_b, io["wkv"], 2048),
                           (wo_b, io["wo"], 512), (wqry_b, io["wqry"], 2048), (up_b, io["up"], NE)):
        step = 512
        for r0 in range(0, rows, step):
            k.dma("gpsimd", dst.re(lambda a: a[r0:r0 + step, :]), src.re(lambda a: a[r0:r0 + step, :]))

    xt = k.tile("xt", [128, 16, N], F32)
    yt = k.tile("yt", [128, 16, N], BF16)
    hn = k.tile("hn", [128, 16, N], BF16)
    rs = k.tile("rs", [128, N], F32)
    wbufs = [k.tile(f"wb{i}", [128, 16, 256], BF16) for i in range(2)]
    gx = k.tile("gx", [128, 16], F32)
    gm = k.tile("gm", [128, 16], F32)
    gf = k.tile("gf", [128, 16], F32)
    gl_ = k.tile("gl", [128, 16], F32)
    KT = k.tile("KT", [128, 16, 128], F32)
    kxT = k.tile("kxT", [128, 4, 256], BF16)
    vx = k.tile("vx", [128, 2, 512], BF16)
    qx = k.tile("qx", [128, 4, N], BF16)
    ox = k.tile("ox", [128, 4, N], BF16)
    pT = [k.tile(f"pT{i}", [128, N], BF16) for i in range(2)]
    qp = k.tile("qp", [128, 8, N], F32)
    GG = k.tile("GG", [128, NI, N], BF16)
    dbuf = [k.tile(f"dbuf{i}", [128, 16, 128], BF16) for i in range(4)]
    ubuf = [k.tile(f"ubuf{i}", [128, 1024], BF16) for i in range(4)]
    glb = [k.tile(f"glb{i}", [128, N], BF16) for i in range(4)]
    At = [k.tile(f"At{i}", [128, NI], BF16) for i in range(4)]
    Bt = [k.tile(f"Bt{i}", [128, 128], BF16) for i in range(4)]
    iT = k.tile("iT", [128, N], F32)
    jT = k.tile("jT", [128, N], F32)
    gT = k.tile("gT", [128, N], F32)
    topv = k.tile("topv", [128, 16, 16], F32)
    topi = k.tile("topi", [128, 16, 16], U32)
    topif = k.tile("topif", [128, 16, 16], F32)
    scr = [k.tile(f"scr{i}", [128, 256], F32) for i in range(2)]
    cand = k.tile("cand", [128, 8, 256], F32)
    bv = k.tile("bv", [128, 8, 16], F32)
    bp = k.tile("bp", [128, 8, 16], U32)
    bpi = k.tile("bpi", [128, 8, 16], U32)
    akf = k.tile("akf", [128, 8, 16], F32)
    bkf = k.tile("bkf", [128, 8, 16], F32)
    gsel = k.tile("gsel", [128, 8, 16], F32)
    zs = k.tile("zs", [128, 8], F32)
    isel = k.tile("isel", [128, 128], F32)
    jsel = k.tile("jsel", [128, 128], F32)
    iota16 = k.tile("iota16", [128, 16], F32)
    k.copy("vector", iota16[:], c["iota_f"][:, 0:16])

    k.dma("sync", gx[:], io["gx"])
    k.dma("sync", gm[:], io["gm"])
    k.dma("sync", gf[:], io["gf"])
    if last:
        k.dma("sync", gl_[:], io["gl"])
    k.dma("sync", KT[:], io["KT"])

    assert 16 * N >= 2048
    for i in range(NE // 128):
        ld = V("xt", xt.t[:].rearrange("p a b -> p (a b)")[:, 0:2048])
        k.dma("sync", ld, io["down"].re(lambda a: a[i * 128:(i + 1) * 128, :]))
        tb = V("hn", hn.t[:].rearrange("p a b -> p (a b)")[:, 0:2048])
        for q4 in range(4):
            ps = pb[q4 % 4]
            for j4 in range(4):
                dc = q4 * 4 + j4
                k.tr(ps[:, j4 * 128:(j4 + 1) * 128], ld.re(lambda a: a[:, dc * 128:(dc + 1) * 128]), c["ident_f"][:])
            k.copy("vector" if q4 % 2 == 0 else "scalar", tb.re(lambda a: a[:, q4 * 512:(q4 + 1) * 512]), ps[:, 0:512])
        k.dma("sync", downT_b.re(lambda a: a[i]), tb)

    k.dma("sync", xt[:, :, 0:256], io["memT"].re(lambda a: a.rearrange("(c p) n -> p c n", p=128)))
    rms_fm(k, c, xt, gm, hn, 256, yt, pb[7][:, 0:256], rs)
    prot = PsumRot([pb[0], pb[1], pb[2], pb[3]])
    linear_fm(k, wkv_b.re(lambda a: a[:, 0:512]), 16, 512, hn, 256, wbufs, prot,
              lambda m, ps: k.copy("vector", kxT[:, m, :], ps))
    for half in range(2):
        wb = wbufs[half % 2]
        k.dma("sync", wb[:, :, 0:256], wkv_b.re(lambda a: a.rearrange("(kc p) m -> p kc m", p=128)[:, :, 512 + half * 256:512 + (half + 1) * 256]))
        for mc in range(2):
            ps = prot.next()
            for kc in range(16):
                k.mm(ps[:, 0:256], hn[:, kc, mc * 128:(mc + 1) * 128], wb[:, kc, 0:256], start=(kc == 0), stop=(kc == 15))
            k.copy("vector", vx[:, mc, half * 256:(half + 1) * 256], ps[:, 0:256])

    xTv = io["xT"].re(lambda a: a.rearrange("(c p) n -> p c n", p=128))
    yTv = io["yT"].re(lambda a: a.rearrange("(c p) n -> p c n", p=128))
    oTv = io["xoT"].re(lambda a: a.rearrange("(c p) n -> p c n", p=128))

    for ti in range(NT // N):
        n0 = ti * N
        if sel is None:
            k.dma("sync", xt[:, :, :], xTv.re(lambda a: a[:, :, n0:n0 + N]))
            k.dma("sync", yt[:, :, :], yTv.re(lambda a: a[:, :, n0:n0 + N]))
        else:
            xtmp = V("GG", GG.t[:, 0:32, :].rearrange("p a b -> p (a b)").bitcast(F32).rearrange("p (c n) -> p c n", c=16))
            ytmp = V("GG", GG.t[:, 32:48, :])
            for j in range(4):
                cj = j * NTS + n0
                k.dma("sync", xtmp, xTv.re(lambda a: a[:, :, cj:cj + N]))
                k.dma("sync", ytmp, yTv.re(lambda a: a[:, :, cj:cj + N]))
                if j == 0:
                    k.ts("vector", xt[:, :, :], xtmp, sel[:, 0:1], None, ALU.mult)
                    k.ts("vector", yt[:, :, :], ytmp, sel[:, 0:1], None, ALU.mult)
                else:
                    k.stt(xt[:, :, :], xtmp, sel[:, j:j + 1], xt[:, :, :], ALU.mult, ALU.add)
                    k.stt(yt[:, :, :], ytmp, sel[:, j:j + 1], yt[:, :, :], ALU.mult, ALU.add)
        prot = PsumRot([pb[0], pb[1], pb[2], pb[3]])

        def add_x(m, ps):
            k.tt("vector", xt[:, m, :], ps, xt[:, m, :], ALU.add)
        if STAGE >= 1:
            linear_fm(k, wout_b, 16, 2048, yt, N, wbufs, prot, add_x)
        if STAGE < 2:
            k.dma("sync", oTv.re(lambda a: a[:, :, n0:n0 + N]), xt[:, :, :])
            continue
        rms_fm(k, c, xt, gx, hn, N, yt, pb[7][:, 0:N], rs)
        linear_fm(k, wq_b, 16, 512, hn, N, wbufs, prot,
                  lambda m, ps: k.act(qx[:, m, :], ps, AF.Copy, scale=128 ** -0.5))
        for hd in range(4):
            for mc in range(2):
                ps = pb[4 + mc]
                k.mm(ps[:, 0:N], kxT[:, hd, mc * 128:(mc + 1) * 128], qx[:, hd, :])
                k.act(pT[mc][:, :], ps[:, 0:N], AF.Exp)
            for mc in range(2):
                k.mm(pb[6][:, 0:N], vx[:, mc, hd * 128:(hd + 1) * 128], pT[mc][:, :], start=(mc == 0), stop=(mc == 1))
            for mc in range(2):
                k.mm(pb[7][:, 0:N], c["ones_b"][:], pT[mc][:, :], start=(mc == 0), stop=(mc == 1))
            k.recip(rs[:, 0:N], pb[7][:, 0:N])
            k.tt("vector", ox[:, hd, :], pb[6][:, 0:N], rs[:, 0:N], ALU.mult)
        linear_fm(k, wo_b, 4, 2048, ox, N, wbufs, prot, add_x)
        if STAGE < 3:
            k.dma("sync", oTv.re(lambda a: a[:, :, n0:n0 + N]), xt[:, :, :])
            continue
        rms_fm(k, c, xt, gf, hn, N, yt, pb[7][:, 0:N], rs)
        linear_fm(k, wqry_b, 16, 1024, hn, N, wbufs, prot,
                  lambda m, ps: k.copy("scalar", qp[:, m, :], ps))
        for tc in range(N // 128):
            for hp in range(16):
                h, p = divmod(hp, 2)
                k.mm(pb[4 + hp // 4][:, (hp % 4) * 128:(hp % 4 + 1) * 128],
                     qp[:, h, tc * 128:(tc + 1) * 128], KT[:, hp, :])
            if SUB < 1:
                k.copy("vector", iT[:, tc * 128:(tc + 1) * 128], pb[4][:, 0:128])
                k.copy("vector", jT[:, tc * 128:(tc + 1) * 128], pb[5][:, 0:128])
                k.copy("vector", gT[:, tc * 128:(tc + 1) * 128], pb[7][:, 384:512])
                continue
            for hp in range(16):
                src = pb[4 + hp // 4][:, (hp % 4) * 128:(hp % 4 + 1) * 128]
                s_ = scr[hp % 2]
                k.gen("vector", lambda e, hp=hp, src=src: e.max(out=topv.t[:, hp, 0:8], in_=src.ap), reads=[src], writes=[topv[:]])
                k.gen("vector", lambda e, hp=hp, src=src: e.max_index(out=topi.t[:, hp, 0:8], in_max=topv.t[:, hp, 0:8], in_values=src.ap),
                      reads=[src, topv[:]], writes=[topi[:]])
                k.gen("vector", lambda e, hp=hp, src=src, s_=s_: e.match_replace(out=s_.t[:, 0:128], in_to_replace=topv.t[:, hp, 0:8], in_values=src.ap, imm_value=NEG),
                      reads=[src, topv[:]], writes=[s_[:]])
                k.gen("vector", lambda e, hp=hp, s_=s_: e.max(out=topv.t[:, hp, 8:16], in_=s_.t[:, 0:128]), reads=[s_[:]], writes=[topv[:]])
                k.gen("vector", lambda e, hp=hp, s_=s_: e.max_index(out=topi.t[:, hp, 8:16], in_max=topv.t[:, hp, 8:16], in_values=s_.t[:, 0:128]),
                      reads=[s_[:], topv[:]], writes=[topi[:]])
            if SUB < 2:
                k.copy("vector", iT[:, tc * 128:(tc + 1) * 128], V("topv", topv.t[:].rearrange("p a b -> p (a b)")[:, 0:128]))
                k.copy("vector", jT[:, tc * 128:(tc + 1) * 128], V("topi", topi.t[:].rearrange("p a b -> p (a b)")[:, 0:128]))
                k.copy("vector", gT[:, tc * 128:(tc + 1) * 128], V("topi", topi.t[:].rearrange("p a b -> p (a b)")[:, 128:256]))
                continue
            k.copy("vector", topif[:], topi[:])
            tv4 = topv.t[:].rearrange("p (h two) a -> p h two a", two=2)
            ti4 = topif.t[:].rearrange("p (h two) a -> p h two a", two=2)
            c4 = cand.t[:].rearrange("p h (a b) -> p h a b", a=16)
            k.tt("vector", V("cand", c4), V("topv", tv4[:, :, 0, :].unsqueeze(3).to_broadcast([128, 8, 16, 16])),
                 V("topv", tv4[:, :, 1, :].unsqueeze(2).to_broadcast([128, 8, 16, 16])), ALU.add)
            for h in range(8):
                s_ = scr[h % 2]
                k.gen("vector", lambda e, h=h: e.max(out=bv.t[:, h, 0:8], in_=cand.t[:, h, :]), reads=[cand[:]], writes=[bv[:]])
                k.gen("vector", lambda e, h=h: e.max_index(out=bp.t[:, h, 0:8], in_max=bv.t[:, h, 0:8], in_values=cand.t[:, h, :]),
                      reads=[cand[:], bv[:]], writes=[bp[:]])
                k.gen("vector", lambda e, h=h, s_=s_: e.match_replace(out=s_.t[:, :], in_to_replace=bv.t[:, h, 0:8], in_values=cand.t[:, h, :], imm_value=NEG),
                      reads=[cand[:], bv[:]], writes=[s_[:]])
                k.gen("vector", lambda e, h=h, s_=s_: e.max(out=bv.t[:, h, 8:16], in_=s_.t[:, :]), reads=[s_[:]], writes=[bv[:]])
                k.gen("vector", lambda e, h=h, s_=s_: e.max_index(out=bp.t[:, h, 8:16], in_max=bv.t[:, h, 8:16], in_values=s_.t[:, :]),
                      reads=[s_[:], bv[:]], writes=[bp[:]])
            if SUB < 3:
                k.copy("vector", iT[:, tc * 128:(tc + 1) * 128], V("bv", bv.t[:].rearrange("p a b -> p (a b)")))
                k.copy("vector", jT[:, tc * 128:(tc + 1) * 128], V("bp", bp.t[:].rearrange("p a b -> p (a b)")))
                continue
            k.tt("vector", gsel[:], bv[:], V("bv", bv.t[:, :, 0:1].to_broadcast([128, 8, 16])), ALU.subtract)
            k.act(gsel[:], gsel[:], AF.Exp)
            k.red(zs[:], gsel[:], ALU.add)
            k.recip(zs[:], zs[:])
            k.tt("vector", gsel[:], gsel[:], V("zs", zs.t[:, :].unsqueeze(2).to_broadcast([128, 8, 16])), ALU.mult)
            k.gen("vector", lambda e: e.tensor_single_scalar(out=bpi.t[:], in_=bp.t[:], scalar=4, op=ALU.logical_shift_right), reads=[bp[:]], writes=[bpi[:]])
            k.copy("vector", akf[:], bpi[:])
            k.gen("vector", lambda e: e.tensor_single_scalar(out=bpi.t[:], in_=bp.t[:], scalar=15, op=ALU.bitwise_and), reads=[bp[:]], writes=[bpi[:]])
            k.copy("vector", bkf[:], bpi[:])
            if SUB < 4:
                k.copy("vector", iT[:, tc * 128:(tc + 1) * 128], V("akf", akf.t[:].rearrange("p a b -> p (a b)")))
                k.copy("vector", jT[:, tc * 128:(tc + 1) * 128], V("bkf", bkf.t[:].rearrange("p a b -> p (a b)")))
                k.copy("vector", gT[:, tc * 128:(tc + 1) * 128], V("gsel", gsel.t[:].rearrange("p a b -> p (a b)")))
                continue
            io16 = iota16.t[:, :].unsqueeze(1).unsqueeze(1).to_broadcast([128, 8, 16, 16])
            for (kf, pp, dst) in ((akf, 0, isel), (bkf, 1, jsel)):
                k.tt("vector", V("cand", c4), V(kf.name, kf.t[:].unsqueeze(3).to_broadcast([128, 8, 16, 16])), V("iota16", io16), ALU.is_equal)
                k.tt("vector", V("cand", c4), V("cand", c4), V("topif", ti4[:, :, pp, :].unsqueeze(2).to_broadcast([128, 8, 16, 16])), ALU.mult)
                k.red(dst[:], V("cand", cand.t[:].rearrange("p h (a b) -> p (h a) b", a=16)), ALU.add)
            for (src, dstT) in ((isel[:], iT), (jsel[:], jT), (V("gsel", gsel.t[:].rearrange("p h a -> p (h a)")), gT)):
                k.tr(pb[0][:, 0:128], src, c["ident_f"][:])
                k.copy("vector", dstT[:, tc * 128:(tc + 1) * 128], pb[0][:, 0:128])
        if STAGE < 4:
            k.dma("sync", V("dram:xoT", oTv.ap[:, 0, n0:n0 + N]), iT[:, :])
            k.dma("sync", V("dram:xoT", oTv.ap[:, 1, n0:n0 + N]), jT[:, :])
            k.dma("sync", V("dram:xoT", oTv.ap[:, 2, n0:n0 + N]), gT[:, :])
            continue
        TPE = 512 // NI
        for ih in range(NIH):
            gro = PsumRot([pb[4], pb[5], pb[6], pb[7]])
            for t0 in range(0, N, TPE):
                ps = gro.next()
                for tq in range(TPE):
                    t = t0 + tq
                    a_ = At[t % 4]
                    b_ = Bt[t % 4]
                    k.ts("vector", a_[:, :], c["iota_f"][:, ih * NI:(ih + 1) * NI], iT[:, t:t + 1], gT[:, t:t + 1], ALU.is_equal, ALU.mult)
                    k.ts("vector", b_[:, :], c["iota_f"][:, :], jT[:, t:t + 1], None, ALU.is_equal)
                    k.mm(ps[:, tq * NI:(tq + 1) * NI], b_[:, :], a_[:, :])
                k.copy("vector" if (t0 // TPE) % 2 == 0 else "scalar",
                       V("GG", GG.t[:, :, t0:t0 + TPE].rearrange("p i t -> p t i")),
                       V(ps.name, ps.t[:, 0:TPE * NI].rearrange("p (t i) -> p t i", i=NI)))
            hro = PsumRot([pb[4], pb[5], pb[6], pb[7]])
            for il in range(NI):
                i = ih * NI + il
                db = dbuf[il % 4]
                k.dma("sync" if il % 2 == 0 else "scalar", V(db.name, db.t[:].rearrange("p a b -> p (a b)")), downT_b.re(lambda a: a[i]))
                ps = hro.next()
                for kc in range(16):
                    k.mm(ps[:, 0:N], db[:, kc, :], hn[:, kc, :], start=(kc == 0), stop=(kc == 15))
                g_ = glb[il % 4]
                k.act(g_[:, :], ps[:, 0:N], AF.Gelu)
                k.tt("vector", GG[:, il, :], g_[:, :], GG[:, il, :], ALU.mult)
            for dh in range(2):
                for b4 in range(4):
                    k.memset("vector", pb[b4][:, :], 0.0)
                for il in range(NI):
                    i = ih * NI + il
                    ub = ubuf[il % 4]
                    k.dma("sync" if il % 2 == 0 else "scalar", ub[:, 0:1024], up_b.re(lambda a: a[i * 128:(i + 1) * 128, dh * 1024:(dh + 1) * 1024]))
                    for dc in range(8):
                        k.mm(pb[dc // 2][:, (dc % 2) * 256:(dc % 2) * 256 + N], ub[:, dc * 128:(dc + 1) * 128], GG[:, il, :],
                             start=False, stop=False, skip_group_check=True)
                for dc in range(8):
                    k.tt("vector", xt[:, dh * 8 + dc, :], pb[dc // 2][:, (dc % 2) * 256:(dc % 2) * 256 + N], xt[:, dh * 8 + dc, :], ALU.add)
        if last:
            rms_fm(k, c, xt, gl_, hn, N, yt, pb[7][:, 0:N], rs)
            for cc in range(16):
                k.stt(xt[:, cc, :], xt[:, cc, :], gl_[:, cc:cc + 1], rs[:, 0:N], ALU.mult, ALU.mult)
        k.dma("sync", oTv.re(lambda a: a[:, :, n0:n0 + N]), xt[:, :, :])


def make_KT(subk):
    KT = np.zeros((128, 16, 128), np.float32)
    for h in range(8):
        for p in range(2):
            KT[64 * p:64 * p + 64, 2 * h + p, :] = subk[h, p].T
    return KT


WA = 1800
C_MI, C_MF = 896, 1024
C_V, C_Z, C_MV, C_MO, C_SM = 1152, 1280, 1536, 1664, 1792
PEN = 10000.0
SA = 99
SUBA = 99


def emit_phase_a(k, c, io, S):
    N = 256
    NB = S // 256
    pb = c["pb"]
    T = k.tile
    w_sb = T("w_sb", [128, 16, WA], BF16)
    kT_all = T("kT_all", [128, S], BF16)
    V_all = T("V_all", [128, S // 128, 2, 66], BF16)
    xt = T("xt", [128, 16, N], F32)
    hn = T("hn", [128, 16, N], BF16)
    rs = T("rs", [128, N], F32)
    gmx = T("gmx", [128, 16], F32)
    qz = [T(f"qz{h}", [128, N], BF16) for h in range(2)]
    kmT = T("kmT", [128, 64], BF16)
    kmf = T("kmf", [128, 64], F32)
    ksum = T("ksum", [128, 1], F32)
    gate_sb = [T(f"gate_sb{h}", [128, 64], F32) for h in range(2)]
    cst = [T(f"cst{h}", [128, 64], F32) for h in range(2)]
    gtmp = T("gtmp", [128, 64], F32)
    m8 = T("m8", [128, 8], F32)
    Ttab = T("Ttab", [128, 2, 2, 64], F32)
    ownc = T("ownc", [128, 2, 2], F32)
    skk = T("skk", [128, 2, 256], BF16)
    cbias = T("cbias", [128, 2, 256], BF16)
    onesrow = T("onesrow", [128, 128], BF16)
    onesrow_f = T("onesrow_f", [128, 128], F32)
    Pb = [T(f"Pb{i}", [128, 256], BF16) for i in range(2)]
    PT = [T(f"PT{i}", [128, 2, 128], BF16) for i in range(3)]
    ya_tok = T("ya_tok", [128, 128], F32)
    rden = T("rden", [128, 1], F32)
    yT_tile = T("yT_tile", [128, 4, N], BF16)
    cw = T("cw", [128, 5, 4], F32)
    cb = T("cb", [128, 5], F32)
    hp = T("hp", [128, 16], F32)
    Aneg = T("Aneg", [128, 4], F32)
    nfb = T("nfb", [128, 1], F32)
    gsn = T("gsn", [128, 256], F32)
    gmn = T("gmn", [128, 128], F32)
    wq_m = T("wq_m", [128, 128], F32)
    wk_m = T("wk_m", [128, 128], F32)
    cin = T("cin", [128, 5, 3 + N], F32)
    cacc = T("cacc", [128, N], F32)
    cout = T("cout", [128, 5, N], F32)
    zs = [T(f"zs{i}", [128, 256], F32) for i in range(2)]
    mv_sb = [T(f"mv_sb{i}", [128, 128], F32) for i in range(2)]
    sig = [T(f"sig{i}", [128, 128], F32) for i in range(2)]
    sm_sb = [T(f"sm_sb{i}", [128, 8], F32) for i in range(2)]
    dt4 = T("dt4", [128, 4], F32)
    a4 = T("a4", [128, 4], F32)
    cs4 = T("cs4", [128, 4], F32)
    ecs = T("ecs", [128, 4], F32)
    dte = T("dte", [128, 4], F32)
    cdb = T("cdb", [128, 4], F32)
    xs_sb = T("xs_sb", [128, 256], F32)
    Xdt = T("Xdt", [128, 256], F32)
    Xdte = T("Xdte", [128, 256], F32)
    B_tok = T("B_tok", [128, 128], F32)
    CBm = T("CBm", [128, 128], F32)
    Ah = [T(f"Ah{i}", [128, 128], F32) for i in range(2)]
    Eh = [T(f"Eh{i}", [128, 128], F32) for i in range(2)]
    MT = [T(f"MT{i}", [128, 128], F32) for i in range(2)]
    hstate = T("hstate", [128, 256], F32)
    y1 = T("y1", [128, 256], F32)
    y2 = T("y2", [128, 256], F32)
    ssq = T("ssq", [128, 1], F32)
    ysn = T("ysn", [128, 256], F32)
    qmT = T("qmT", [128, N], F32)
    kmT2 = T("kmT2", [128, N], F32)
    k_tok = T("k_tok", [128, 128], F32)
    KQm = T("KQm", [128, 128], F32)
    vw = T("vw", [128, 130], F32)
    vwe = T("vwe", [128, 130], F32)
    Cn = T("Cn", [128, 130], F32)
    t1 = T("t1", [128, 130], F32)
    t2 = T("t2", [128, 130], F32)
    dd = T("dd", [128, 1], F32)
    hm = T("hm", [128, 128], F32)
    hm2 = T("hm2", [128, 128], F32)
    cols = T("cols", [128, 8], F32)
    spsl = T("spsl", [128, 2], F32)
    NR = 12
    R = T("R", [128, NR, 128], F32)
    rowsb = T("rowsb", [128, 512], F32)
    ms = T("ms", [128, 8], F32)
    R_L1, R_IG, R_BN, R_W, R_CM, R_MX, R_EU, R_IW, R_EMT, R_EW, R_WE, R_Z = range(12)

    wv = io["win"].re(lambda a: a.rearrange("(kc p) m -> p kc m", p=128))
    for q4 in range(4):
        k.dma("gpsimd", w_sb[:, q4 * 4:(q4 + 1) * 4, :], wv.re(lambda a: a[:, q4 * 4:(q4 + 1) * 4, :]))
    for dst, nm in ((gmx, "gmix"), (cw, "cw"), (cb, "cb"), (hp, "hp"), (gsn, "gsn"), (gmn, "gmn"), (wq_m, "wqm"), (wk_m, "wkm"),
                    (Ttab, "Ttab"), (ownc, "ownc")):
        k.dma("sync", dst[:], io[nm])
    k.dma("sync", xt[:, 0:2, :], io["skk"])
    k.dma("sync", xt[:, 2:4, :], io["cbias"])
    k.copy("vector", skk[:], xt[:, 0:2, :])
    k.copy("vector", cbias[:], xt[:, 2:4, :])
    k.memset("vector", onesrow[:], 0.0)
    k.memset("vector", onesrow[0:1, :], 1.0)
    k.memset("vector", onesrow_f[:], 0.0)
    k.memset("vector", onesrow_f[0:1, :], 1.0)
    k.act(Aneg[:], hp[:, 4:8], AF.Exp)
    k.ts("vector", Aneg[:], Aneg[:], -1.0, None, ALU.mult)
    k.ts("vector", nfb[:], hp[:, 13:14], -1.0, None, ALU.mult)
    for h in range(2):
        k.memset("vector", qz[h][:], 0.0)
        k.memset("vector", gate_sb[h][:], NEG)
        k.memset("vector", cst[h][:], 0.0)
    k.memset("vector", kmT[:], 0.0)
    k.memset("vector", kmf[:], 0.0)
    k.memset("gpsimd", V_all[:], 1.0)
    k.memset("vector", cin[:], 0.0)
    k.memset("vector", hstate[:], 0.0)
    k.memset("vector", Cn[:], 0.0)
    k.memset("vector", vw[:], 0.0)
    k.memset("vector", vwe[:], 0.0)
    k.memset("gpsimd", R[:], 0.0)
    k.memset("vector", ms[:], 0.0)
    k.memset("vector", ms[0:1, 0:1], NEG)

    k.memset("vector", yT_tile[:], 0.0)
    xTv = io["xT"].re(lambda a: a.rearrange("(c p) n -> p c n", p=128))
    yTv = io["yT"].re(lambda a: a.rearrange("(c p) n -> p c n", p=128))
    rotA = PsumRot([pb[0], pb[1]])
    one1 = c["ones_f"][:, 0:1]

    for ti in range(S // N):
        t0 = ti * N
        blk = ti
        k.dma("sync", xt[:, :, :], xTv.re(lambda a: a[:, :, t0:t0 + N]))
        if SUBA < 0.1:
            k.dma("sync", yTv.re(lambda a: a[:, :, t0:t0 + N]), yT_tile[:, :, :])
            continue
        rms_fm(k, c, xt, gmx, hn, N, hn, pb[7][:, 0:N], rs)
        for m in range(7 if SUBA >= 1 else (0 if SUBA < 0.3 else (1 if SUBA < 0.5 else 2))):
            ps = rotA.next()
            for kc in range(16):
                k.mm(ps[:, 0:N], w_sb[:, kc, m * 128:(m + 1) * 128], hn[:, kc, :], start=(kc == 0), stop=(kc == 15))
            if m == 0:
                k.act(qz[0][0:64, :], ps[0:64, 0:N], AF.Copy, scale=0.125)
                k.act(qz[1][64:128, :], ps[64:128, 0:N], AF.Copy, scale=0.125)
            elif m == 1:
                k.act(kT_all[:, t0:t0 + N], ps[:, 0:N], AF.Copy, accum=ksum[:])
                k.ts("vector", kmf[:, blk:blk + 1], ksum[:], 1.0 / 256, None, ALU.mult)
                k.copy("vector", kmT[:], kmf[:])
            else:
                j = m - 2
                k.copy("vector" if j % 2 == 0 else "scalar", cin[:, j, 3:3 + N], ps[:, 0:N])
        if SUBA < 2:
            k.dma("sync", yTv.re(lambda a: a[:, :, t0:t0 + N]), yT_tile[:, :, :])
            continue
        for r, col in ((0, C_MI), (1, C_MF)):
            for kc in range(16):
                k.mm(pb[7][:, r * 256:(r + 1) * 256], w_sb[:, kc, col:col + 128], hn[:, kc, :], start=(kc == 0), stop=(kc == 15))
        k.copy("vector", rowsb[0:1, :], pb[7][0:1, 0:512])
        if SUBA < 3:
            k.dma("sync", yTv.re(lambda a: a[:, :, t0:t0 + N]), yT_tile[:, :, :])
            continue
        for ch in range(2):
            c0 = ch * 128
            b1 = rotA.next()
            b2 = rotA.next()
            for kc in range(16):
                k.mm(b1[:, 0:512], hn[:, kc, c0:c0 + 128], w_sb[:, kc, C_V:C_MO], start=(kc == 0), stop=(kc == 15))
            for kc in range(16):
                k.mm(b2[:, 0:136], hn[:, kc, c0:c0 + 128], w_sb[:, kc, C_MO:C_MO + 136], start=(kc == 0), stop=(kc == 15))
            gch = ti * 2 + ch
            k.copy("vector", V("V_all", V_all.t[:, gch, :, 0:64]), V(b1.name, b1.t[:, 0:128].rearrange("p (h d) -> p h d", h=2)))
            k.act(zs[ch][:], b1[:, 128:384], AF.Silu)
            k.copy("vector", mv_sb[ch][:], b1[:, 384:512])
            k.act(sig[ch][:], b2[:, 0:128], AF.Sigmoid)
            k.copy("vector", sm_sb[ch][:], b2[:, 128:136])

        si = 0
        for qc in range(2 if SA >= 1 else 0):
            qs = slice(qc * 128, (qc + 1) * 128)
            for h in range(2):
                if blk > 0:
                    k.mm(pb[7][:, 0:64], qz[h][:, qs], kmT[:, 0:64])
                    k.copy("vector", gate_sb[h][:, 0:blk], pb[7][:, 0:blk])
                    k.gen("vector", lambda e, h=h: e.max(out=m8.t[:, :], in_=gate_sb[h].t[:, :]), reads=[gate_sb[h][:]], writes=[m8[:]])
                    k.ts("vector", gtmp[:, 0:blk], gate_sb[h][:, 0:blk], m8[:, 2:3], PEN, ALU.is_ge, ALU.mult)
                    k.tt("vector", cst[h][:, 0:blk], gtmp[:, 0:blk], Ttab[:, qc, h, NB - 1 - blk:NB - 1], ALU.add)
                nblk = blk + 1
                ug0 = si
                si += nblk

                def st1(n, h=h, qs=qs, qc=qc, ug0=ug0):
                    u = ug0 + n
                    sp_, P_ = pb[2 + u % 2], Pb[u % 2]
                    own = (n == blk)
                    k.mm(sp_[:, 0:256], qz[h][:, qs], kT_all[:, n * 256:(n + 1) * 256], start=True, stop=False)
                    k.mm(sp_[:, 0:256], onesrow[:], skk[:, h, :], start=False, stop=(not own))
                    if own:
                        k.mm(sp_[:, 0:256], c["ident_b"][:], cbias[:, qc, :], start=False, stop=True)
                    bias = ownc[:, qc, h:h + 1] if own else cst[h][:, n:n + 1]
                    k.act(P_[:, :], sp_[:, 0:256], AF.Exp, bias=bias)

                def st2(n, ug0=ug0):
                    u = ug0 + n
                    pt_, P_, PT_ = pb[4 + u % 2], Pb[u % 2], PT[u % 3]
                    ptv = V(pt_.name, pt_.t[:, 0:128].bitcast(BF16).rearrange("p (c q) -> p c q", c=2))
                    for kc in range(2):
                        k.tr(V(pt_.name, ptv.ap[:, kc, :]), P_[:, kc * 128:(kc + 1) * 128], c["ident_b"][:])
                    k.copy("vector", PT_[:, :, :], ptv)

                def st3(n, h=h, ug0=ug0):
                    u = ug0 + n
                    PT_ = PT[u % 3]
                    own = (n == blk)
                    for kc in range(2):
                        k.mm(pb[6][:, 0:66], PT_[:, kc, :], V("V_all", V_all.t[:, n * 2 + kc, h, :]),
                             start=(n == 0 and kc == 0), stop=(own and kc == 1))

                for step in range(nblk + 2):
                    if step < nblk:
                        st1(step)
                    if 1 <= step <= nblk:
                        st2(step - 1)
                    if step >= 2:
                        st3(step - 2)
                k.recip(rden[:], pb[6][:, 64:65])
                k.ts("vector", ya_tok[:, h * 64:(h + 1) * 64], pb[6][:, 0:64], rden[:, 0:1], None, ALU.mult)
            k.tr(pb[7][:, 0:128], ya_tok[:], c["ident_f"][:])
            k.copy("scalar", yT_tile[:, 0, qs], pb[7][:, 0:128])

        if SA < 2:
            k.dma("sync", yTv.re(lambda a: a[:, :, t0:t0 + N]), yT_tile[:, :, :])
            continue
        for j in range(5):
            k.ts("vector", cacc[:], cin[:, j, 0:N], cw[:, j, 0:1], None, ALU.mult)
            for tp in range(1, 4):
                k.stt(cacc[:], cin[:, j, tp:tp + N], cw[:, j, tp:tp + 1], cacc[:], ALU.mult, ALU.add)
            k.act(cout[:, j, :], cacc[:], AF.Silu, bias=cb[:, j:j + 1])
            k.copy("vector", cin[:, j, 0:3], cin[:, j, N:N + 3])
        rot = PsumRot([pb[0], pb[1], pb[2], pb[3], pb[4], pb[5], pb[6]])
        ps = rot.next()
        k.mm(ps[:, 0:N], wq_m[:], cout[:, 4, :])
        k.copy("vector", qmT[:], ps[:, 0:N])
        ps = rot.next()
        k.mm(ps[:, 0:N], wk_m[:], cout[:, 4, :])
        k.act(kmT2[:], ps[:, 0:N], AF.Copy, scale=128 ** -0.5)

        for ch in range(2 if SA >= 3 else 0):
            cs_ = slice(ch * 128, (ch + 1) * 128)
            k.tt("vector", dt4[:], sm_sb[ch][:, 0:4], hp[:, 0:4], ALU.add)
            k.act(dt4[:], dt4[:], AF.Exp)
            k.act(dt4[:], dt4[:], AF.Ln, bias=one1)
            k.tt("vector", a4[:], dt4[:], Aneg[:], ALU.mult)
            k.mm(pb[7][:, 0:4], c["tri_f"][:], a4[:])
            k.mm(pb[7][:, 4:8], c["ones_f"][:], a4[:])
            k.copy("vector", cs4[:], pb[7][:, 0:4])
            k.act(ecs[:], pb[7][:, 0:4], AF.Exp)
            k.tt("vector", dte[:], pb[7][:, 4:8], cs4[:], ALU.subtract)
            k.act(dte[:], dte[:], AF.Exp)
            k.act(cdb[:], pb[7][:, 4:8], AF.Exp)
            pxs = rot.next()
            for j in range(2):
                k.tr(pxs[:, j * 128:(j + 1) * 128], cout[:, j, cs_], c["ident_f"][:])
            k.copy("scalar", xs_sb[:], pxs[:, 0:256])
            for h in range(4):
                hs = slice(h * 64, (h + 1) * 64)
                k.ts("vector", Xdt[:, hs], xs_sb[:, hs], dt4[:, h:h + 1], None, ALU.mult)
                k.ts("vector", Xdte[:, hs], Xdt[:, hs], dte[:, h:h + 1], None, ALU.mult)
            pbt = rot.next()
            k.tr(pbt[:, 0:128], cout[:, 2, cs_], c["ident_f"][:])
            k.copy("scalar", B_tok[:], pbt[:, 0:128])
            pcb = rot.next()
            k.mm(pcb[:, 0:128], cout[:, 2, cs_], cout[:, 3, cs_])
            k.tt("vector", CBm[:], pcb[:, 0:128], c["tri_f"][:], ALU.mult)
            pyd = rot.next()
            for h in range(4):
                A_, E_, M_ = Ah[h % 2], Eh[h % 2], MT[h % 2]
                k.ts("vector", A_[:], c["upp_f"][:], a4[:, h:h + 1], None, ALU.mult)
                pdm = rot.next()
                k.mm(pdm[:, 0:128], A_[:], c["tri_f"][:])
                k.act(E_[:], pdm[:, 0:128], AF.Exp)
                k.tt("vector", M_[:], E_[:], CBm[:], ALU.mult)
                k.mm(pyd[:, h * 64:(h + 1) * 64], M_[:], Xdt[:, h * 64:(h + 1) * 64])
            pyo = rot.next()
            k.mm(pyo[:, 0:256], cout[:, 3, cs_], hstate[:])
            pst = rot.next()
            k.mm(pst[:, 0:256], B_tok[:], Xdte[:])
            for h in range(4):
                hs = slice(h * 64, (h + 1) * 64)
                k.stt(y1[:, hs], xs_sb[:, hs], hp[:, 8 + h:9 + h], pyd[:, hs], ALU.mult, ALU.add)
                k.stt(y2[:, hs], pyo[:, hs], ecs[:, h:h + 1], y1[:, hs], ALU.mult, ALU.add)
                k.stt(hstate[:, hs], hstate[:, hs], cdb[:, h:h + 1], pst[:, hs], ALU.mult, ALU.add)
            k.tt("vector", y2[:], y2[:], zs[ch][:], ALU.mult)
            k.act(y1[:], y2[:], AF.Square, accum=ssq[:])
            k.act(ssq[:], ssq[:], AF.Ln, bias=c["eps"][:, 0:1], scale=1.0 / 256)
            k.act(ssq[:], ssq[:], AF.Exp, scale=-0.5)
            k.stt(ysn[:], y2[:], ssq[:, 0:1], gsn[:], ALU.mult, ALU.mult)
            for j in range(2):
                pt2 = rot.next()
                k.tr(pt2[:, 0:128], ysn[:, j * 128:(j + 1) * 128], c["ident_f"][:])
                k.copy("scalar", yT_tile[:, 1 + j, cs_], pt2[:, 0:128])

            if SA < 4:
                continue
            def row(r):
                return R[0:1, r, :]
            k.act(row(R_L1), rowsb[0:1, 256 + ch * 128:256 + (ch + 1) * 128], AF.Exp, bias=nfb[0:1, 0:1], scale=-1.0)
            k.act(row(R_L1), row(R_L1), AF.Ln, bias=one1.re(lambda a: a[0:1, :]))
            k.ts("vector", row(R_IG), rowsb[0:1, ch * 128:(ch + 1) * 128], hp[0:1, 12:13], None, ALU.add)
            k.gen("vector", lambda e, ch=ch: e.tensor_tensor_scan(out=R.t[0:1, R_BN, :], data0=R.t[0:1, R_L1, :],
                                                                   data1=R.t[0:1, R_Z, :], initial=0.0, op0=ALU.add, op1=ALU.add),
                  reads=[R[:]], writes=[R[:]])
            k.tt("vector", row(R_W), row(R_IG), row(R_BN), ALU.add)
            k.gen("vector", lambda e, ch=ch: e.tensor_tensor_scan(out=R.t[0:1, R_CM, :], data0=R.t[0:1, R_W, :],
                                                                   data1=R.t[0:1, R_W, :], initial=NEG, op0=ALU.max, op1=ALU.max),
                  reads=[R[:]], writes=[R[:]])
            k.ts("vector", row(R_MX), row(R_CM), ms[0:1, 0:1], None, ALU.max)
            k.act(row(R_EU), row(R_MX), AF.Exp, scale=-1.0)
            k.act(row(R_IW), row(R_MX), AF.Exp, scale=-1.0, bias=ms[0:1, 0:1])
            k.tt("vector", row(R_EMT), row(R_BN), row(R_MX), ALU.subtract)
            k.act(row(R_EMT), row(R_EMT), AF.Exp)
            k.act(row(R_EW), row(R_W), AF.Exp)
            cmL = R[0:1, R_CM, 127:128]
            bnL = R[0:1, R_BN, 127:128]
            k.ts("vector", ms[0:1, 1:2], cmL, -1.0, None, ALU.mult)
            k.act(row(R_WE), row(R_W), AF.Exp, bias=ms[0:1, 1:2])
            k.tt("vector", ms[0:1, 2:3], ms[0:1, 0:1], bnL, ALU.subtract)
            k.tt("vector", ms[0:1, 3:4], cmL, bnL, ALU.subtract)
            k.tt("vector", ms[0:1, 4:5], ms[0:1, 2:3], ms[0:1, 3:4], ALU.max)
            k.ts("vector", ms[0:1, 5:6], ms[0:1, 4:5], -1.0, None, ALU.mult)
            k.act(R[0:1, R_Z + 0, 0:0 + 1] if False else ms[0:1, 6:7], ms[0:1, 2:3], AF.Exp, bias=ms[0:1, 5:6])
            k.act(ms[0:1, 7:8], ms[0:1, 3:4], AF.Exp, bias=ms[0:1, 5:6])
            pcl = rot.next()
            for ci, r in enumerate((R_EU, R_IW, R_EMT, R_EW, R_WE)):
                k.mm(pcl[:, 2 * ci:2 * ci + 2], R[:, r, :], c["ones_f"][:, 0:2])
            k.mm(pcl[:, 16:18], onesrow_f[:], ms[:, 6:8])
            k.copy("vector", cols[:, 0:5], V(pcl.name, pcl.t[:, 0:10].rearrange("p (c two) -> p c two", two=2)[:, :, 0]))
            k.copy("vector", spsl[:], pcl[:, 16:18])
            pkt = rot.next()
            k.mm(pkt[:, 0:128], cout[:, 4, cs_], wk_m[:])
            k.act(k_tok[:], pkt[:, 0:128], AF.Copy, scale=128 ** -0.5)
            pkq = rot.next()
            k.mm(pkq[:, 0:128], kmT2[:, cs_], qmT[:, cs_])
            k.tt("vector", KQm[:], pkq[:, 0:128], c["tri_f"][:], ALU.mult)
            k.ts("vector", vw[:, 0:128], mv_sb[ch][:], cols[:, 3:4], None, ALU.mult)
            k.copy("vector", vw[:, 128:129], cols[:, 3:4])
            k.ts("vector", vwe[:, 0:128], mv_sb[ch][:], cols[:, 4:5], None, ALU.mult)
            k.copy("vector", vwe[:, 128:129], cols[:, 4:5])
            pin = rot.next()
            k.mm(pin[:, 0:130], KQm[:], vw[:])
            pit = rot.next()
            k.mm(pit[:, 0:130], qmT[:, cs_], Cn[:])
            k.ts("vector", t1[:], pin[:, 0:130], cols[:, 0:1], None, ALU.mult)
            k.stt(t2[:], pit[:, 0:130], cols[:, 1:2], t1[:], ALU.mult, ALU.add)
            k.ts("vector", dd[:], t2[:, 128:129], -1.0, None, ALU.mult)
            k.tt("vector", dd[:], dd[:], t2[:, 128:129], ALU.max)
            k.tt("vector", dd[:], dd[:], cols[:, 2:3], ALU.max)
            k.recip(dd[:], dd[:])
            k.ts("vector", hm[:], t2[:, 0:128], dd[:, 0:1], None, ALU.mult)
            k.act(hm2[:], hm[:], AF.Square, accum=ssq[:])
            k.act(ssq[:], ssq[:], AF.Ln, bias=c["eps"][:, 0:1], scale=1.0 / 128)
            k.act(ssq[:], ssq[:], AF.Exp, scale=-0.5)
            k.stt(hm2[:], hm[:], ssq[:, 0:1], gmn[:], ALU.mult, ALU.mult)
            k.tt("vector", hm2[:], hm2[:], sig[ch][:], ALU.mult)
            pt3 = rot.next()
            k.tr(pt3[:, 0:128], hm2[:], c["ident_f"][:])
            k.copy("scalar", yT_tile[:, 3, cs_], pt3[:, 0:128])
            pcl2 = rot.next()
            k.mm(pcl2[:, 0:130], k_tok[:], vwe[:])
            k.ts("vector", t1[:], pcl2[:, 0:130], spsl[:, 1:2], None, ALU.mult)
            k.stt(Cn[:], Cn[:], spsl[:, 0:1], t1[:], ALU.mult, ALU.add)
            k.copy("vector", ms[0:1, 0:1], ms[0:1, 4:5])
        k.dma("sync", yTv.re(lambda a: a[:, :, t0:t0 + N]), yT_tile[:, :, :])


def phase_a_host_inputs(inp, l, b, g, S):
    w_in = inp["w_in"][l]
    o = np.cumsum([0, 512, 512, 512, 1024, 2048, 16, 512, 512, 512, 4, 4])
    oq, ok, ov, oz, oxbc, odt, omu, omv, omo, omi, omf = o[:11]
    win = np.zeros((2048, WA), np.float32)
    win[:, 0:128] = w_in[:, oq + 128 * g: oq + 128 * (g + 1)]
    win[:, 128:256] = w_in[:, ok + 128 * g: ok + 128 * (g + 1)]
    win[:, 256:512] = w_in[:, oxbc + 256 * g: oxbc + 256 * (g + 1)]
    win[:, 512:640] = w_in[:, oxbc + 1024 + 128 * g: oxbc + 1024 + 128 * (g + 1)]
    win[:, 640:768] = w_in[:, oxbc + 1536 + 128 * g: oxbc + 1536 + 128 * (g + 1)]
    win[:, 768:896] = w_in[:, omu + 128 * g: omu + 128 * (g + 1)]
    win[:, C_V:C_V + 128] = w_in[:, ov + 128 * g: ov + 128 * (g + 1)]
    win[:, C_Z:C_Z + 256] = w_in[:, oz + 256 * g: oz + 256 * (g + 1)]
    win[:, C_MV:C_MV + 128] = w_in[:, omv + 128 * g: omv + 128 * (g + 1)]
    win[:, C_MO:C_MO + 128] = w_in[:, omo + 128 * g: omo + 128 * (g + 1)]
    win[:, C_SM:C_SM + 4] = w_in[:, odt + 4 * g: odt + 4 * (g + 1)]
    win[:, C_MI] = w_in[:, omi + g]
    win[:, C_MF] = w_in[:, omf + g]
    cw = np.zeros((128, 5, 4), np.float32)
    cb = np.zeros((128, 5), np.float32)
    scw, scb = inp["ssm_conv_w"][l], inp["ssm_conv_b"][l]
    chans = [np.arange(256 * g, 256 * g + 128), np.arange(256 * g + 128, 256 * g + 256),
             np.arange(1024 + 128 * g, 1024 + 128 * (g + 1)), np.arange(1536 + 128 * g, 1536 + 128 * (g + 1))]
    for j, ch in enumerate(chans):
        cw[:, j, :] = scw[:, ch].T
        cb[:, j] = scb[ch]
    cw[:, 4, :] = inp["mlstm_conv_w"][l][:, 128 * g:128 * (g + 1)].T
    cb[:, 4] = inp["mlstm_conv_b"][l][128 * g:128 * (g + 1)]
    hp = np.zeros((128, 16), np.float32)
    hp[:, 0:4] = inp["ssm_dt_bias"][l][4 * g:4 * g + 4]
    hp[:, 4:8] = inp["ssm_A_log"][l][4 * g:4 * g + 4]
    hp[:, 8:12] = inp["ssm_D"][l][4 * g:4 * g + 4]
    hp[:, 12] = inp["mlstm_i_bias"][l][g]
    hp[:, 13] = inp["mlstm_f_bias"][l][g]
    gsn = np.broadcast_to(inp["ssm_norm_g"][l][256 * g:256 * (g + 1)], (128, 256)).copy()
    gmn = np.broadcast_to(inp["mlstm_norm_g"][l][128 * g:128 * (g + 1)], (128, 128)).copy()
    NB = S // 256
    slopes = 2.0 ** (-8.0 * (np.arange(1, 9)) / 8)
    Ttab = np.zeros((128, 2, 2, 64), np.float32)
    ownc = np.zeros((128, 2, 2), np.float32)
    skk = np.zeros((128, 2, 256), np.float32)
    cbias = np.zeros((128, 2, 256), np.float32)
    tl = np.arange(128)
    for par in range(2):
        qq = tl + 128 * par
        for hh in range(2):
            sl = slopes[2 * g + hh]
            for m in range(NB):
                Ttab[:, par, hh, m] = -PEN - sl * (qq + 256.0 * (NB - 1 - m))
            ownc[:, par, hh] = -sl * qq
        cbias[:, par, :] = np.where(np.arange(256)[None, :] <= qq[:, None], 0.0, -PEN)
    for hh in range(2):
        skk[0, hh, :] = slopes[2 * g + hh] * np.arange(256)
    gm = inp["mix_norm_g"][l]
    return dict(win=win, gmix=np.ascontiguousarray(gm.reshape(16, 128).T), cw=cw, cb=cb, hp=hp, gsn=gsn, gmn=gmn,
                wqm=np.ascontiguousarray(inp["mlstm_wq"][l][g]), wkm=np.ascontiguousarray(inp["mlstm_wk"][l][g]),
                Ttab=Ttab, ownc=ownc, skk=skk, cbias=cbias)


A_INPUT_SHAPES = dict(win=[2048, WA], gmix=[128, 16], cw=[128, 5, 4], cb=[128, 5], hp=[128, 16], gsn=[128, 256], gmn=[128, 128],
                      wqm=[128, 128], wkm=[128, 128], Ttab=[128, 2, 2, 64], ownc=[128, 2, 2], skk=[128, 2, 256], cbias=[128, 2, 256])


from concourse.bass_utils import run_bass_kernel_spmd

SEQ = 16384
_CACHE = {}


def _build_a(S):
    key = ("a", S)
    if key in _CACHE:
        return _CACHE[key]
    nc = bass.Bass("TRN2", target_bir_lowering=False)
    k = K(nc)
    io = {nm: V("dram:" + nm, nc.dram_tensor(nm, list(shp), F32, kind="ExternalInput").ap()) for nm, shp in A_INPUT_SHAPES.items()}
    io["xT"] = V("dram:xT", nc.dram_tensor("xT", [2048, S], F32, kind="ExternalInput").ap())
    io["yT"] = V("dram:yT", nc.dram_tensor("yT", [512, S], BF16, kind="ExternalOutput").ap())
    c = make_consts(k)
    emit_phase_a(k, c, io, S)
    k.P.build()
    _CACHE[key] = nc
    return nc


B_SHAPES = dict(wout=[2048, 2048], wq=[2048, 512], wkv=[2048, 1024], wo=[512, 2048], wqry=[2048, 1024], KT=[128, 16, 128],
                down=[16384, 2048], up=[16384, 2048], gx=[128, 16], gm=[128, 16], gf=[128, 16], gl=[128, 16], memT=[2048, 256])


def _build_b(NT, last):
    key = ("b", NT, last)
    if key in _CACHE:
        return _CACHE[key]
    nc = bass.Bass("TRN2", target_bir_lowering=False)
    k = K(nc)
    io = {nm: V("dram:" + nm, nc.dram_tensor(nm, list(shp), F32, kind="ExternalInput").ap()) for nm, shp in B_SHAPES.items()}
    io["xT"] = V("dram:xT", nc.dram_tensor("xT", [2048, NT], F32, kind="ExternalInput").ap())
    io["yT"] = V("dram:yT", nc.dram_tensor("yT", [2048, NT], BF16, kind="ExternalInput").ap())
    io["xoT"] = V("dram:xoT", nc.dram_tensor("xoT", [2048, NT], F32, kind="ExternalOutput").ap())
    c = make_consts(k)
    emit_phase_b(k, c, io, NT, last=last)
    k.P.build()
    _CACHE[key] = nc
    return nc


def _gfm(g):
    return np.ascontiguousarray(np.asarray(g, np.float32).reshape(16, 128).T)


def kernel(**inputs):
    inp = {k_: np.asarray(v) for k_, v in inputs.items()}
    x = inp["x"]
    Bsz, S, _ = x.shape
    NT = S // 4
    xT = [np.ascontiguousarray(x[b].T) for b in range(Bsz)]
    memT = [np.ascontiguousarray(inp["mem"][b].T) for b in range(Bsz)]
    perm = np.concatenate([np.concatenate([128 * g + np.arange(128), 512 + 256 * g + np.arange(256), 1536 + 128 * g + np.arange(128)])
                           for g in range(4)])
    depth = inp["w_in"].shape[0]
    for l in range(depth):
        ncA = _build_a(S)
        in_maps = []
        for core in range(8):
            b, g = divmod(core, 4)
            m = phase_a_host_inputs(inp, l, b, g, S)
            m["xT"] = xT[b]
            in_maps.append(m)
        resA = run_bass_kernel_spmd(ncA, in_maps, core_ids=list(range(8))).results
        yT = [np.concatenate([np.asarray(resA[b * 4 + g]["yT"]) for g in range(4)], axis=0) for b in range(Bsz)]
        del resA, in_maps
        last = (l == depth - 1)
        ncB = _build_b(NT, last)
        common = dict(wout=np.ascontiguousarray(inp["w_out"][l][perm]), wq=inp["xattn_w_q"][l], wkv=inp["xattn_w_kv"][l], wo=inp["xattn_w_o"][l],
                      wqry=inp["peer_w_query"][l], KT=make_KT(inp["peer_sub_keys"][l]), down=inp["peer_down"][l], up=inp["peer_up"][l],
                      gx=_gfm(inp["xattn_norm_g"][l]), gm=_gfm(inp["mem_norm_g"][l]), gf=_gfm(inp["ffn_norm_g"][l]), gl=_gfm(inp["final_norm_g"]))
        in_maps = []
        for core in range(8):
            b, j = divmod(core, 4)
            sl = slice(j * NT, (j + 1) * NT)
            m = dict(common)
            m["xT"] = np.ascontiguousarray(xT[b][:, sl])
            m["yT"] = np.ascontiguousarray(yT[b][:, sl])
            m["memT"] = memT[b]
            in_maps.append(m)
        resB = run_bass_kernel_spmd(ncB, in_maps, core_ids=list(range(8))).results
        for core in range(8):
            b, j = divmod(core, 4)
            xT[b][:, j * NT:(j + 1) * NT] = np.asarray(resB[core]["xoT"])
        del resB, in_maps
    out = np.stack([np.ascontiguousarray(xT[b].T) for b in range(Bsz)], axis=0).astype(np.float32)
    return out
```

```python
from contextlib import ExitStack
import concourse.bass as bass
import concourse.mybir as mybir

ENGS = ["tensor", "vector", "scalar", "gpsimd", "sync"]


class Prog:
    def __init__(self, nc, dma_slots=None):
        self.nc = nc
        self.ops = []
        self.stack = ExitStack()
        self.dma_slots = dma_slots or {"sync": 8, "gpsimd": 8, "scalar": 8}
        self._n = 0

    def sb(self, name, shape, dt):
        return self.stack.enter_context(self.nc.sbuf_tensor(name, list(shape), dt))

    def ps(self, name, shape, dt):
        return self.stack.enter_context(self.nc.psum_tensor(name, list(shape), dt))

    def op(self, eng, fn, reads=(), writes=()):
        self.ops.append(("c", eng, fn, tuple(reads), tuple(writes)))

    def dma(self, q, fn, reads=(), writes=()):
        self.ops.append(("d", q, fn, tuple(reads), tuple(writes)))

    def barrier(self):
        self.ops.append(("b", None, None, (), ()))

    def build(self, final_wait_eng="sync"):
        nc = self.nc
        st = self.stack
        esem = {e: st.enter_context(nc.semaphore("s_" + e)) for e in ENGS}
        dsem = {}
        for q, n in self.dma_slots.items():
            dsem[q] = [st.enter_context(nc.semaphore(f"d_{q}{i}")) for i in range(n)]
        ecount = {e: 0 for e in ENGS}
        dcount = {q: [0] * n for q, n in self.dma_slots.items()}
        dnext = {q: 0 for q in self.dma_slots}
        waited = {e: {} for e in ENGS}
        last_w = {}
        readers = {}
        streams = {e: [] for e in ENGS}
        semobj = {}
        all_tokens = []

        def need(eng, tok, waits):
            if tok is None:
                return
            sid, val = tok
            if waited[eng].get(sid, 0) >= val:
                return
            waited[eng][sid] = val
            waits.append(tok)

        pend = {e: {} for e in ENGS}
        latest = {}
        for kind, eng, fn, reads, writes in self.ops:
            if kind == "b":
                for e in ENGS:
                    pend[e] = dict(latest)
                continue
            waits = []
            if pend[eng]:
                for sid_, val_ in pend[eng].items():
                    need(eng, (sid_, val_), waits)
                pend[eng] = {}
            own = id(esem[eng])
            toks = []
            for r in reads:
                toks.append(last_w.get(r))
            for w in writes:
                toks.append(last_w.get(w))
                for t in readers.get(w, {}).items():
                    toks.append(t)
            for t in toks:
                if t is None:
                    continue
                if kind == "c" and eng == "tensor" and t[0] == own:
                    continue
                need(eng, t, waits)
            if kind == "c":
                ecount[eng] += 1
                tok = (id(esem[eng]), ecount[eng])
                semobj[tok[0]] = esem[eng]
                streams[eng].append((waits, fn, esem[eng], 1))
                waited[eng][tok[0]] = max(waited[eng].get(tok[0], 0), 0)
            else:
                slot = dnext[eng]
                dnext[eng] = (slot + 1) % len(dsem[eng])
                s = dsem[eng][slot]
                prev = dcount[eng][slot]
                if prev:
                    need(eng, (id(s), prev), waits)
                dcount[eng][slot] = prev + 16
                tok = (id(s), prev + 16)
                semobj[tok[0]] = s
                streams[eng].append((waits, fn, s, 16))
            all_tokens.append(tok)
            latest[tok[0]] = max(latest.get(tok[0], 0), tok[1])
            for r in reads:
                d = readers.setdefault(r, {})
                d[tok[0]] = max(d.get(tok[0], 0), tok[1])
            for w in writes:
                last_w[w] = tok
                readers[w] = {}

        fin = {}
        for sid, val in all_tokens:
            fin[sid] = max(fin.get(sid, 0), val)
        self.n_instr = {e: len(streams[e]) for e in ENGS}

        with nc.Block() as block:
            def emit(e, name):
                for waits, fn, s, inc in streams[name]:
                    for sid, val in waits:
                        e.wait_ge(semobj[sid], val)
                    fn(e).then_inc(s, inc)
                if name == final_wait_eng:
                    for sid, val in fin.items():
                        e.wait_ge(semobj[sid], val)

            @block.tensor
            def _(e):
                emit(e, "tensor")

            @block.vector
            def _(e):
                emit(e, "vector")

            @block.scalar
            def _(e):
                emit(e, "scalar")

            @block.gpsimd
            def _(e):
                emit(e, "gpsimd")

            @block.sync
            def _(e):
                emit(e, "sync")
        self.stack.close()


import numpy as np

import numpy as np
import concourse.bass as bass
import concourse.mybir as mybir

F32 = mybir.dt.float32
BF16 = mybir.dt.bfloat16
U32 = mybir.dt.uint32
I32 = mybir.dt.int32
AF = mybir.ActivationFunctionType
ALU = mybir.AluOpType
AX = mybir.AxisListType

D = 2048
EPS = 1e-6
NEG = -1e30
STAGE = 99
SUB = 99


class V:
    def __init__(self, res, ap):
        self.res, self.ap = res, ap

    def re(self, fn):
        return V(self.res, fn(self.ap))


class Tl:
    def __init__(self, P, name, shape, dt, psum=False, view=None):
        self.name = name
        if view is not None:
            self.t = view
        else:
            self.t = (P.ps if psum else P.sb)("t_" + name, shape, dt)

    def __getitem__(self, idx):
        return V(self.name, self.t[idx])


_DT_BYTES = {}


def _dt_bytes(dt):
    if dt in (F32, U32, I32):
        return 4
    if dt == BF16:
        return 2
    raise ValueError(dt)


class K:
    def __init__(self, nc):
        self.nc = nc
        self.P = Prog(nc)
        self._u = 0

    def use_arena(self, nbytes):
        self._arena = self.P.sb("arena", [128, nbytes // 2], BF16)
        self._arena_n = nbytes
        self._arena_off = 0

    def arena_mark(self):
        return self._arena_off

    def arena_reset(self, mark):
        self._arena_off = mark

    def tile(self, name, shape, dt, psum=False):
        if not hasattr(self, "_tiles"):
            self._tiles = {}
        if name in self._tiles:
            return self._tiles[name]
        arena = getattr(self, "_arena", None)
        if arena is None or psum:
            t = Tl(self.P, name, shape, dt, psum)
        else:
            nel = 1
            for d_ in shape[1:]:
                nel *= d_
            nb = (nel * _dt_bytes(dt) + 31) // 32 * 32
            off = self._arena_off
            assert off + nb <= self._arena_n, f"arena overflow at {name}: {off}+{nb} > {self._arena_n}"
            self._arena_off = off + nb
            v = arena[0:shape[0], off // 2:(off + nel * _dt_bytes(dt)) // 2]
            if dt != BF16:
                v = v.bitcast(dt)
            if len(shape) > 2:
                names = " ".join(f"d{i}" for i in range(1, len(shape)))
                kw = {f"d{i}": shape[i] for i in range(1, len(shape))}
                v = v.rearrange(f"p ({names}) -> p {names}", **kw)
            t = Tl(self.P, name, shape, dt, view=v)
        self._tiles[name] = t
        return t

    def dram(self, name, shape, dt, kind="Internal"):
        if not hasattr(self, "_drams"):
            self._drams = {}
        if name not in self._drams:
            self._drams[name] = V("dram:" + name, self.nc.dram_tensor(name, list(shape), dt, kind=kind).ap())
        return self._drams[name]

    @staticmethod
    def _r(*xs):
        return [x.res for x in xs if isinstance(x, V)]

    @staticmethod
    def _a(x):
        return x.ap if isinstance(x, V) else x

    def mm(self, out, lhsT, rhs, start=True, stop=True, **kw):
        self.P.op("tensor", lambda e: e.matmul(out.ap, lhsT=lhsT.ap, rhs=rhs.ap, start=start, stop=stop, **kw),
                  reads=self._r(lhsT, rhs), writes=self._r(out))

    def tr(self, out, in_, ident):
        self.P.op("tensor", lambda e: e.transpose(out.ap, in_.ap, ident.ap),
                  reads=self._r(in_, ident), writes=self._r(out))

    def act(self, out, in_, func, bias=None, scale=None, accum=None):
        kw = {}
        if bias is not None:
            kw["bias"] = self._a(bias)
        if scale is not None:
            kw["scale"] = self._a(scale)
        if accum is not None:
            kw["accum_out"] = accum.ap
        self.P.op("scalar", lambda e: e.activation(out=out.ap, in_=in_.ap, func=func, **kw),
                  reads=self._r(in_, bias, scale), writes=self._r(out, accum))

    def tt(self, eng, out, in0, in1, op):
        self.P.op(eng, lambda e: e.tensor_tensor(out=out.ap, in0=in0.ap, in1=in1.ap, op=op),
                  reads=self._r(in0, in1), writes=self._r(out))

    def ts(self, eng, out, in0, s1, s2, op0, op1=None, accum=None):
        kw = {}
        if op1 is not None:
            kw["op1"] = op1
        if accum is not None:
            kw["accum_out"] = accum.ap
        self.P.op(eng, lambda e: e.tensor_scalar(out=out.ap, in0=in0.ap, scalar1=self._a(s1), scalar2=self._a(s2), op0=op0, **kw),
                  reads=self._r(in0, s1, s2), writes=self._r(out, accum))

    def stt(self, out, in0, scalar, in1, op0, op1):
        self.P.op("vector", lambda e: e.scalar_tensor_tensor(out=out.ap, in0=in0.ap, scalar=self._a(scalar), in1=in1.ap, op0=op0, op1=op1),
                  reads=self._r(in0, scalar, in1), writes=self._r(out))

    def copy(self, eng, out, in_):
        if eng == "scalar":
            self.P.op("scalar", lambda e: e.activation(out=out.ap, in_=in_.ap, func=AF.Copy),
                      reads=self._r(in_), writes=self._r(out))
        else:
            self.P.op(eng, lambda e: e.tensor_copy(out=out.ap, in_=in_.ap), reads=self._r(in_), writes=self._r(out))

    def memset(self, eng, out, val):
        self.P.op(eng, lambda e: e.memset(out.ap, val), writes=self._r(out))

    def red(self, out, in_, op, axis=AX.X):
        self.P.op("vector", lambda e: e.tensor_reduce(out=out.ap, in_=in_.ap, axis=axis, op=op),
                  reads=self._r(in_), writes=self._r(out))

    def recip(self, out, in_):
        self.P.op("vector", lambda e: e.reciprocal(out=out.ap, in_=in_.ap), reads=self._r(in_), writes=self._r(out))

    def dma(self, q, out, in_, **kw):
        self.P.dma(q, lambda e: e.dma_start(out=out.ap, in_=in_.ap, **kw), reads=self._r(in_), writes=self._r(out))

    def gen(self, eng, fn, reads=(), writes=()):
        self.P.op(eng, fn, reads=self._r(*reads), writes=self._r(*writes))


def make_consts(k):
    c = {}
    c["ident_f"] = k.tile("ident_f", [128, 128], F32)
    c["ident_b"] = k.tile("ident_b", [128, 128], BF16)
    c["ones_b"] = k.tile("ones_b", [128, 128], BF16)
    c["ones_f"] = k.tile("ones_f", [128, 128], F32)
    c["iota_f"] = k.tile("iota_f", [128, 128], F32)
    c["tri_f"] = k.tile("tri_f", [128, 128], F32)
    c["upp_f"] = k.tile("upp_f", [128, 128], F32)
    k.memset("gpsimd", c["ones_f"][:], 1.0)
    k.memset("gpsimd", c["ones_b"][:], 1.0)
    for nm in ("ident_f", "ident_b"):
        t = c[nm]
        k.memset("gpsimd", t[:], 1.0)
        k.gen("gpsimd", lambda e, t=t: e.affine_select(out=t.t[:], in_=t.t[:], pattern=[[-1, 128]], compare_op=ALU.is_equal,
                                                       fill=0.0, base=0, channel_multiplier=1), reads=[t[:]], writes=[t[:]])
    t = c["tri_f"]
    k.memset("gpsimd", t[:], 1.0)
    k.gen("gpsimd", lambda e, t=t: e.affine_select(out=t.t[:], in_=t.t[:], pattern=[[1, 128]], compare_op=ALU.is_ge,
                                                   fill=0.0, base=0, channel_multiplier=-1), reads=[t[:]], writes=[t[:]])
    t2 = c["upp_f"]
    k.ts("gpsimd", t2[:], t[:], -1.0, 1.0, ALU.mult, ALU.add)
    c["eps"] = k.tile("eps_t", [128, 1], F32)
    k.memset("gpsimd", c["eps"][:], EPS)
    c["pb"] = [k.tile(f"pb{i}", [128, 512], F32, psum=True) for i in range(8)]
    it = c["iota_f"]
    k.gen("gpsimd", lambda e: e.iota(it.t[:], pattern=[[1, 128]], base=0, channel_multiplier=0,
                                     allow_small_or_imprecise_dtypes=True), writes=[it[:]])
    return c


def rms_fm(k, c, xt, g, hn, N, sq, pbank, rs):
    k.act(sq[:, :, 0:N], xt[:, :, 0:N], AF.Square)
    for cc in range(16):
        k.mm(pbank, c["ones_b"][:], sq[:, cc, 0:N], start=(cc == 0), stop=(cc == 15))
    k.act(rs[:, 0:N], pbank, AF.Ln, bias=c["eps"][:, 0:1], scale=1.0 / D)
    k.act(rs[:, 0:N], rs[:, 0:N], AF.Exp, scale=-0.5)
    for cc in range(16):
        k.stt(hn[:, cc, 0:N], xt[:, cc, 0:N], g[:, cc:cc + 1], rs[:, 0:N], ALU.mult, ALU.mult)


class PsumRot:
    def __init__(self, banks):
        self.banks = banks
        self.i = 0

    def next(self):
        b = self.banks[self.i % len(self.banks)]
        self.i += 1
        return b


def linear_fm(k, w_dram, Kc, M, rhs, N, wbufs, prot, evac, q="sync", mblk=256):
    wv = w_dram.re(lambda a: a.rearrange("(kc p) m -> p kc m", p=128))
    bi = 0
    for m0 in range(0, M, mblk):
        mw = min(mblk, M - m0)
        wb = wbufs[bi % len(wbufs)]
        bi += 1
        k.dma(q, wb[:, 0:Kc, 0:mw], wv.re(lambda a: a[:, :, m0:m0 + mw]))
        for mi in range(mw // 128):
            ps = prot.next()
            for kc in range(Kc):
                k.mm(ps[:, 0:N], wb[:, kc, mi * 128:(mi + 1) * 128], rhs[:, kc, 0:N], start=(kc == 0), stop=(kc == Kc - 1))
            evac((m0 // 128) + mi, ps[:, 0:N])


def emit_phase_b(k, c, io, NT, N=256, NIH=2, last=False, lname="L", NE=16384, sel=None, NTS=None):
    nc = k.nc
    NI = 128 // NIH
    pb = c["pb"]
    wout_b = k.dram(lname + "wout_b", [2048, 2048], BF16)
    wq_b = k.dram(lname + "wq_b", [2048, 512], BF16)
    wkv_b = k.dram(lname + "wkv_b", [2048, 1024], BF16)
    wo_b = k.dram(lname + "wo_b", [512, 2048], BF16)
    wqry_b = k.dram(lname + "wqry_b", [2048, 1024], BF16)
    up_b = k.dram(lname + "up_b", [16384, 2048], BF16)
    downT_b = k.dram(lname + "downT_b", [128, 128, 2048], BF16)
    for dst, src, rows in ((wout_b, io["wout"], 2048), (wq_b, io["wq"], 2048), (wkv_b, io["wkv"], 2048),
                           (wo_b, io["wo"], 512), (wqry_b, io["wqry"], 2048), (up_b, io["up"], NE)):
        step = 512
        for r0 in range(0, rows, step):
            k.dma("gpsimd", dst.re(lambda a: a[r0:r0 + step, :]), src.re(lambda a: a[r0:r0 + step, :]))

    xt = k.tile("xt", [128, 16, N], F32)
    yt = k.tile("yt", [128, 16, N], BF16)
    hn = k.tile("hn", [128, 16, N], BF16)
    rs = k.tile("rs", [128, N], F32)
    wbufs = [k.tile(f"wb{i}", [128, 16, 256], BF16) for i in range(2)]
    gx = k.tile("gx", [128, 16], F32)
    gm = k.tile("gm", [128, 16], F32)
    gf = k.tile("gf", [128, 16], F32)
    gl_ = k.tile("gl", [128, 16], F32)
    KT = k.tile("KT", [128, 16, 128], F32)
    kxT = k.tile("kxT", [128, 4, 256], BF16)
    vx = k.tile("vx", [128, 2, 512], BF16)
    qx = k.tile("qx", [128, 4, N], BF16)
    ox = k.tile("ox", [128, 4, N], BF16)
    pT = [k.tile(f"pT{i}", [128, N], BF16) for i in range(2)]
    qp = k.tile("qp", [128, 8, N], F32)
    GG = k.tile("GG", [128, NI, N], BF16)
    dbuf = [k.tile(f"dbuf{i}", [128, 16, 128], BF16) for i in range(6)]
    ubuf = [k.tile(f"ubuf{i}", [128, 1024], BF16) for i in range(6)]
    glb = [k.tile(f"glb{i}", [128, N], BF16) for i in range(4)]
    At = [k.tile(f"At{i}", [128, NI], BF16) for i in range(8)]
    Bt = [k.tile(f"Bt{i}", [128, 128], BF16) for i in range(8)]
    iT = k.tile("iT", [128, N], F32)
    jT = k.tile("jT", [128, N], F32)
    gT = k.tile("gT", [128, N], F32)
    topv = k.tile("topv", [128, 16, 16], F32)
    topi = k.tile("topi", [128, 16, 16], U32)
    topif = k.tile("topif", [128, 16, 16], F32)
    scr = [k.tile(f"scr{i}", [128, 256], F32) for i in range(2)]
    cand = k.tile("cand", [128, 8, 256], F32)
    bv = k.tile("bv", [128, 8, 16], F32)
    bp = k.tile("bp", [128, 8, 16], U32)
    bpi = k.tile("bpi", [128, 8, 16], U32)
    akf = k.tile("akf", [128, 8, 16], F32)
    bkf = k.tile("bkf", [128, 8, 16], F32)
    gsel = k.tile("gsel", [128, 8, 16], F32)
    zs = k.tile("zs", [128, 8], F32)
    isel = k.tile("isel", [128, 128], F32)
    jsel = k.tile("jsel", [128, 128], F32)
    iota16 = k.tile("iota16", [128, 16], F32)
    k.copy("vector", iota16[:], c["iota_f"][:, 0:16])

    k.dma("sync", gx[:], io["gx"])
    k.dma("sync", gm[:], io["gm"])
    k.dma("sync", gf[:], io["gf"])
    if last:
        k.dma("sync", gl_[:], io["gl"])
    k.dma("sync", KT[:], io["KT"])

    assert 16 * N >= 2048
    for i in range(NE // 128):
        ld = V("xt", xt.t[:].rearrange("p a b -> p (a b)")[:, 0:2048])
        k.dma("sync", ld, io["down"].re(lambda a: a[i * 128:(i + 1) * 128, :]))
        tb = V("hn", hn.t[:].rearrange("p a b -> p (a b)")[:, 0:2048])
        for q4 in range(4):
            ps = pb[q4 % 4]
            for j4 in range(4):
                dc = q4 * 4 + j4
                k.tr(ps[:, j4 * 128:(j4 + 1) * 128], ld.re(lambda a: a[:, dc * 128:(dc + 1) * 128]), c["ident_f"][:])
            k.copy("vector" if q4 % 2 == 0 else "scalar", tb.re(lambda a: a[:, q4 * 512:(q4 + 1) * 512]), ps[:, 0:512])
        k.dma("sync", downT_b.re(lambda a: a[i]), tb)

    k.dma("sync", xt[:, :, 0:256], io["memT"].re(lambda a: a.rearrange("(c p) n -> p c n", p=128)))
    rms_fm(k, c, xt, gm, hn, 256, yt, pb[7][:, 0:256], rs)
    prot = PsumRot([pb[0], pb[1], pb[2], pb[3]])
    linear_fm(k, wkv_b.re(lambda a: a[:, 0:512]), 16, 512, hn, 256, wbufs, prot,
              lambda m, ps: k.copy("vector", kxT[:, m, :], ps))
    for half in range(2):
        wb = wbufs[half % 2]
        k.dma("sync", wb[:, :, 0:256], wkv_b.re(lambda a: a.rearrange("(kc p) m -> p kc m", p=128)[:, :, 512 + half * 256:512 + (half + 1) * 256]))
        for mc in range(2):
            ps = prot.next()
            for kc in range(16):
                k.mm(ps[:, 0:256], hn[:, kc, mc * 128:(mc + 1) * 128], wb[:, kc, 0:256], start=(kc == 0), stop=(kc == 15))
            k.copy("vector", vx[:, mc, half * 256:(half + 1) * 256], ps[:, 0:256])

    xTv = io["xT"].re(lambda a: a.rearrange("(c p) n -> p c n", p=128))
    yTv = io["yT"].re(lambda a: a.rearrange("(c p) n -> p c n", p=128))
    oTv = io["xoT"].re(lambda a: a.rearrange("(c p) n -> p c n", p=128))

    for ti in range(NT // N):
        n0 = ti * N
        if sel is None:
            k.dma("sync", xt[:, :, :], xTv.re(lambda a: a[:, :, n0:n0 + N]))
            k.dma("sync", yt[:, :, :], yTv.re(lambda a: a[:, :, n0:n0 + N]))
        else:
            xtmp = V("GG", GG.t[:, 0:32, :].rearrange("p a b -> p (a b)").bitcast(F32).rearrange("p (c n) -> p c n", c=16))
            ytmp = V("GG", GG.t[:, 32:48, :])
            for j in range(4):
                cj = j * NTS + n0
                k.dma("sync", xtmp, xTv.re(lambda a: a[:, :, cj:cj + N]))
                k.dma("sync", ytmp, yTv.re(lambda a: a[:, :, cj:cj + N]))
                if j == 0:
                    k.ts("vector", xt[:, :, :], xtmp, sel[:, 0:1], None, ALU.mult)
                    k.ts("vector", yt[:, :, :], ytmp, sel[:, 0:1], None, ALU.mult)
                else:
                    k.stt(xt[:, :, :], xtmp, sel[:, j:j + 1], xt[:, :, :], ALU.mult, ALU.add)
                    k.stt(yt[:, :, :], ytmp, sel[:, j:j + 1], yt[:, :, :], ALU.mult, ALU.add)
        prot = PsumRot([pb[0], pb[1], pb[2], pb[3]])

        def add_x(m, ps):
            k.tt("vector", xt[:, m, :], ps, xt[:, m, :], ALU.add)
        if STAGE >= 1:
            linear_fm(k, wout_b, 16, 2048, yt, N, wbufs, prot, add_x)
        if STAGE < 2:
            k.dma("sync", oTv.re(lambda a: a[:, :, n0:n0 + N]), xt[:, :, :])
            continue
        rms_fm(k, c, xt, gx, hn, N, yt, pb[7][:, 0:N], rs)
        linear_fm(k, wq_b, 16, 512, hn, N, wbufs, prot,
                  lambda m, ps: k.act(qx[:, m, :], ps, AF.Copy, scale=128 ** -0.5))
        for hd in range(4):
            for mc in range(2):
                ps = pb[4 + mc]
                k.mm(ps[:, 0:N], kxT[:, hd, mc * 128:(mc + 1) * 128], qx[:, hd, :])
                k.act(pT[mc][:, :], ps[:, 0:N], AF.Exp)
            for mc in range(2):
                k.mm(pb[6][:, 0:N], vx[:, mc, hd * 128:(hd + 1) * 128], pT[mc][:, :], start=(mc == 0), stop=(mc == 1))
            for mc in range(2):
                k.mm(pb[7][:, 0:N], c["ones_b"][:], pT[mc][:, :], start=(mc == 0), stop=(mc == 1))
            k.recip(rs[:, 0:N], pb[7][:, 0:N])
            k.tt("vector", ox[:, hd, :], pb[6][:, 0:N], rs[:, 0:N], ALU.mult)
        linear_fm(k, wo_b, 4, 2048, ox, N, wbufs, prot, add_x)
        if STAGE < 3:
            k.dma("sync", oTv.re(lambda a: a[:, :, n0:n0 + N]), xt[:, :, :])
            continue
        rms_fm(k, c, xt, gf, hn, N, yt, pb[7][:, 0:N], rs)
        linear_fm(k, wqry_b, 16, 1024, hn, N, wbufs, prot,
                  lambda m, ps: k.copy("scalar", qp[:, m, :], ps))
        for tc in range(N // 128):
            for hp in range(16):
                h, p = divmod(hp, 2)
                k.mm(pb[4 + hp // 4][:, (hp % 4) * 128:(hp % 4 + 1) * 128],
                     qp[:, h, tc * 128:(tc + 1) * 128], KT[:, hp, :])
            if SUB < 1:
                k.copy("vector", iT[:, tc * 128:(tc + 1) * 128], pb[4][:, 0:128])
                k.copy("vector", jT[:, tc * 128:(tc + 1) * 128], pb[5][:, 0:128])
                k.copy("vector", gT[:, tc * 128:(tc + 1) * 128], pb[7][:, 384:512])
                continue
            for hp in range(16):
                src = pb[4 + hp // 4][:, (hp % 4) * 128:(hp % 4 + 1) * 128]
                s_ = scr[hp % 2]
                k.gen("vector", lambda e, hp=hp, src=src: e.max(out=topv.t[:, hp, 0:8], in_=src.ap), reads=[src], writes=[topv[:]])
                k.gen("vector", lambda e, hp=hp, src=src: e.max_index(out=topi.t[:, hp, 0:8], in_max=topv.t[:, hp, 0:8], in_values=src.ap),
                      reads=[src, topv[:]], writes=[topi[:]])
                k.gen("vector", lambda e, hp=hp, src=src, s_=s_: e.match_replace(out=s_.t[:, 0:128], in_to_replace=topv.t[:, hp, 0:8], in_values=src.ap, imm_value=NEG),
                      reads=[src, topv[:]], writes=[s_[:]])
                k.gen("vector", lambda e, hp=hp, s_=s_: e.max(out=topv.t[:, hp, 8:16], in_=s_.t[:, 0:128]), reads=[s_[:]], writes=[topv[:]])
                k.gen("vector", lambda e, hp=hp, s_=s_: e.max_index(out=topi.t[:, hp, 8:16], in_max=topv.t[:, hp, 8:16], in_values=s_.t[:, 0:128]),
                      reads=[s_[:], topv[:]], writes=[topi[:]])
            if SUB < 2:
                k.copy("vector", iT[:, tc * 128:(tc + 1) * 128], V("topv", topv.t[:].rearrange("p a b -> p (a b)")[:, 0:128]))
                k.copy("vector", jT[:, tc * 128:(tc + 1) * 128], V("topi", topi.t[:].rearrange("p a b -> p (a b)")[:, 0:128]))
                k.copy("vector", gT[:, tc * 128:(tc + 1) * 128], V("topi", topi.t[:].rearrange("p a b -> p (a b)")[:, 128:256]))
                continue
            k.copy("vector", topif[:], topi[:])
            tv4 = topv.t[:].rearrange("p (h two) a -> p h two a", two=2)
            ti4 = topif.t[:].rearrange("p (h two) a -> p h two a", two=2)
            c4 = cand.t[:].rearrange("p h (a b) -> p h a b", a=16)
            k.tt("vector", V("cand", c4), V("topv", tv4[:, :, 0, :].unsqueeze(3).to_broadcast([128, 8, 16, 16])),
                 V("topv", tv4[:, :, 1, :].unsqueeze(2).to_broadcast([128, 8, 16, 16])), ALU.add)
            for h in range(8):
                s_ = scr[h % 2]
                k.gen("vector", lambda e, h=h: e.max(out=bv.t[:, h, 0:8], in_=cand.t[:, h, :]), reads=[cand[:]], writes=[bv[:]])
                k.gen("vector", lambda e, h=h: e.max_index(out=bp.t[:, h, 0:8], in_max=bv.t[:, h, 0:8], in_values=cand.t[:, h, :]),
                      reads=[cand[:], bv[:]], writes=[bp[:]])
                k.gen("vector", lambda e, h=h, s_=s_: e.match_replace(out=s_.t[:, :], in_to_replace=bv.t[:, h, 0:8], in_values=cand.t[:, h, :], imm_value=NEG),
                      reads=[cand[:], bv[:]], writes=[s_[:]])
                k.gen("vector", lambda e, h=h, s_=s_: e.max(out=bv.t[:, h, 8:16], in_=s_.t[:, :]), reads=[s_[:]], writes=[bv[:]])
                k.gen("vector", lambda e, h=h, s_=s_: e.max_index(out=bp.t[:, h, 8:16], in_max=bv.t[:, h, 8:16], in_values=s_.t[:, :]),
                      reads=[s_[:], bv[:]], writes=[bp[:]])
            if SUB < 3:
                k.copy("vector", iT[:, tc * 128:(tc + 1) * 128], V("bv", bv.t[:].rearrange("p a b -> p (a b)")))
                k.copy("vector", jT[:, tc * 128:(tc + 1) * 128], V("bp", bp.t[:].rearrange("p a b -> p (a b)")))
                continue
            k.tt("vector", gsel[:], bv[:], V("bv", bv.t[:, :, 0:1].to_broadcast([128, 8, 16])), ALU.subtract)
            k.act(gsel[:], gsel[:], AF.Exp)
            k.red(zs[:], gsel[:], ALU.add)
            k.recip(zs[:], zs[:])
            k.tt("vector", gsel[:], gsel[:], V("zs", zs.t[:, :].unsqueeze(2).to_broadcast([128, 8, 16])), ALU.mult)
            k.gen("vector", lambda e: e.tensor_single_scalar(out=bpi.t[:], in_=bp.t[:], scalar=4, op=ALU.logical_shift_right), reads=[bp[:]], writes=[bpi[:]])
            k.copy("vector", akf[:], bpi[:])
            k.gen("vector", lambda e: e.tensor_single_scalar(out=bpi.t[:], in_=bp.t[:], scalar=15, op=ALU.bitwise_and), reads=[bp[:]], writes=[bpi[:]])
            k.copy("vector", bkf[:], bpi[:])
            if SUB < 4:
                k.copy("vector", iT[:, tc * 128:(tc + 1) * 128], V("akf", akf.t[:].rearrange("p a b -> p (a b)")))
                k.copy("vector", jT[:, tc * 128:(tc + 1) * 128], V("bkf", bkf.t[:].rearrange("p a b -> p (a b)")))
                k.copy("vector", gT[:, tc * 128:(tc + 1) * 128], V("gsel", gsel.t[:].rearrange("p a b -> p (a b)")))
                continue
            io16 = iota16.t[:, :].unsqueeze(1).unsqueeze(1).to_broadcast([128, 8, 16, 16])
            for (kf, pp, dst) in ((akf, 0, isel), (bkf, 1, jsel)):
                k.tt("vector", V("cand", c4), V(kf.name, kf.t[:].unsqueeze(3).to_broadcast([128, 8, 16, 16])), V("iota16", io16), ALU.is_equal)
                k.tt("vector", V("cand", c4), V("cand", c4), V("topif", ti4[:, :, pp, :].unsqueeze(2).to_broadcast([128, 8, 16, 16])), ALU.mult)
                k.red(dst[:], V("cand", cand.t[:].rearrange("p h (a b) -> p (h a) b", a=16)), ALU.add)
            for (src, dstT) in ((isel[:], iT), (jsel[:], jT), (V("gsel", gsel.t[:].rearrange("p h a -> p (h a)")), gT)):
                k.tr(pb[0][:, 0:128], src, c["ident_f"][:])
                k.copy("vector", dstT[:, tc * 128:(tc + 1) * 128], pb[0][:, 0:128])
        if STAGE < 4:
            k.dma("sync", V("dram:xoT", oTv.ap[:, 0, n0:n0 + N]), iT[:, :])
            k.dma("sync", V("dram:xoT", oTv.ap[:, 1, n0:n0 + N]), jT[:, :])
            k.dma("sync", V("dram:xoT", oTv.ap[:, 2, n0:n0 + N]), gT[:, :])
            continue
        TPE = 512 // NI
        for ih in range(NIH):
            gro = PsumRot([pb[4], pb[5], pb[6], pb[7]])
            for t0 in range(0, N, TPE):
                ps = gro.next()
                for tq in range(TPE):
                    t = t0 + tq
                    a_ = At[t % 8]
                    b_ = Bt[t % 8]
                    k.ts("vector", a_[:, :], c["iota_f"][:, ih * NI:(ih + 1) * NI], iT[:, t:t + 1], gT[:, t:t + 1], ALU.is_equal, ALU.mult)
                    k.ts("vector", b_[:, :], c["iota_f"][:, :], jT[:, t:t + 1], None, ALU.is_equal)
                    k.mm(ps[:, tq * NI:(tq + 1) * NI], b_[:, :], a_[:, :])
                k.copy("vector" if (t0 // TPE) % 2 == 0 else "scalar",
                       V("GG", GG.t[:, :, t0:t0 + TPE].rearrange("p i t -> p t i")),
                       V(ps.name, ps.t[:, 0:TPE * NI].rearrange("p (t i) -> p t i", i=NI)))
            hro = PsumRot([pb[4], pb[5], pb[6], pb[7]])
            for il in range(NI):
                i = ih * NI + il
                db = dbuf[il % 6]
                k.dma("sync" if il % 2 == 0 else "scalar", V(db.name, db.t[:].rearrange("p a b -> p (a b)")), downT_b.re(lambda a: a[i]))
                ps = hro.next()
                for kc in range(16):
                    k.mm(ps[:, 0:N], db[:, kc, :], hn[:, kc, :], start=(kc == 0), stop=(kc == 15))
                g_ = glb[il % 4]
                k.act(g_[:, :], ps[:, 0:N], AF.Gelu)
                k.tt("vector", GG[:, il, :], g_[:, :], GG[:, il, :], ALU.mult)
            for dh in range(2):
                for b4 in range(4):
                    k.memset("vector", pb[b4][:, :], 0.0)
                for il in range(NI):
                    i = ih * NI + il
                    ub = ubuf[il % 6]
                    k.dma("sync" if il % 2 == 0 else "scalar", ub[:, 0:1024], up_b.re(lambda a: a[i * 128:(i + 1) * 128, dh * 1024:(dh + 1) * 1024]))
                    for dc in range(8):
                        k.mm(pb[dc // 2][:, (dc % 2) * 256:(dc % 2) * 256 + N], ub[:, dc * 128:(dc + 1) * 128], GG[:, il, :],
                             start=False, stop=False, skip_group_check=True)
                for dc in range(8):
                    k.tt("vector", xt[:, dh * 8 + dc, :], pb[dc // 2][:, (dc % 2) * 256:(dc % 2) * 256 + N], xt[:, dh * 8 + dc, :], ALU.add)
        if last:
            rms_fm(k, c, xt, gl_, hn, N, yt, pb[7][:, 0:N], rs)
            for cc in range(16):
                k.stt(xt[:, cc, :], xt[:, cc, :], gl_[:, cc:cc + 1], rs[:, 0:N], ALU.mult, ALU.mult)
        k.dma("sync", oTv.re(lambda a: a[:, :, n0:n0 + N]), xt[:, :, :])


def make_KT(subk):
    KT = np.zeros((128, 16, 128), np.float32)
    for h in range(8):
        for p in range(2):
            KT[64 * p:64 * p + 64, 2 * h + p, :] = subk[h, p].T
    return KT


WA = 1800
C_MI, C_MF = 896, 1024
C_V, C_Z, C_MV, C_MO, C_SM = 1152, 1280, 1536, 1664, 1792
PEN = 10000.0
SA = 99
MERGE = 0
SUBA = 99


def emit_phase_a(k, c, io, S):
    N = 256
    NB = S // 256
    pb = c["pb"]
    T = k.tile
    w_sb = T("w_sb", [128, 16, WA], BF16)
    kT_all = T("kT_all", [128, S], BF16)
    V_all = T("V_all", [128, S // 128, 2, 66], BF16)
    xt = T("xt", [128, 16, N], F32)
    hn = T("hn", [128, 16, N], BF16)
    rs = T("rs", [128, N], F32)
    gmx = T("gmx", [128, 16], F32)
    qz = [T(f"qz{h}", [128, N], BF16) for h in range(2)]
    kmT = T("kmT", [128, 64], BF16)
    kmf = T("kmf", [128, 64], F32)
    ksum = T("ksum", [128, 1], F32)
    gate_sb = [T(f"gate_sb{h}", [128, 64], F32) for h in range(2)]
    cst = [T(f"cst{h}", [128, 64], F32) for h in range(2)]
    gtmp = T("gtmp", [128, 64], F32)
    m8 = T("m8", [128, 8], F32)
    Ttab = T("Ttab", [128, 2, 2, 64], F32)
    ownc = T("ownc", [128, 2, 2], F32)
    skk = T("skk", [128, 2, 256], BF16)
    cbias = T("cbias", [128, 2, 256], BF16)
    onesrow = T("onesrow", [128, 128], BF16)
    onesrow_f = T("onesrow_f", [128, 128], F32)
    Pb = [T(f"Pb{i}", [128, 256], BF16) for i in range(2)]
    PT = [T(f"PT{i}", [128, 2, 128], BF16) for i in range(3)]
    ya_tok = T("ya_tok", [128, 128], F32)
    rden = T("rden", [128, 1], F32)
    yT_tile = T("yT_tile", [128, 4, N], BF16)
    cw = T("cw", [128, 5, 4], F32)
    cb = T("cb", [128, 5], F32)
    hp = T("hp", [128, 16], F32)
    Aneg = T("Aneg", [128, 4], F32)
    nfb = T("nfb", [128, 1], F32)
    gsn = T("gsn", [128, 256], F32)
    gmn = T("gmn", [128, 128], F32)
    wq_m = T("wq_m", [128, 128], F32)
    wk_m = T("wk_m", [128, 128], F32)
    cin = T("cin", [128, 5, 3 + N], F32)
    cacc = T("cacc", [128, N], F32)
    cout = T("cout", [128, 5, N], F32)
    zs = [T(f"zs{i}", [128, 256], F32) for i in range(2)]
    mv_sb = [T(f"mv_sb{i}", [128, 128], F32) for i in range(2)]
    sig = [T(f"sig{i}", [128, 128], F32) for i in range(2)]
    sm_sb = [T(f"sm_sb{i}", [128, 8], F32) for i in range(2)]
    dt4 = T("dt4", [128, 4], F32)
    a4 = T("a4", [128, 4], F32)
    cs4 = T("cs4", [128, 4], F32)
    ecs = T("ecs", [128, 4], F32)
    dte = T("dte", [128, 4], F32)
    cdb = T("cdb", [128, 4], F32)
    xs_sb = T("xs_sb", [128, 256], F32)
    Xdt = T("Xdt", [128, 256], F32)
    Xdte = T("Xdte", [128, 256], F32)
    B_tok = T("B_tok", [128, 128], F32)
    CBm = T("CBm", [128, 128], F32)
    Ah = [T(f"Ah{i}", [128, 128], F32) for i in range(2)]
    Eh = [T(f"Eh{i}", [128, 128], F32) for i in range(2)]
    MT = [T(f"MT{i}", [128, 128], F32) for i in range(2)]
    hstate = T("hstate", [128, 256], F32)
    y1 = T("y1", [128, 256], F32)
    y2 = T("y2", [128, 256], F32)
    ssq = T("ssq", [128, 1], F32)
    ysn = T("ysn", [128, 256], F32)
    qmT = T("qmT", [128, N], F32)
    kmT2 = T("kmT2", [128, N], F32)
    k_tok = T("k_tok", [128, 128], F32)
    KQm = T("KQm", [128, 128], F32)
    vw = T("vw", [128, 130], F32)
    vwe = T("vwe", [128, 130], F32)
    Cn = T("Cn", [128, 130], F32)
    t1 = T("t1", [128, 130], F32)
    t2 = T("t2", [128, 130], F32)
    dd = T("dd", [128, 1], F32)
    hm = T("hm", [128, 128], F32)
    hm2 = T("hm2", [128, 128], F32)
    cols = T("cols", [128, 8], F32)
    spsl = T("spsl", [128, 2], F32)
    NR = 12
    R = T("R", [128, NR, 128], F32)
    rowsb = T("rowsb", [128, 512], F32)
    ms = T("ms", [128, 8], F32)
    R_L1, R_IG, R_BN, R_W, R_CM, R_MX, R_EU, R_IW, R_EMT, R_EW, R_WE, R_Z = range(12)

    wv = io["win"].re(lambda a: a.rearrange("(kc p) m -> p kc m", p=128))
    for q4 in range(4):
        k.dma("gpsimd", w_sb[:, q4 * 4:(q4 + 1) * 4, :], wv.re(lambda a: a[:, q4 * 4:(q4 + 1) * 4, :]))
    for dst, nm in ((gmx, "gmix"), (cw, "cw"), (cb, "cb"), (hp, "hp"), (gsn, "gsn"), (gmn, "gmn"), (wq_m, "wqm"), (wk_m, "wkm"),
                    (Ttab, "Ttab"), (ownc, "ownc")):
        k.dma("sync", dst[:], io[nm])
    k.dma("sync", xt[:, 0:2, :], io["skk"])
    k.dma("sync", xt[:, 2:4, :], io["cbias"])
    k.copy("vector", skk[:], xt[:, 0:2, :])
    k.copy("vector", cbias[:], xt[:, 2:4, :])
    k.memset("vector", onesrow[:], 0.0)
    k.memset("vector", onesrow[0:1, :], 1.0)
    k.memset("vector", onesrow_f[:], 0.0)
    k.memset("vector", onesrow_f[0:1, :], 1.0)
    k.act(Aneg[:], hp[:, 4:8], AF.Exp)
    k.ts("vector", Aneg[:], Aneg[:], -1.0, None, ALU.mult)
    k.ts("vector", nfb[:], hp[:, 13:14], -1.0, None, ALU.mult)
    for h in range(2):
        k.memset("vector", qz[h][:], 0.0)
        k.memset("vector", gate_sb[h][:], NEG)
        k.memset("vector", cst[h][:], 0.0)
    k.memset("vector", kmT[:], 0.0)
    k.memset("vector", kmf[:], 0.0)
    k.memset("gpsimd", V_all[:], 1.0)
    k.memset("vector", cin[:], 0.0)
    k.memset("vector", hstate[:], 0.0)
    k.memset("vector", Cn[:], 0.0)
    k.memset("vector", vw[:], 0.0)
    k.memset("vector", vwe[:], 0.0)
    k.memset("gpsimd", R[:], 0.0)
    k.memset("vector", ms[:], 0.0)
    k.memset("vector", ms[0:1, 0:1], NEG)

    k.memset("vector", yT_tile[:], 0.0)
    xTv = io["xT"].re(lambda a: a.rearrange("(c p) n -> p c n", p=128))
    yTv = io["yT"].re(lambda a: a.rearrange("(c p) n -> p c n", p=128))
    rotA = PsumRot([pb[0], pb[1]])
    one1 = c["ones_f"][:, 0:1]

    for ti in range(S // N):
        t0 = ti * N
        blk = ti
        k.dma("sync", xt[:, :, :], xTv.re(lambda a: a[:, :, t0:t0 + N]))
        if SUBA < 0.1:
            k.dma("sync", yTv.re(lambda a: a[:, :, t0:t0 + N]), yT_tile[:, :, :])
            continue
        rms_fm(k, c, xt, gmx, hn, N, hn, pb[7][:, 0:N], rs)
        for m in range(7 if SUBA >= 1 else (0 if SUBA < 0.3 else (1 if SUBA < 0.5 else 2))):
            ps = rotA.next()
            for kc in range(16):
                k.mm(ps[:, 0:N], w_sb[:, kc, m * 128:(m + 1) * 128], hn[:, kc, :], start=(kc == 0), stop=(kc == 15))
            if m == 0:
                k.act(qz[0][0:64, :], ps[0:64, 0:N], AF.Copy, scale=0.125)
                k.act(qz[1][64:128, :], ps[64:128, 0:N], AF.Copy, scale=0.125)
            elif m == 1:
                k.act(kT_all[:, t0:t0 + N], ps[:, 0:N], AF.Copy, accum=ksum[:])
                k.ts("vector", kmf[:, blk:blk + 1], ksum[:], 1.0 / 256, None, ALU.mult)
                k.copy("vector", kmT[:], kmf[:])
            else:
                j = m - 2
                k.copy("vector" if j % 2 == 0 else "scalar", cin[:, j, 3:3 + N], ps[:, 0:N])
        if SUBA < 2:
            k.dma("sync", yTv.re(lambda a: a[:, :, t0:t0 + N]), yT_tile[:, :, :])
            continue
        for r, col in ((0, C_MI), (1, C_MF)):
            for kc in range(16):
                k.mm(pb[7][:, r * 256:(r + 1) * 256], w_sb[:, kc, col:col + 128], hn[:, kc, :], start=(kc == 0), stop=(kc == 15))
        k.copy("vector", rowsb[0:1, :], pb[7][0:1, 0:512])
        if SUBA < 3:
            k.dma("sync", yTv.re(lambda a: a[:, :, t0:t0 + N]), yT_tile[:, :, :])
            continue
        for ch in range(2):
            c0 = ch * 128
            b1 = rotA.next()
            b2 = rotA.next()
            for kc in range(16):
                k.mm(b1[:, 0:512], hn[:, kc, c0:c0 + 128], w_sb[:, kc, C_V:C_MO], start=(kc == 0), stop=(kc == 15))
            for kc in range(16):
                k.mm(b2[:, 0:136], hn[:, kc, c0:c0 + 128], w_sb[:, kc, C_MO:C_MO + 136], start=(kc == 0), stop=(kc == 15))
            gch = ti * 2 + ch
            k.copy("vector", V("V_all", V_all.t[:, gch, :, 0:64]), V(b1.name, b1.t[:, 0:128].rearrange("p (h d) -> p h d", h=2)))
            k.act(zs[ch][:], b1[:, 128:384], AF.Silu)
            k.copy("vector", mv_sb[ch][:], b1[:, 384:512])
            k.act(sig[ch][:], b2[:, 0:128], AF.Sigmoid)
            k.copy("vector", sm_sb[ch][:], b2[:, 128:136])

        _mark_moba = len(k.P.ops)
        si = 0
        for qc in range(2 if SA >= 1 else 0):
            qs = slice(qc * 128, (qc + 1) * 128)
            for h in range(2):
                if blk > 0:
                    k.mm(pb[4][:, 256:320], qz[h][:, qs], kmT[:, 0:64])
                    k.copy("vector", gate_sb[h][:, 0:blk], pb[4][:, 256:256 + blk])
                    k.gen("vector", lambda e, h=h: e.max(out=m8.t[:, :], in_=gate_sb[h].t[:, :]), reads=[gate_sb[h][:]], writes=[m8[:]])
                    k.ts("vector", gtmp[:, 0:blk], gate_sb[h][:, 0:blk], m8[:, 2:3], PEN, ALU.is_ge, ALU.mult)
                    k.tt("vector", cst[h][:, 0:blk], gtmp[:, 0:blk], Ttab[:, qc, h, NB - 1 - blk:NB - 1], ALU.add)
                nblk = blk + 1
                ug0 = si
                si += nblk

                def st1(n, h=h, qs=qs, qc=qc, ug0=ug0):
                    u = ug0 + n
                    sp_, P_ = pb[2 + u % 2], Pb[u % 2]
                    own = (n == blk)
                    k.mm(sp_[:, 0:256], qz[h][:, qs], kT_all[:, n * 256:(n + 1) * 256], start=True, stop=False)
                    k.mm(sp_[:, 0:256], onesrow[:], skk[:, h, :], start=False, stop=(not own))
                    if own:
                        k.mm(sp_[:, 0:256], c["ident_b"][:], cbias[:, qc, :], start=False, stop=True)
                    bias = ownc[:, qc, h:h + 1] if own else cst[h][:, n:n + 1]
                    k.act(P_[:, :], sp_[:, 0:256], AF.Exp, bias=bias)

                def st2(n, ug0=ug0):
                    u = ug0 + n
                    pt_, P_, PT_ = pb[4 + u % 2], Pb[u % 2], PT[u % 3]
                    ptv = V(pt_.name, pt_.t[:, 0:128].bitcast(BF16).rearrange("p (c q) -> p c q", c=2))
                    for kc in range(2):
                        k.tr(V(pt_.name, ptv.ap[:, kc, :]), P_[:, kc * 128:(kc + 1) * 128], c["ident_b"][:])
                    k.copy("vector", PT_[:, :, :], ptv)

                def st3(n, h=h, ug0=ug0):
                    u = ug0 + n
                    PT_ = PT[u % 3]
                    own = (n == blk)
                    for kc in range(2):
                        k.mm(pb[6][:, 0:66], PT_[:, kc, :], V("V_all", V_all.t[:, n * 2 + kc, h, :]),
                             start=(n == 0 and kc == 0), stop=(own and kc == 1))

                for step in range(nblk + 2):
                    if step < nblk:
                        st1(step)
                    if 1 <= step <= nblk:
                        st2(step - 1)
                    if step >= 2:
                        st3(step - 2)
                k.recip(rden[:], pb[6][:, 64:65])
                k.ts("vector", ya_tok[:, h * 64:(h + 1) * 64], pb[6][:, 0:64], rden[:, 0:1], None, ALU.mult)
            k.tr(pb[5][:, 256:384], ya_tok[:], c["ident_f"][:])
            k.copy("scalar", yT_tile[:, 0, qs], pb[5][:, 256:384])

        _mark_ssd = len(k.P.ops)
        if SA < 2:
            k.dma("sync", yTv.re(lambda a: a[:, :, t0:t0 + N]), yT_tile[:, :, :])
            continue
        for j in range(5):
            k.ts("vector", cacc[:], cin[:, j, 0:N], cw[:, j, 0:1], None, ALU.mult)
            for tp in range(1, 4):
                k.stt(cacc[:], cin[:, j, tp:tp + N], cw[:, j, tp:tp + 1], cacc[:], ALU.mult, ALU.add)
            k.act(cout[:, j, :], cacc[:], AF.Silu, bias=cb[:, j:j + 1])
            k.copy("vector", cin[:, j, 0:3], cin[:, j, N:N + 3])
        bA, bB, bC = pb[0], pb[1], pb[7]
        ps = bA
        k.mm(ps[:, 0:N], wq_m[:], cout[:, 4, :])
        k.copy("vector", qmT[:], ps[:, 0:N])
        ps = bB
        k.mm(ps[:, 0:N], wk_m[:], cout[:, 4, :])
        k.act(kmT2[:], ps[:, 0:N], AF.Copy, scale=128 ** -0.5)

        for ch in range(2 if SA >= 3 else 0):
            cs_ = slice(ch * 128, (ch + 1) * 128)
            k.tt("vector", dt4[:], sm_sb[ch][:, 0:4], hp[:, 0:4], ALU.add)
            k.act(dt4[:], dt4[:], AF.Exp)
            k.act(dt4[:], dt4[:], AF.Ln, bias=one1)
            k.tt("vector", a4[:], dt4[:], Aneg[:], ALU.mult)
            k.mm(bA[:, 0:4], c["tri_f"][:], a4[:])
            k.mm(bA[:, 4:8], c["ones_f"][:], a4[:])
            k.copy("vector", cs4[:], bA[:, 0:4])
            k.act(ecs[:], bA[:, 0:4], AF.Exp)
            k.tt("vector", dte[:], bA[:, 4:8], cs4[:], ALU.subtract)
            k.act(dte[:], dte[:], AF.Exp)
            k.act(cdb[:], bA[:, 4:8], AF.Exp)
            pxs = bB
            for j in range(2):
                k.tr(pxs[:, j * 128:(j + 1) * 128], cout[:, j, cs_], c["ident_f"][:])
            k.copy("scalar", xs_sb[:], pxs[:, 0:256])
            for h in range(4):
                hs = slice(h * 64, (h + 1) * 64)
                k.ts("vector", Xdt[:, hs], xs_sb[:, hs], dt4[:, h:h + 1], None, ALU.mult)
                k.ts("vector", Xdte[:, hs], Xdt[:, hs], dte[:, h:h + 1], None, ALU.mult)
            pbt = bA
            k.tr(pbt[:, 0:128], cout[:, 2, cs_], c["ident_f"][:])
            k.copy("scalar", B_tok[:], pbt[:, 0:128])
            pcb = bB
            k.mm(pcb[:, 0:128], cout[:, 2, cs_], cout[:, 3, cs_])
            k.tt("vector", CBm[:], pcb[:, 0:128], c["tri_f"][:], ALU.mult)
            pyd = bC
            for h in range(4):
                A_, E_, M_ = Ah[h % 2], Eh[h % 2], MT[h % 2]
                k.ts("vector", A_[:], c["upp_f"][:], a4[:, h:h + 1], None, ALU.mult)
                pdm = bA if h % 2 == 0 else bB
                k.mm(pdm[:, 0:128], A_[:], c["tri_f"][:])
                k.act(E_[:], pdm[:, 0:128], AF.Exp)
                k.tt("vector", M_[:], E_[:], CBm[:], ALU.mult)
                k.mm(pyd[:, h * 64:(h + 1) * 64], M_[:], Xdt[:, h * 64:(h + 1) * 64])
            pyo = bA
            k.mm(pyo[:, 0:256], cout[:, 3, cs_], hstate[:])
            pst = bB
            k.mm(pst[:, 0:256], B_tok[:], Xdte[:])
            for h in range(4):
                hs = slice(h * 64, (h + 1) * 64)
                k.stt(y1[:, hs], xs_sb[:, hs], hp[:, 8 + h:9 + h], pyd[:, hs], ALU.mult, ALU.add)
                k.stt(y2[:, hs], pyo[:, hs], ecs[:, h:h + 1], y1[:, hs], ALU.mult, ALU.add)
                k.stt(hstate[:, hs], hstate[:, hs], cdb[:, h:h + 1], pst[:, hs], ALU.mult, ALU.add)
            k.tt("vector", y2[:], y2[:], zs[ch][:], ALU.mult)
            k.act(y1[:], y2[:], AF.Square, accum=ssq[:])
            k.act(ssq[:], ssq[:], AF.Ln, bias=c["eps"][:, 0:1], scale=1.0 / 256)
            k.act(ssq[:], ssq[:], AF.Exp, scale=-0.5)
            k.stt(ysn[:], y2[:], ssq[:, 0:1], gsn[:], ALU.mult, ALU.mult)
            for j in range(2):
                pt2 = bA if j % 2 == 0 else bB
                k.tr(pt2[:, 0:128], ysn[:, j * 128:(j + 1) * 128], c["ident_f"][:])
                k.copy("scalar", yT_tile[:, 1 + j, cs_], pt2[:, 0:128])

            if SA < 4:
                continue
            def row(r):
                return R[0:1, r, :]
            k.act(row(R_L1), rowsb[0:1, 256 + ch * 128:256 + (ch + 1) * 128], AF.Exp, bias=nfb[0:1, 0:1], scale=-1.0)
            k.act(row(R_L1), row(R_L1), AF.Ln, bias=one1.re(lambda a: a[0:1, :]))
            k.ts("vector", row(R_IG), rowsb[0:1, ch * 128:(ch + 1) * 128], hp[0:1, 12:13], None, ALU.add)
            k.gen("vector", lambda e, ch=ch: e.tensor_tensor_scan(out=R.t[0:1, R_BN, :], data0=R.t[0:1, R_L1, :],
                                                                   data1=R.t[0:1, R_Z, :], initial=0.0, op0=ALU.add, op1=ALU.add),
                  reads=[R[:]], writes=[R[:]])
            k.tt("vector", row(R_W), row(R_IG), row(R_BN), ALU.add)
            k.gen("vector", lambda e, ch=ch: e.tensor_tensor_scan(out=R.t[0:1, R_CM, :], data0=R.t[0:1, R_W, :],
                                                                   data1=R.t[0:1, R_W, :], initial=NEG, op0=ALU.max, op1=ALU.max),
                  reads=[R[:]], writes=[R[:]])
            k.ts("vector", row(R_MX), row(R_CM), ms[0:1, 0:1], None, ALU.max)
            k.act(row(R_EU), row(R_MX), AF.Exp, scale=-1.0)
            k.act(row(R_IW), row(R_MX), AF.Exp, scale=-1.0, bias=ms[0:1, 0:1])
            k.tt("vector", row(R_EMT), row(R_BN), row(R_MX), ALU.subtract)
            k.act(row(R_EMT), row(R_EMT), AF.Exp)
            k.act(row(R_EW), row(R_W), AF.Exp)
            cmL = R[0:1, R_CM, 127:128]
            bnL = R[0:1, R_BN, 127:128]
            k.ts("vector", ms[0:1, 1:2], cmL, -1.0, None, ALU.mult)
            k.act(row(R_WE), row(R_W), AF.Exp, bias=ms[0:1, 1:2])
            k.tt("vector", ms[0:1, 2:3], ms[0:1, 0:1], bnL, ALU.subtract)
            k.tt("vector", ms[0:1, 3:4], cmL, bnL, ALU.subtract)
            k.tt("vector", ms[0:1, 4:5], ms[0:1, 2:3], ms[0:1, 3:4], ALU.max)
            k.ts("vector", ms[0:1, 5:6], ms[0:1, 4:5], -1.0, None, ALU.mult)
            k.act(R[0:1, R_Z + 0, 0:0 + 1] if False else ms[0:1, 6:7], ms[0:1, 2:3], AF.Exp, bias=ms[0:1, 5:6])
            k.act(ms[0:1, 7:8], ms[0:1, 3:4], AF.Exp, bias=ms[0:1, 5:6])
            pcl = bA
            for ci, r in enumerate((R_EU, R_IW, R_EMT, R_EW, R_WE)):
                k.mm(pcl[:, 2 * ci:2 * ci + 2], R[:, r, :], c["ones_f"][:, 0:2])
            k.mm(pcl[:, 16:18], onesrow_f[:], ms[:, 6:8])
            k.copy("vector", cols[:, 0:5], V(pcl.name, pcl.t[:, 0:10].rearrange("p (c two) -> p c two", two=2)[:, :, 0]))
            k.copy("vector", spsl[:], pcl[:, 16:18])
            pkt = bB
            k.mm(pkt[:, 0:128], cout[:, 4, cs_], wk_m[:])
            k.act(k_tok[:], pkt[:, 0:128], AF.Copy, scale=128 ** -0.5)
            pkq = bA
            k.mm(pkq[:, 0:128], kmT2[:, cs_], qmT[:, cs_])
            k.tt("vector", KQm[:], pkq[:, 0:128], c["tri_f"][:], ALU.mult)
            k.ts("vector", vw[:, 0:128], mv_sb[ch][:], cols[:, 3:4], None, ALU.mult)
            k.copy("vector", vw[:, 128:129], cols[:, 3:4])
            k.ts("vector", vwe[:, 0:128], mv_sb[ch][:], cols[:, 4:5], None, ALU.mult)
            k.copy("vector", vwe[:, 128:129], cols[:, 4:5])
            pin = bB
            k.mm(pin[:, 0:130], KQm[:], vw[:])
            pit = bC
            k.mm(pit[:, 0:130], qmT[:, cs_], Cn[:])
            k.ts("vector", t1[:], pin[:, 0:130], cols[:, 0:1], None, ALU.mult)
            k.stt(t2[:], pit[:, 0:130], cols[:, 1:2], t1[:], ALU.mult, ALU.add)
            k.ts("vector", dd[:], t2[:, 128:129], -1.0, None, ALU.mult)
            k.tt("vector", dd[:], dd[:], t2[:, 128:129], ALU.max)
            k.tt("vector", dd[:], dd[:], cols[:, 2:3], ALU.max)
            k.recip(dd[:], dd[:])
            k.ts("vector", hm[:], t2[:, 0:128], dd[:, 0:1], None, ALU.mult)
            k.act(hm2[:], hm[:], AF.Square, accum=ssq[:])
            k.act(ssq[:], ssq[:], AF.Ln, bias=c["eps"][:, 0:1], scale=1.0 / 128)
            k.act(ssq[:], ssq[:], AF.Exp, scale=-0.5)
            k.stt(hm2[:], hm[:], ssq[:, 0:1], gmn[:], ALU.mult, ALU.mult)
            k.tt("vector", hm2[:], hm2[:], sig[ch][:], ALU.mult)
            pt3 = bA
            k.tr(pt3[:, 0:128], hm2[:], c["ident_f"][:])
            k.copy("scalar", yT_tile[:, 3, cs_], pt3[:, 0:128])
            pcl2 = bB
            k.mm(pcl2[:, 0:130], k_tok[:], vwe[:])
            k.ts("vector", t1[:], pcl2[:, 0:130], spsl[:, 1:2], None, ALU.mult)
            k.stt(Cn[:], Cn[:], spsl[:, 0:1], t1[:], ALU.mult, ALU.add)
            k.copy("vector", ms[0:1, 0:1], ms[0:1, 4:5])
        sa_, sb_ = k.P.ops[_mark_moba:_mark_ssd], k.P.ops[_mark_ssd:]
        merged, ia, ib = [], 0, 0
        while ia < len(sa_) or ib < len(sb_):
            if ib >= len(sb_) or (ia < len(sa_) and ia * len(sb_) <= ib * len(sa_)):
                merged.append(sa_[ia]); ia += 1
            else:
                merged.append(sb_[ib]); ib += 1
        if MERGE:
            k.P.ops[_mark_moba:] = merged
        k.dma("sync", yTv.re(lambda a: a[:, :, t0:t0 + N]), yT_tile[:, :, :])


def phase_a_host_inputs(inp, l, b, g, S):
    w_in = inp["w_in"][l]
    o = np.cumsum([0, 512, 512, 512, 1024, 2048, 16, 512, 512, 512, 4, 4])
    oq, ok, ov, oz, oxbc, odt, omu, omv, omo, omi, omf = o[:11]
    win = np.zeros((2048, WA), np.float32)
    win[:, 0:128] = w_in[:, oq + 128 * g: oq + 128 * (g + 1)]
    win[:, 128:256] = w_in[:, ok + 128 * g: ok + 128 * (g + 1)]
    win[:, 256:512] = w_in[:, oxbc + 256 * g: oxbc + 256 * (g + 1)]
    win[:, 512:640] = w_in[:, oxbc + 1024 + 128 * g: oxbc + 1024 + 128 * (g + 1)]
    win[:, 640:768] = w_in[:, oxbc + 1536 + 128 * g: oxbc + 1536 + 128 * (g + 1)]
    win[:, 768:896] = w_in[:, omu + 128 * g: omu + 128 * (g + 1)]
    win[:, C_V:C_V + 128] = w_in[:, ov + 128 * g: ov + 128 * (g + 1)]
    win[:, C_Z:C_Z + 256] = w_in[:, oz + 256 * g: oz + 256 * (g + 1)]
    win[:, C_MV:C_MV + 128] = w_in[:, omv + 128 * g: omv + 128 * (g + 1)]
    win[:, C_MO:C_MO + 128] = w_in[:, omo + 128 * g: omo + 128 * (g + 1)]
    win[:, C_SM:C_SM + 4] = w_in[:, odt + 4 * g: odt + 4 * (g + 1)]
    win[:, C_MI] = w_in[:, omi + g]
    win[:, C_MF] = w_in[:, omf + g]
    cw = np.zeros((128, 5, 4), np.float32)
    cb = np.zeros((128, 5), np.float32)
    scw, scb = inp["ssm_conv_w"][l], inp["ssm_conv_b"][l]
    chans = [np.arange(256 * g, 256 * g + 128), np.arange(256 * g + 128, 256 * g + 256),
             np.arange(1024 + 128 * g, 1024 + 128 * (g + 1)), np.arange(1536 + 128 * g, 1536 + 128 * (g + 1))]
    for j, ch in enumerate(chans):
        cw[:, j, :] = scw[:, ch].T
        cb[:, j] = scb[ch]
    cw[:, 4, :] = inp["mlstm_conv_w"][l][:, 128 * g:128 * (g + 1)].T
    cb[:, 4] = inp["mlstm_conv_b"][l][128 * g:128 * (g + 1)]
    hp = np.zeros((128, 16), np.float32)
    hp[:, 0:4] = inp["ssm_dt_bias"][l][4 * g:4 * g + 4]
    hp[:, 4:8] = inp["ssm_A_log"][l][4 * g:4 * g + 4]
    hp[:, 8:12] = inp["ssm_D"][l][4 * g:4 * g + 4]
    hp[:, 12] = inp["mlstm_i_bias"][l][g]
    hp[:, 13] = inp["mlstm_f_bias"][l][g]
    gsn = np.broadcast_to(inp["ssm_norm_g"][l][256 * g:256 * (g + 1)], (128, 256)).copy()
    gmn = np.broadcast_to(inp["mlstm_norm_g"][l][128 * g:128 * (g + 1)], (128, 128)).copy()
    NB = S // 256
    slopes = 2.0 ** (-8.0 * (np.arange(1, 9)) / 8)
    Ttab = np.zeros((128, 2, 2, 64), np.float32)
    ownc = np.zeros((128, 2, 2), np.float32)
    skk = np.zeros((128, 2, 256), np.float32)
    cbias = np.zeros((128, 2, 256), np.float32)
    tl = np.arange(128)
    for par in range(2):
        qq = tl + 128 * par
        for hh in range(2):
            sl = slopes[2 * g + hh]
            for m in range(NB):
                Ttab[:, par, hh, m] = -PEN - sl * (qq + 256.0 * (NB - 1 - m))
            ownc[:, par, hh] = -sl * qq
        cbias[:, par, :] = np.where(np.arange(256)[None, :] <= qq[:, None], 0.0, -PEN)
    for hh in range(2):
        skk[0, hh, :] = slopes[2 * g + hh] * np.arange(256)
    gm = inp["mix_norm_g"][l]
    return dict(win=win, gmix=np.ascontiguousarray(gm.reshape(16, 128).T), cw=cw, cb=cb, hp=hp, gsn=gsn, gmn=gmn,
                wqm=np.ascontiguousarray(inp["mlstm_wq"][l][g]), wkm=np.ascontiguousarray(inp["mlstm_wk"][l][g]),
                Ttab=Ttab, ownc=ownc, skk=skk, cbias=cbias)


A_INPUT_SHAPES = dict(win=[2048, WA], gmix=[128, 16], cw=[128, 5, 4], cb=[128, 5], hp=[128, 16], gsn=[128, 256], gmn=[128, 128],
                      wqm=[128, 128], wkm=[128, 128], Ttab=[128, 2, 2, 64], ownc=[128, 2, 2], skk=[128, 2, 256], cbias=[128, 2, 256])


from concourse.bass_utils import run_bass_kernel_spmd

SEQ = 16384
_CACHE = {}


def _build_a(S):
    key = ("a", S)
    if key in _CACHE:
        return _CACHE[key]
    nc = bass.Bass("TRN2", target_bir_lowering=False)
    k = K(nc)
    io = {nm: V("dram:" + nm, nc.dram_tensor(nm, list(shp), F32, kind="ExternalInput").ap()) for nm, shp in A_INPUT_SHAPES.items()}
    io["xT"] = V("dram:xT", nc.dram_tensor("xT", [2048, S], F32, kind="ExternalInput").ap())
    io["yT"] = V("dram:yT", nc.dram_tensor("yT", [512, S], BF16, kind="ExternalOutput").ap())
    c = make_consts(k)
    emit_phase_a(k, c, io, S)
    k.P.build()
    _CACHE[key] = nc
    return nc


B_SHAPES = dict(wout=[2048, 2048], wq=[2048, 512], wkv=[2048, 1024], wo=[512, 2048], wqry=[2048, 1024], KT=[128, 16, 128],
                down=[16384, 2048], up=[16384, 2048], gx=[128, 16], gm=[128, 16], gf=[128, 16], gl=[128, 16], memT=[2048, 256])


def _build_b(NT, last):
    key = ("b", NT, last)
    if key in _CACHE:
        return _CACHE[key]
    nc = bass.Bass("TRN2", target_bir_lowering=False)
    k = K(nc)
    io = {nm: V("dram:" + nm, nc.dram_tensor(nm, list(shp), F32, kind="ExternalInput").ap()) for nm, shp in B_SHAPES.items()}
    io["xT"] = V("dram:xT", nc.dram_tensor("xT", [2048, NT], F32, kind="ExternalInput").ap())
    io["yT"] = V("dram:yT", nc.dram_tensor("yT", [2048, NT], BF16, kind="ExternalInput").ap())
    io["xoT"] = V("dram:xoT", nc.dram_tensor("xoT", [2048, NT], F32, kind="ExternalOutput").ap())
    c = make_consts(k)
    emit_phase_b(k, c, io, NT, last=last)
    k.P.build()
    _CACHE[key] = nc
    return nc


def _gfm(g):
    return np.ascontiguousarray(np.asarray(g, np.float32).reshape(16, 128).T)


def kernel(**inputs):
    inp = {k_: np.asarray(v) for k_, v in inputs.items()}
    x = inp["x"]
    Bsz, S, _ = x.shape
    NT = S // 4
    xT = [np.ascontiguousarray(x[b].T) for b in range(Bsz)]
    memT = [np.ascontiguousarray(inp["mem"][b].T) for b in range(Bsz)]
    perm = np.concatenate([np.concatenate([128 * g + np.arange(128), 512 + 256 * g + np.arange(256), 1536 + 128 * g + np.arange(128)])
                           for g in range(4)])
    depth = inp["w_in"].shape[0]
    for l in range(depth):
        ncA = _build_a(S)
        in_maps = []
        for core in range(8):
            b, g = divmod(core, 4)
            m = phase_a_host_inputs(inp, l, b, g, S)
            m["xT"] = xT[b]
            in_maps.append(m)
        resA = run_bass_kernel_spmd(ncA, in_maps, core_ids=list(range(8))).results
        yT = [np.concatenate([np.asarray(resA[b * 4 + g]["yT"]) for g in range(4)], axis=0) for b in range(Bsz)]
        del resA, in_maps
        last = (l == depth - 1)
        ncB = _build_b(NT, last)
        common = dict(wout=np.ascontiguousarray(inp["w_out"][l][perm]), wq=inp["xattn_w_q"][l], wkv=inp["xattn_w_kv"][l], wo=inp["xattn_w_o"][l],
                      wqry=inp["peer_w_query"][l], KT=make_KT(inp["peer_sub_keys"][l]), down=inp["peer_down"][l], up=inp["peer_up"][l],
                      gx=_gfm(inp["xattn_norm_g"][l]), gm=_gfm(inp["mem_norm_g"][l]), gf=_gfm(inp["ffn_norm_g"][l]), gl=_gfm(inp["final_norm_g"]))
        in_maps = []
        for core in range(8):
            b, j = divmod(core, 4)
            sl = slice(j * NT, (j + 1) * NT)
            m = dict(common)
            m["xT"] = np.ascontiguousarray(xT[b][:, sl])
            m["yT"] = np.ascontiguousarray(yT[b][:, sl])
            m["memT"] = memT[b]
            in_maps.append(m)
        resB = run_bass_kernel_spmd(ncB, in_maps, core_ids=list(range(8))).results
        for core in range(8):
            b, j = divmod(core, 4)
            xT[b][:, j * NT:(j + 1) * NT] = np.asarray(resB[core]["xoT"])
        del resB, in_maps
    out = np.stack([np.ascontiguousarray(xT[b].T) for b in range(Bsz)], axis=0).astype(np.float32)
    return out
```
